# Optimizing a Trainium2 kernel written in Bass

```python
import jax, jax.numpy as jnp
from jax import lax
import numpy as np

D_MODEL = 1024
BATCH = 16
SEQ = 2048
DEPTH = 4

HEAD_DIM = 64
A_HEADS = 8
A_KV_HEADS = 2
B_GROUPS = ((128, 1), (512, 4), (2048, 16))
B_HEADS_PER_GROUP = 4
B_HEADS = len(B_GROUPS) * B_HEADS_PER_GROUP
GRID_W = 64
ROPE_THETA = 10000.0
Q_BLOCK = 128
N_EXPERTS = 16
N_GROUPS = 4
EXPERTS_PER_GROUP = N_EXPERTS // N_GROUPS
TOP_K = 2
D_EXPERT = 256
LN_EPS = 1e-5
RMS_EPS = 1e-6
DN_ALPHA = (2.0 * DEPTH) ** 0.25
DN_BETA = (8.0 * DEPTH) ** -0.25

A_Q = A_HEADS * HEAD_DIM
A_KV = A_KV_HEADS * HEAD_DIM
B_W = B_HEADS * HEAD_DIM
B_OUT = B_HEADS_PER_GROUP * HEAD_DIM
N_IN = A_Q + 2 * A_KV + 3 * B_W + 2 * D_MODEL
IN_SPLITS = (A_Q, A_Q + A_KV, A_Q + 2 * A_KV, A_Q + 2 * A_KV + B_W,
             A_Q + 2 * A_KV + 2 * B_W, A_Q + 2 * A_KV + 3 * B_W)

kernel_name = "hybrid_gqa_dilated_moe_encoder"


def _layer_norm(x, g=None, b=None):
    xf = x.astype(jnp.float32)
    mu = jnp.mean(xf, -1, keepdims=True)
    var = jnp.mean(jnp.square(xf - mu), -1, keepdims=True)
    y = (xf - mu) * lax.rsqrt(var + LN_EPS)
    if g is not None:
        y = y * g.astype(jnp.float32) + b.astype(jnp.float32)
    return y.astype(x.dtype)


def _rms_norm(x, g):
    xf = x.astype(jnp.float32)
    y = xf * lax.rsqrt(jnp.mean(jnp.square(xf), -1, keepdims=True) + RMS_EPS)
    return (y * g.astype(jnp.float32)).astype(x.dtype)


def _axial_rope_tables(seq):
    rows = seq // GRID_W
    row = jnp.repeat(jnp.arange(rows), GRID_W).astype(jnp.float32)
    col = jnp.tile(jnp.arange(GRID_W), rows).astype(jnp.float32)
    axis_dim = HEAD_DIM // 2
    inv = ROPE_THETA ** (-jnp.arange(0, axis_dim, 2, dtype=jnp.float32) / axis_dim)
    ang = jnp.concatenate([row[:, None] * inv, col[:, None] * inv], -1)
    return jnp.cos(ang), jnp.sin(ang)


def _apply_rope(x, cos, sin):
    xf = x.astype(jnp.float32).reshape(x.shape[:-1] + (HEAD_DIM // 2, 2))
    x0, x1 = xf[..., 0], xf[..., 1]
    c = cos[None, :, None, :]
    s = sin[None, :, None, :]
    out = jnp.stack([x0 * c - x1 * s, x0 * s + x1 * c], -1).reshape(x.shape)
    return out.astype(x.dtype)


def _alibi_slopes(n):
    return jnp.exp2(-8.0 * jnp.arange(1, n + 1, dtype=jnp.float32) / n)


def _global_gqa(q, k, v):
    b, s = q.shape[0], q.shape[1]
    rep = A_HEADS // A_KV_HEADS
    nblk = s // Q_BLOCK
    scale = HEAD_DIM ** -0.5
    qb = q.reshape(b, nblk, Q_BLOCK, A_KV_HEADS, rep, HEAD_DIM).transpose(1, 0, 2, 3, 4, 5)

    def block(qi):
        sc = jnp.einsum('bqgrd,bkgd->bgrqk', qi, k).astype(jnp.float32) * scale
        p = jax.nn.softmax(sc, axis=-1)
        return jnp.einsum('bgrqk,bkgd->bqgrd', p.astype(v.dtype), v)

    o = lax.map(block, qb)
    return o.transpose(1, 0, 2, 3, 4, 5).reshape(b, s, A_Q)


def _dilated_group(q, k, v, window, dilation, slopes):
    b, s, h = q.shape[0], q.shape[1], q.shape[2]
    reach = window // (2 * dilation)
    offs = dilation * jnp.arange(-reach, reach + 1)
    nblk = s // Q_BLOCK
    scale = HEAD_DIM ** -0.5
    qb = q.reshape(b, nblk, Q_BLOCK, h, HEAD_DIM).transpose(1, 0, 2, 3, 4)
    starts = jnp.arange(nblk) * Q_BLOCK
    bias = -slopes[:, None] * jnp.abs(offs).astype(jnp.float32)[None, :]

    def block(args):
        qi, t0 = args
        pos = t0 + jnp.arange(Q_BLOCK)[:, None] + offs[None, :]
        valid = (pos >= 0) & (pos < s)
        idx = jnp.clip(pos, 0, s - 1)
        kg = jnp.take(k, idx, axis=1)
        vg = jnp.take(v, idx, axis=1)
        sc = jnp.einsum('bqhd,bqkhd->bhqk', qi, kg).astype(jnp.float32) * scale + bias[None, :, None, :]
        sc = jnp.where(valid[None, None], sc, -jnp.inf)
        lse = jax.nn.logsumexp(sc, axis=-1)
        p = jnp.exp(sc - lse[..., None])
        o = jnp.einsum('bhqk,bqkhd->bqhd', p.astype(v.dtype), vg)
        return o, lse.transpose(0, 2, 1)

    o, lse = lax.map(block, (qb, starts))
    o = o.transpose(1, 0, 2, 3, 4).reshape(b, s, h, HEAD_DIM)
    lse = lse.transpose(1, 0, 2, 3).reshape(b, s, h)
    return o, lse


def _mixer(h, w_in, qn_g, kn_g, w_pa, w_pb, w_o, cos, sin, slopes):
    b, s, _ = h.shape
    proj = h @ w_in
    qa, ka, va, qd, kd, vd, gates = jnp.split(proj, IN_SPLITS, axis=-1)
    qa = _apply_rope(_rms_norm(qa.reshape(b, s, A_HEADS, HEAD_DIM), qn_g), cos, sin)
    ka = _apply_rope(_rms_norm(ka.reshape(b, s, A_KV_HEADS, HEAD_DIM), kn_g), cos, sin)
    va = va.reshape(b, s, A_KV_HEADS, HEAD_DIM)
    out_a = _global_gqa(qa, ka, va)
    qd = qd.reshape(b, s, B_HEADS, HEAD_DIM)
    kd = kd.reshape(b, s, B_HEADS, HEAD_DIM)
    vd = vd.reshape(b, s, B_HEADS, HEAD_DIM)
    outs, lses = [], []
    for g, (window, dilation) in enumerate(B_GROUPS):
        sl = slice(g * B_HEADS_PER_GROUP, (g + 1) * B_HEADS_PER_GROUP)
        o, l = _dilated_group(qd[:, :, sl], kd[:, :, sl], vd[:, :, sl], window, dilation, slopes[sl])
        outs.append(o)
        lses.append(l)
    wts = jax.nn.softmax(jnp.stack(lses, 0), axis=0)
    out_b = jnp.sum(wts[..., None].astype(h.dtype) * jnp.stack(outs, 0), 0).reshape(b, s, B_OUT)
    g_a, g_b = jnp.split(jax.nn.sigmoid(gates), 2, axis=-1)
    merged = g_a * (out_a @ w_pa) + g_b * (out_b @ w_pb)
    return merged @ w_o


def _moe(h, w_router, router_bias, w_gate, w_up, w_down):
    b, s, d = h.shape
    t = h.reshape(b * s, d)
    scores = jax.nn.sigmoid((t @ w_router).astype(jnp.float32))
    sel = scores + router_bias.astype(jnp.float32)
    grp_score = jnp.sum(lax.top_k(sel.reshape(-1, N_GROUPS, EXPERTS_PER_GROUP), TOP_K)[0], -1)
    best = jnp.argmax(grp_score, axis=-1)
    in_group = (jnp.arange(N_EXPERTS) // EXPERTS_PER_GROUP)[None, :] == best[:, None]
    _, idx = lax.top_k(jnp.where(in_group, sel, -jnp.inf), TOP_K)
    top_s = jnp.take_along_axis(scores, idx, axis=-1)
    gate = top_s / jnp.sum(top_s, -1, keepdims=True)
    combine = jnp.sum(jax.nn.one_hot(idx, N_EXPERTS, dtype=jnp.float32) * gate[..., None], 1)
    hg = jnp.einsum('td,edf->tef', t, w_gate)
    hu = jnp.einsum('td,edf->tef', t, w_up)
    act = jax.nn.silu(hg) * hu * combine[..., None].astype(t.dtype)
    y = jnp.einsum('tef,efd->td', act, w_down)
    return y.reshape(b, s, d)


def setup_inputs(seed: int = 0) -> dict:
    key = jax.random.key(seed)
    ks = jax.random.split(key, 24)
    f32 = jnp.float32
    n = lambda k, shape, sc: jax.random.normal(k, shape, f32) * sc
    d = D_MODEL
    return {
        "x": n(ks[0], (BATCH, SEQ, d), 1.0),
        "c": n(ks[1], (BATCH, d), 1.0),
        "w_ada": n(ks[2], (DEPTH, d, 6 * d), 0.5 * d ** -0.5),
        "b_ada": n(ks[3], (DEPTH, 6 * d), 0.02),
        "w_in": n(ks[4], (DEPTH, d, N_IN), d ** -0.5),
        "q_norm_g": 1.0 + n(ks[5], (DEPTH, HEAD_DIM), 0.02),
        "k_norm_g": 1.0 + n(ks[6], (DEPTH, HEAD_DIM), 0.02),
        "w_branch_a": n(ks[7], (DEPTH, A_Q, d), A_Q ** -0.5),
        "w_branch_b": n(ks[8], (DEPTH, B_OUT, d), B_OUT ** -0.5),
        "w_out": n(ks[9], (DEPTH, d, d), DN_BETA * d ** -0.5),
        "ln1_g": 1.0 + n(ks[10], (DEPTH, d), 0.02),
        "ln1_b": n(ks[11], (DEPTH, d), 0.02),
        "w_router": n(ks[12], (d, N_EXPERTS), d ** -0.5),
        "router_bias": n(ks[13], (N_EXPERTS,), 0.01),
        "w_exp_gate": n(ks[14], (DEPTH, N_EXPERTS, d, D_EXPERT), d ** -0.5),
        "w_exp_up": n(ks[15], (DEPTH, N_EXPERTS, d, D_EXPERT), d ** -0.5),
        "w_exp_down": n(ks[16], (DEPTH, N_EXPERTS, D_EXPERT, d), DN_BETA * D_EXPERT ** -0.5),
        "ln2_g": 1.0 + n(ks[17], (DEPTH, d), 0.02),
        "ln2_b": n(ks[18], (DEPTH, d), 0.02),
    }


def reference(x, c, w_ada, b_ada, w_in, q_norm_g, k_norm_g, w_branch_a, w_branch_b, w_out,
              ln1_g, ln1_b, w_router, router_bias, w_exp_gate, w_exp_up, w_exp_down, ln2_g, ln2_b):
    s = x.shape[1]
    cos, sin = _axial_rope_tables(s)
    slopes = _alibi_slopes(B_HEADS)
    cond = jax.nn.silu(c)
    for l in range(DEPTH):
        mod = cond @ w_ada[l] + b_ada[l]
        sh1, sc1, g1, sh2, sc2, g2 = [m[:, None, :] for m in jnp.split(mod, 6, axis=-1)]
        h = _layer_norm(x) * (1.0 + sc1) + sh1
        mix = _mixer(h, w_in[l], q_norm_g[l], k_norm_g[l], w_branch_a[l], w_branch_b[l],
                     w_out[l], cos, sin, slopes)
        x = _layer_norm(DN_ALPHA * x + g1 * mix, ln1_g[l], ln1_b[l])
        h = _layer_norm(x) * (1.0 + sc2) + sh2
        ffn = _moe(h, w_router, router_bias, w_exp_gate[l], w_exp_up[l], w_exp_down[l])
        x = _layer_norm(DN_ALPHA * x + g2 * ffn, ln2_g[l], ln2_b[l])
    return x
```

```python
import numpy as np
from contextlib import ExitStack
import concourse.bass as bass
import concourse.mybir as mybir
from concourse.bass_utils import run_bass_kernel_spmd

F32 = mybir.dt.float32
BF16 = mybir.dt.bfloat16
ALU = mybir.AluOpType
AF = mybir.ActivationFunctionType
AX = mybir.AxisListType

ENGS = ("pe", "act", "dve", "pool", "sp")

DEPTH = 4
S = 2048
D = 1024
NT = 16
ALPHA = (2.0 * DEPTH) ** 0.25
B_GROUPS = ((128, 1), (512, 4), (2048, 16))
import os as _os
_DBG_GROUPS = [int(c_) for c_ in _os.environ.get('DBG_GROUPS', '012')]
_P3 = int(_os.environ.get('DBG_P3', '99'))
_GATHER = int(_os.environ.get('KGATHER', '0'))
_SAME_ENG_FIFO = _os.environ.get('KFIFO', '').split(',')
_SUB = int(_os.environ.get('DBG_SUB', '99'))


_op_counter = [0]


class _Op:
    __slots__ = ("eng", "fn", "deps", "needed", "semval", "dma_sem", "dma_val", "ndma", "semidx")

    def __init__(self, eng, fn, deps):
        _op_counter[0] += 1
        self.semidx = _op_counter[0]
        self.eng = eng
        self.fn = fn
        self.deps = deps
        self.needed = False
        self.semval = 0
        self.dma_sem = None
        self.dma_val = 0
        self.ndma = 0


def _base(k):
    return k[0] if isinstance(k, tuple) else k


class Prog:
    def __init__(self, nc):
        self.nc = nc
        self.ops = {e: [] for e in ENGS}
        self.last_w = {}
        self.readers = {}
        self.by_base = {}
        self.base_deps = {}
        self.dma_slots = {}
        self.n_dma_sems = 0

    def _deps(self, reads, writes, eng=None):
        deps = []
        for r in reads:
            w = self.last_w.get(r)
            if w is not None:
                deps.append(w)
            if _base(r) == "ps":
                for o in self.readers.get(r, ()):
                    if o.eng != eng:
                        deps.append(o)
            bd = self.base_deps.get(_base(r))
            if bd:
                deps.extend(bd)
        for w_ in writes:
            w = self.last_w.get(w_)
            if w is not None:
                deps.append(w)
            rs = self.readers.get(w_)
            if rs:
                deps.extend(rs)
            bd = self.base_deps.get(_base(w_))
            if bd:
                deps.extend(bd)
        return deps

    def _track(self, o, reads, writes):
        for r in reads:
            self.readers.setdefault(r, []).append(o)
            self.by_base.setdefault(_base(r), set()).add(r)
        for w in writes:
            self.last_w[w] = o
            self.readers[w] = []
            self.by_base.setdefault(_base(w), set()).add(w)

    @staticmethod
    def _compress(deps):
        best = {}
        for d in deps:
            if d.dma_sem is not None:
                k = ("d", d.dma_sem)
                prev = best.get(k)
                if prev is None or d.dma_val > prev.dma_val:
                    best[k] = d
            else:
                prev = best.get(d.eng)
                if prev is None or d.semidx > prev.semidx:
                    best[d.eng] = d
        return list(best.values())

    def op(self, eng, fn, reads=(), writes=()):
        deps = self._compress(self._deps(reads, writes, eng))
        o = _Op(eng, fn, deps)
        for d in deps:
            d.needed = True
        self.ops[eng].append(o)
        self._track(o, reads, writes)
        return o

    def dma(self, queue, fn, reads=(), writes=(), slot=None, n=1):
        deps = self._compress(self._deps(reads, writes))
        o = _Op(queue, fn, deps)
        for d in deps:
            d.needed = True
        if slot is None:
            if writes:
                slot = ("w", writes[0])
            else:
                self._auto = getattr(self, "_auto", 0) + 1
                slot = ("auto", self._auto % 16)
        st = self.dma_slots.get(slot)
        if st is None:
            st = [self.n_dma_sems, 0]
            self.n_dma_sems += 1
            self.dma_slots[slot] = st
        st[1] += 16 * n
        o.dma_sem = st[0]
        o.dma_val = st[1]
        o.ndma = n
        self.ops[queue].append(o)
        self._track(o, reads, writes)
        return o

    def recycle(self, new_base, old_bases):
        acc = {}
        dmas = []

        def add(o):
            if o.dma_sem is not None:
                dmas.append(o)
            else:
                prev = acc.get(o.eng)
                if prev is None or o.semidx > prev.semidx:
                    acc[o.eng] = o

        for ob in old_bases:
            for o in self.base_deps.get(ob, []):
                add(o)
            for k in self.by_base.get(ob, ()):
                for o in self.readers.get(k, []):
                    add(o)
                w = self.last_w.get(k)
                if w is not None:
                    add(w)
        for k in self.by_base.get(new_base, ()):
            self.last_w.pop(k, None)
            self.readers.pop(k, None)
        self.base_deps[new_base] = self._compress(list(acc.values()) + dmas)
        self.by_base[new_base] = set()

    def emit(self, final_wait_ops=()):
        nc = self.nc
        with ExitStack() as es:
            EPOCH = 12000
            dsem = [es.enter_context(nc.semaphore("d_%d" % i)) for i in range(self.n_dma_sems)]
            esem = {}
            for e in ENGS:
                c = 0
                for o in self.ops[e]:
                    if o.dma_sem is None and o.needed:
                        o.semval = (c // EPOCH, c % EPOCH + 1)
                        c += 1
                esem[e] = [es.enter_context(nc.semaphore("s_%s_%d" % (e, i))) for i in range(c // EPOCH + 1)]
            self.sem_counts = {e: len(v) for e, v in esem.items()}
            block = es.enter_context(nc.Block())

            def run(eng_name):
                def body(eng):
                    waited = {}
                    for o in self.ops[eng_name]:
                        for d in o.deps:
                            if d.dma_sem is not None:
                                key = ("d", d.dma_sem)
                                val = d.dma_val
                                sem = dsem[d.dma_sem]
                            else:
                                if d.eng == eng_name and (eng_name == "pe" or eng_name in _SAME_ENG_FIFO):
                                    continue
                                key = d.eng
                                val = d.semval
                                if waited.get(key, (-1, 0)) >= val:
                                    continue
                                waited[key] = val
                                eng.wait_ge(esem[d.eng][val[0]], val[1])
                                continue
                            if waited.get(key, 0) >= val:
                                continue
                            waited[key] = val
                            eng.wait_ge(sem, val)
                        if o.dma_sem is not None:
                            insts = o.fn(eng)
                            assert len(insts) == o.ndma, (len(insts), o.ndma)
                            for ins in insts:
                                ins.then_inc(dsem[o.dma_sem], 16)
                        else:
                            ins = o.fn(eng)
                            if o.needed:
                                ins.then_inc(esem[eng_name][o.semval[0]], 1)
                    if eng_name == "sp":
                        for d in final_wait_ops:
                            eng.wait_ge(dsem[d.dma_sem], d.dma_val)
                return body

            block.tensor(run("pe"))
            block.scalar(run("act"))
            block.vector(run("dve"))
            block.gpsimd(run("pool"))
            block.sync(run("sp"))


class Arena:
    def __init__(self, p, ap2d, name):
        self.p = p
        self.ap = ap2d
        self.n = ap2d.shape[1]
        self.name = name
        self.regions = []

    def alloc(self, base, lo_bytes, free_shape, dt):
        nel = int(np.prod(free_shape))
        nb = nel * (4 if dt == F32 else 2)
        lo = lo_bytes // 2
        hi = lo + nb // 2
        assert hi <= self.n, (self.name, base, hi, self.n)
        old = []
        keep = []
        for (l, h, b) in self.regions:
            if l < hi and lo < h:
                if b not in old:
                    old.append(b)
                if l < lo:
                    keep.append((l, lo, b))
                if h > hi:
                    keep.append((hi, h, b))
            else:
                keep.append((l, h, b))
        keep.append((lo, hi, base))
        self.regions = keep
        self.p.recycle(base, old)
        v = self.ap[:, lo:hi]
        if dt == F32:
            v = v.bitcast(F32)
        if len(free_shape) == 2:
            v = v.rearrange("p (a b) -> p a b", b=free_shape[1])
        elif len(free_shape) == 3:
            v = v.rearrange("p (a b c) -> p a b c", b=free_shape[1], c=free_shape[2])
        return v


def _const_tables():
    pos = np.arange(S)
    row = (pos // 64).astype(np.float32)
    col = (pos % 64).astype(np.float32)
    inv = (10000.0 ** (-np.arange(0, 32, 2, dtype=np.float32) / 32.0)).astype(np.float32)
    ang = np.concatenate([row[:, None] * inv, col[:, None] * inv], -1).astype(np.float32)
    cos = np.cos(ang).astype(np.float32).reshape(NT, 128, 32).transpose(1, 0, 2)
    sin = np.sin(ang).astype(np.float32).reshape(NT, 128, 32).transpose(1, 0, 2)
    slopes = np.exp2(-8.0 * np.arange(1, 13, dtype=np.float32) / 12.0).astype(np.float32)
    i = np.arange(128)[:, None]
    c = np.arange(384)[None, :]
    delta = np.abs(c - 128 - i).astype(np.float32)
    E = np.zeros((128, 12, 384), np.float32)
    for h in range(12):
        d = B_GROUPS[h // 4][1]
        E[:, h, :] = np.where(delta <= 64, np.exp(-slopes[h] * d * delta), 0.0)
    sel = np.zeros((128, 16, 128), np.float32)
    for e in range(16):
        sel[e, e, :] = 1.0
    return np.ascontiguousarray(cos), np.ascontiguousarray(sin), E, sel


def build_nc(n_layers=DEPTH, n_seq=2, stage=99, dbg=None, n_cores=8):
    nc = bass.Bass("TRN2", target_bir_lowering=False)
    dbg = dbg or {}

    def din(name, shape, dt=F32):
        return nc.dram_tensor(name, list(shape), dt, kind="ExternalInput").ap()

    x_in = din("x", [2, S, D])
    cT_in = din("cT", [128, 8, 2])
    gather_jobs = []

    def gathered(name, rows, cols):
        if n_cores == 1 or not _GATHER:
            full = din(name, [DEPTH, rows, cols])
            return [full[l_] for l_ in range(DEPTH)]
        shd = din(name + "_sh", [DEPTH, rows // n_cores, cols])
        fulls = []
        for l_ in range(DEPTH):
            bnc = nc.dram_tensor("%s_b%d" % (name, l_), [rows // n_cores, cols], F32, kind="Internal").ap()
            ful = nc.dram_tensor("%s_f%d" % (name, l_), [rows, cols], F32, kind="Internal", addr_space="Shared").ap()
            gather_jobs.append((name, l_, shd[l_], bnc, ful))
            fulls.append(ful)
        return fulls

    w_ada = gathered("w_ada", D, 6 * D)
    b_adaT = din("b_adaT", [128, DEPTH, 48])
    w_in = gathered("w_in", D, 5120)
    qkg_in = din("qkg", [128, DEPTH, 640])
    w_po = gathered("w_po", 1792, D)
    w_pa = [w_[0:512, :] for w_ in w_po]
    w_pb = [w_[512:768, :] for w_ in w_po]
    w_o = [w_[768:1792, :] for w_ in w_po]
    lnp_in = din("lnp", [128, DEPTH, 4, D])
    w_r = din("w_r", [D, 16])
    rb_in = din("rb", [128, 16])
    w_eg = [w_.rearrange("(e d) f -> e d f", e=16) for w_ in gathered("w_eg", 16 * D, 256)]
    w_eu = [w_.rearrange("(e d) f -> e d f", e=16) for w_ in gathered("w_eu", 16 * D, 256)]
    w_ed = [w_.rearrange("(e f) d -> e f d", e=16) for w_ in gathered("w_ed", 16 * 256, D)]
    cos_in = din("cos_t", [128, NT, 32])
    sin_in = din("sin_t", [128, NT, 32])
    E_in = din("E_t", [128, 12, 384])
    sel_in = din("sel_t", [128, 16, 128])
    idn_in = din("idn", [128, 128])
    y_out = nc.dram_tensor("y", [2, S, D], F32, kind="ExternalOutput").ap()
    xs = nc.dram_tensor("xs", [2, S, D], F32, kind="Internal").ap()
    dbg_t = {k: nc.dram_tensor("dbg_" + k, list(shp), F32, kind="ExternalOutput").ap() for k, shp in dbg.items()}

    es = ExitStack()
    with es:
        def sb(name, shape, dt):
            return es.enter_context(nc.sbuf_tensor(name, list(shape), dt))

        p = Prog(nc)
        finals = []

        hT = sb("hT", [128, 8, S], BF16)
        ACT_A = sb("arenaA", [128, 28 * 1024], BF16)
        W_A = sb("arenaW", [128, 31 * 1024], BF16)
        T_A = sb("arenaT", [128, 15 * 1024 + 512], BF16)
        identf = sb("identf", [128, 128], F32)
        identb = sb("identb", [128, 128], BF16)
        onesf = sb("onesf", [128, 128], F32)
        selb = sb("selb", [128, 16, 128], BF16)
        combT = sb("combT", [128, S], BF16)
        modT = sb("modT", [128, 48, 2], F32)
        badaT = sb("badaT", [128, DEPTH, 48], F32)
        condT = sb("condT", [128, 8, 2], F32)
        lnp = sb("lnp_sb", [128, 2, D], F32)
        gbc = sb("gbc", [128, D], F32)
        wR = sb("wR", [128, 8, 16], F32)
        rb = sb("rb_sb", [128, 16], F32)
        small = sb("small", [128, 256], F32)
        diag = sb("diag", [128, 2, 128], F32)
        ps_all = es.enter_context(nc.psum_tensor("ps_all", [128, 8 * 512], F32))

        arA = Arena(p, ACT_A[:], "A")
        arW = Arena(p, W_A[:], "W")
        arT = Arena(p, T_A[:], "T")

        def PS(b, n=512, off=0):
            return ps_all[:, b * 512 + off: b * 512 + off + n]

        def PSB(b, nbanks=1):
            return ps_all[:, b * 512:(b + nbanks) * 512].bitcast(BF16)

        psk = lambda b: ("ps", b)
        bank_rr = [0]
        bank_lim = [8]

        def nb(n=1):
            b = bank_rr[0]
            if b % n:
                b += n - (b % n)
            if b + n > bank_lim[0]:
                b = 0
            bank_rr[0] = (b + n) % bank_lim[0]
            return b

        order = {"w_ada": 0, "w_in": 1, "w_po": 2, "w_eg": 3, "w_eu": 4, "w_ed": 5}
        for (name_, l_, shd_, bnc_, ful_) in sorted(gather_jobs, key=lambda j_: (j_[1], order[j_[0]])):
            if l_ >= n_layers:
                continue
            p.dma("sp", lambda e, shd_=shd_, bnc_=bnc_: [e.dma_start(out=bnc_, in_=shd_)], writes=[(name_, "b", l_)], slot=("gb", l_ % 2, name_))
            p.dma("pool", lambda e, bnc_=bnc_, ful_=ful_: [e.collective_compute("AllGather", ALU.bypass, replica_groups=[list(range(n_cores))],
                                                                                 ins=[bnc_], outs=[ful_])],
                  reads=[(name_, "b", l_)], writes=[(name_, "f", l_)], slot=("gf", l_ % 2, name_))

        p.dma("sp", lambda e: [e.dma_start(out=identf[:], in_=idn_in[:])], writes=["identf"])
        p.op("dve", lambda e: e.tensor_copy(out=identb[:], in_=identf[:]), reads=["identf"], writes=["identb"])
        p.op("dve", lambda e: e.memset(onesf[:], 1.0), writes=["onesf"])
        p.op("dve", lambda e: e.memset(combT[:], 0.0), writes=["combT"])
        p.dma("pool", lambda e: [e.dma_start(out=selb[:], in_=sel_in[:])], writes=["selb"])
        p.dma("sp", lambda e: [e.dma_start(out=badaT[:], in_=b_adaT[:])], writes=["badaT"])
        p.dma("sp", lambda e: [e.dma_start(out=condT[:], in_=cT_in[:])], writes=["condT"])
        p.dma("sp", lambda e: [e.dma_start(out=wR[:], in_=w_r.rearrange("(k p) n -> p k n", p=128))], writes=["wR"])
        p.dma("sp", lambda e: [e.dma_start(out=rb[:], in_=rb_in[:])], writes=["rb"])
        p.op("act", lambda e: e.activation(out=condT[:], in_=condT[:], func=AF.Silu), reads=["condT"], writes=["condT"])

        def dbg_out(name, src_ap, reads):
            if name in dbg_t:
                finals.append(p.dma("sp", lambda e: [e.dma_start(out=dbg_t[name], in_=src_ap)], reads=reads))

        def ln_stats(src, mv, rstd, key_src, tag):
            st = small[:, 0:12].rearrange("p (a b) -> p a b", b=6)
            for i in range(2):
                p.op("dve", lambda e, i=i: e.bn_stats(out=st[:, i, :], in_=src[:, i * 512:(i + 1) * 512]),
                     reads=[key_src], writes=[("st", i)])
            p.op("dve", lambda e: e.bn_aggr(out=mv, in_=small[:, 0:12]), reads=[("st", 0), ("st", 1)], writes=[("mv", tag)])
            p.op("act", lambda e: e.activation(out=rstd, in_=mv[:, 1:2], func=AF.Sqrt, bias=1e-5, scale=1.0),
                 reads=[("mv", tag)], writes=[("rstd", tag)])
            p.op("dve", lambda e: e.reciprocal(out=rstd, in_=rstd), reads=[("rstd", tag)], writes=[("rstd", tag)])
            p.op("dve", lambda e: e.scalar_tensor_tensor(out=small[:, 19:20], in0=mv[:, 0:1], scalar=-1.0, in1=rstd, op0=ALU.mult, op1=ALU.mult),
                 reads=[("mv", tag), ("rstd", tag)], writes=[("nbias", tag)])

        def norm_act(out_ap, in_ap, rstd, tag, reads, writes):
            p.op("act", lambda e: e.activation(out=out_ap, in_=in_ap, func=AF.Identity, bias=small[:, 19:20], scale=rstd),
                 reads=list(reads) + [("rstd", tag), ("nbias", tag)], writes=list(writes))

        def make_bc(dst, chunk0, b):
            for half in range(2):
                bk = nb()
                for cc in range(4):
                    c = half * 4 + cc
                    dg = diag[:, c % 2, :]
                    p.op("dve", lambda e, dg=dg, c=c: e.tensor_scalar(out=dg, in0=identf[:], scalar1=modT[:, chunk0 + c, b:b + 1],
                                                                      scalar2=None, op0=ALU.mult),
                         reads=["identf", "modT"], writes=[("diag", c % 2)])
                    p.op("pe", lambda e, dg=dg, cc=cc, bk=bk: e.matmul(PS(bk, 128, cc * 128), lhsT=onesf[:], rhs=dg, start=True, stop=True),
                         reads=["onesf", ("diag", c % 2)], writes=[psk(bk)])
                p.op("act", lambda e, half=half, bk=bk: e.copy(out=dst[:, half * 512:(half + 1) * 512], in_=PS(bk)),
                     reads=[psk(bk)], writes=["gbc"])

        def phase0(l):
            wst = [arW.alloc("wada%d" % i, 44 * 1024 + i * 8192, [8, 256], F32) for i in range(2)]
            bkm = nb()
            for s_ in range(24):
                buf = wst[s_ % 2]
                key = ("wada", s_ % 2)
                p.dma("sp", lambda e, buf=buf, s_=s_: [e.dma_start(out=buf, in_=w_ada[l][:, s_ * 256:(s_ + 1) * 256].rearrange("(k p) n -> p k n", p=128))],
                      reads=[("w_ada", "f", l)], writes=["wada%d" % (s_ % 2)], slot=key)
                for jj in range(2):
                    j = 2 * s_ + jj
                    for k in range(8):
                        p.op("pe", lambda e, buf=buf, jj=jj, j=j, k=k: e.matmul(PS(bkm, 2, 2 * j), lhsT=buf[:, k, jj * 128:(jj + 1) * 128],
                                                                                 rhs=condT[:, k, :], start=(k == 0), stop=(k == 7)),
                             reads=["wada%d" % (s_ % 2), "condT"], writes=[psk(bkm)])
            p.op("dve", lambda e: e.tensor_tensor(out=modT[:], in0=PS(bkm, 96).rearrange("p (j b) -> p j b", b=2),
                                                  in1=badaT[:, l, :].unsqueeze(2).to_broadcast([128, 48, 2]), op=ALU.add),
                 reads=[psk(bkm), "badaT"], writes=["modT"])
            for c0 in (8, 32):
                p.op("dve", lambda e, c0=c0: e.tensor_scalar(out=modT[:, c0:c0 + 8, :], in0=modT[:, c0:c0 + 8, :], scalar1=1.0, scalar2=None, op0=ALU.add),
                     reads=["modT"], writes=["modT"])
            if l == 0:
                dbg_out("modT", modT[:], ["modT"])

        def block(l, b):
                x_src = x_in if l == 0 else xs
                last = (l == n_layers - 1)
                x_dst = y_out if last else xs
                xkey = lambda t: ("xs", b, t)

                xt = [arT.alloc("xt%d" % i, i * 4096, [D], F32) for i in range(3)]
                xh = [arT.alloc("xh%d" % i, 12288 + i * 2048, [D], BF16) for i in range(2)]
                mv = small[:, 16:18]
                rstd = small[:, 18:19]
                for tp in range(NT // 2):
                    bk = nb(2)
                    pTv = PSB(bk, 2).rearrange("p (c n) -> p c n", n=256)
                    for ti in range(2):
                        t = tp * 2 + ti
                        xb = xt[t % 3]
                        xk = "xt%d" % (t % 3)
                        p.dma("sp", lambda e, xb=xb, t=t: [e.dma_start(out=xb, in_=x_src[b, t * 128:(t + 1) * 128, :])],
                              reads=[xkey(t)], writes=[xk], slot=("xt", t % 3))
                        ln_stats(xb, mv, rstd, xk, "p1")
                        hb = xh[t % 2]
                        hk = "xh%d" % (t % 2)
                        norm_act(hb, xb, rstd, "p1", [xk], [hk])
                        for c in range(8):
                            p.op("pe", lambda e, hb=hb, c=c, ti=ti, pTv=pTv: e.transpose(out=pTv[:, c, ti * 128:(ti + 1) * 128], in_=hb[:, c * 128:(c + 1) * 128], identity=identb[:]),
                                 reads=[hk, "identb"], writes=[psk(bk + c // 4)])
                    for c in range(8):
                        dst = hT[:, c, tp * 256:(tp + 1) * 256]
                        if c < 4:
                            p.op("act", lambda e, c=c, dst=dst, pTv=pTv: e.activation(out=dst, in_=pTv[:, c, :], func=AF.Identity,
                                                                                    bias=modT[:, c, b:b + 1], scale=modT[:, 8 + c, b:b + 1]),
                                 reads=[psk(bk + c // 4), "modT"], writes=[("hT", tp)])
                        else:
                            p.op("dve", lambda e, c=c, dst=dst, pTv=pTv: e.tensor_scalar(out=dst, in0=pTv[:, c, :], scalar1=modT[:, 8 + c, b:b + 1],
                                                                                       scalar2=modT[:, c, b:b + 1], op0=ALU.mult, op1=ALU.add),
                                 reads=[psk(bk + c // 4), "modT"], writes=[("hT", tp)])
                if l == 0 and b == 0:
                    dbg_out("hT", None, None) if False else None
                hT_all = [("hT", tp) for tp in range(8)]
                if stage <= 1:
                    return

                out_aT = arA.alloc("out_aT", 0, [4, S], BF16)
                out_bT = arA.alloc("out_bT", 16384, [2, S], BF16)
                qaT = arA.alloc("qaT", 24576, [4, S], BF16)
                kaT = arA.alloc("kaT", 40960, [S], BF16)
                vaP = arA.alloc("vaP", 45056, [NT, 2, 128], BF16)
                wA = arW.alloc("wA", 0, [8, 768], BF16)
                Et = arW.alloc("Et", 48 * 1024, [12, 384], BF16)
                cos_t = arW.alloc("cos", 16 * 1024, [NT, 32], F32)
                sin_t = arW.alloc("sin", 18 * 1024, [NT, 32], F32)
                qkg = arW.alloc("qkg", 12 * 1024, [640], F32)
                p.dma("pool", lambda e: [e.dma_start(out=Et, in_=E_in[:])], writes=["Et"])
                p.dma("sp", lambda e: [e.dma_start(out=cos_t, in_=cos_in[:])], writes=["cos"])
                p.dma("sp", lambda e: [e.dma_start(out=sin_t, in_=sin_in[:])], writes=["sin"])
                p.dma("sp", lambda e: [e.dma_start(out=qkg, in_=qkg_in[:, l, :])], writes=["qkg"])
                p.dma("pool", lambda e: [e.dma_start(out=wA, in_=w_in[l][:, 0:768].rearrange("(k p) n -> p k n", p=128))], reads=[("w_in", "f", l)], writes=["wA"])
                sq = arT.alloc("sq", 0, [640], F32)
                qn = arT.alloc("qn", 2560, [10, 64], F32)
                qr = arT.alloc("qr", 5120, [10, 64], BF16)
                ra = arT.alloc("ra", 6400, [10, 32], F32)
                rb_ = arT.alloc("rb_", 7680, [10, 32], F32)
                pt = [arT.alloc("pt%d" % i, 9216 + i * 1024, [512], BF16) for i in range(4)]
                Rr = [arT.alloc("Rr%d" % i, 13312 + i * 2048, [512], F32) for i in range(2)]
                ss = small[:, 32:42]
                rr = small[:, 48:58]
                p.op("pool", lambda e: e.memset(vaP, 1.0), writes=["vaP"])
                for t in range(NT):
                    bq = nb()
                    bkv = nb()
                    tp = t // 2
                    for k in range(8):
                        lh = hT[:, k, t * 128:(t + 1) * 128]
                        p.op("pe", lambda e, lh=lh, k=k, bq=bq: e.matmul(PS(bq), lhsT=lh, rhs=wA[:, k, 0:512], start=(k == 0), stop=(k == 7)),
                             reads=[("hT", tp), "wA"], writes=[psk(bq)])
                        p.op("pe", lambda e, lh=lh, k=k, bkv=bkv: e.matmul(PS(bkv, 256), lhsT=lh, rhs=wA[:, k, 512:768], start=(k == 0), stop=(k == 7)),
                             reads=[("hT", tp), "wA"], writes=[psk(bkv)])
                    p.op("act", lambda e, bq=bq: e.activation(out=sq[:, 0:512], in_=PS(bq), func=AF.Square), reads=[psk(bq)], writes=["sq"])
                    p.op("act", lambda e, bkv=bkv: e.activation(out=sq[:, 512:640], in_=PS(bkv, 128), func=AF.Square), reads=[psk(bkv)], writes=["sq"])
                    p.op("dve", lambda e: e.tensor_reduce(out=ss, in_=sq.rearrange("p (h d) -> p h d", d=64), axis=AX.X, op=ALU.add),
                         reads=["sq"], writes=["ss"])
                    p.op("act", lambda e: e.activation(out=rr, in_=ss, func=AF.Sqrt, bias=1e-6, scale=1.0 / 64.0), reads=["ss"], writes=["rr"])
                    p.op("dve", lambda e: e.reciprocal(out=rr, in_=rr), reads=["rr"], writes=["rr"])
                    p.op("dve", lambda e, bq=bq: e.tensor_tensor(out=qn[:, 0:8, :].rearrange("p (c g) d -> p g c d", g=2),
                                                               in0=PS(bq).rearrange("p (g c d) -> p g c d", g=2, c=4),
                                                               in1=rr[:, 0:8].rearrange("p (g c) -> p g c", g=2).unsqueeze(3).to_broadcast([128, 2, 4, 64]), op=ALU.mult),
                         reads=[psk(bq), "rr"], writes=["qn"])
                    p.op("dve", lambda e, bkv=bkv: e.tensor_tensor(out=qn[:, 8:10, :], in0=PS(bkv, 128).rearrange("p (h d) -> p h d", d=64),
                                                                 in1=rr[:, 8:10].unsqueeze(2).to_broadcast([128, 2, 64]), op=ALU.mult),
                         reads=[psk(bkv), "rr"], writes=["qn"])
                    p.op("dve", lambda e: e.tensor_tensor(out=qn, in0=qn, in1=qkg.rearrange("p (h d) -> p h d", d=64), op=ALU.mult),
                         reads=["qn", "qkg"], writes=["qn"])
                    qv = qn.rearrange("p h (d two) -> p h d two", two=2)
                    x0 = qv[:, :, :, 0]
                    x1 = qv[:, :, :, 1]
                    cb_ = cos_t[:, t, :].unsqueeze(1).to_broadcast([128, 10, 32])
                    sb_ = sin_t[:, t, :].unsqueeze(1).to_broadcast([128, 10, 32])
                    p.op("dve", lambda e, x0=x0, cb_=cb_: e.tensor_tensor(out=ra, in0=x0, in1=cb_, op=ALU.mult), reads=["qn", "cos"], writes=["ra"])
                    p.op("dve", lambda e, x1=x1, sb_=sb_: e.tensor_tensor(out=rb_, in0=x1, in1=sb_, op=ALU.mult), reads=["qn", "sin"], writes=["rb_"])
                    p.op("dve", lambda e: e.tensor_tensor(out=qr[:, :, 0:32], in0=ra, in1=rb_, op=ALU.subtract), reads=["ra", "rb_"], writes=["qr"])
                    p.op("dve", lambda e, x0=x0, sb_=sb_: e.tensor_tensor(out=ra, in0=x0, in1=sb_, op=ALU.mult), reads=["qn", "sin", "qr"], writes=["ra"])
                    p.op("dve", lambda e, x1=x1, cb_=cb_: e.tensor_tensor(out=rb_, in0=x1, in1=cb_, op=ALU.mult), reads=["qn", "cos", "qr"], writes=["rb_"])
                    p.op("dve", lambda e: e.tensor_tensor(out=qr[:, :, 32:64], in0=ra, in1=rb_, op=ALU.add), reads=["ra", "rb_"], writes=["qr"])
                    bt = nb()
                    pTq = PSB(bt)[:, 0:640].rearrange("p (c n) -> p c n", n=128)
                    for c in range(5):
                        src = qr.rearrange("p h d -> p (h d)")[:, c * 128:(c + 1) * 128]
                        p.op("pe", lambda e, c=c, src=src, pTq=pTq: e.transpose(out=pTq[:, c, :], in_=src, identity=identb[:]),
                             reads=["qr", "identb"], writes=[psk(bt)])
                    p.op("act", lambda e, t=t, pTq=pTq: e.copy(out=qaT[:, :, t * 128:(t + 1) * 128], in_=pTq[:, 0:4, :]), reads=[psk(bt)], writes=[("qaT", t // 4)])
                    p.op("act", lambda e, t=t, pTq=pTq: e.copy(out=kaT[:, t * 128:(t + 1) * 128], in_=pTq[:, 4, :]), reads=[psk(bt)], writes=[("kaT", t)])
                    p.op("dve", lambda e, t=t, bkv=bkv: e.tensor_copy(out=vaP[:, t, 0, 0:64], in_=PS(bkv, 64, 128)), reads=[psk(bkv), "vaP"], writes=[("vaP", t)])
                    p.op("dve", lambda e, t=t, bkv=bkv: e.tensor_copy(out=vaP[:, t, 1, 64:128], in_=PS(bkv, 64, 192)), reads=[psk(bkv), "vaP"], writes=[("vaP", t)])
                if l == 0 and b == 0:
                    dbg_out("qaT", None, None) if False else None

                if stage <= 2:
                    return
                ptc = [0]
                for g in range(2):
                    pb0 = 64 * g
                    for c in range(4):
                        for qc in range(4):
                            bo = nb()
                            pend = []

                            def do_pv(kb, bs, bo=bo, g=g):
                                i = ptc[0] % 4
                                ptc[0] += 1
                                ptb = pt[i]
                                p.op("act", lambda e, bs=bs, ptb=ptb: e.activation(out=ptb, in_=PS(bs), func=AF.Exp, scale=0.125),
                                     reads=[psk(bs)], writes=["pt%d" % i])
                                p.op("pe", lambda e, kb=kb, ptb=ptb: e.matmul(PS(bo), lhsT=vaP[:, kb, g, :], rhs=ptb, start=(kb == 0), stop=(kb == NT - 1)),
                                     reads=["pt%d" % i, ("vaP", kb)], writes=[psk(bo)])

                            for kb in range(NT):
                                bs = nb()
                                while bs == bo:
                                    bs = nb()
                                p.op("pe", lambda e, kb=kb, bs=bs, c=c, qc=qc, pb0=pb0: e.matmul(
                                    PS(bs), lhsT=kaT[pb0:pb0 + 64, kb * 128:(kb + 1) * 128], rhs=qaT[pb0:pb0 + 64, c, qc * 512:(qc + 1) * 512], start=True, stop=True),
                                     reads=[("kaT", kb), ("qaT", qc)], writes=[psk(bs)])
                                pend.append((kb, bs))
                                if len(pend) > 2:
                                    do_pv(*pend.pop(0))
                            while pend:
                                do_pv(*pend.pop(0))
                            ri = (g * 16 + c * 4 + qc) % 2
                            R = Rr[ri]
                            zlo = 64 - pb0
                            p.op("dve", lambda e, R=R, bo=bo, zlo=zlo: e.reciprocal(out=R[zlo:zlo + 64, :], in_=PS(bo)[zlo:zlo + 64, :]),
                                 reads=[psk(bo)], writes=["Rr%d" % ri])
                            p.op("dve", lambda e, R=R, bo=bo, zlo=zlo, pb0=pb0, c=c, qc=qc: e.tensor_tensor(
                                out=out_aT[pb0:pb0 + 64, c, qc * 512:(qc + 1) * 512], in0=PS(bo)[pb0:pb0 + 64, :], in1=R[zlo:zlo + 64, :], op=ALU.mult),
                                 reads=[psk(bo), "Rr%d" % ri], writes=[("out_aT", qc)])
                if l == 0 and b == 0 and "out_aT" in dbg_t:
                    tmpf = arW.alloc("dbgf", 12 * 1024, [4, 1024], F32)
                    for hh in range(2):
                        p.op("dve", lambda e, hh=hh: e.tensor_copy(out=tmpf, in_=out_aT[:, :, hh * 1024:(hh + 1) * 1024]), reads=[("out_aT", q_) for q_ in range(4)], writes=["dbgf"])
                        finals.append(p.dma("sp", lambda e, hh=hh: [e.dma_start(out=dbg_t["out_aT"][:, :, hh * 1024:(hh + 1) * 1024], in_=tmpf)], reads=["dbgf"], writes=["dbgf_d"]))
                        p.op("dve", lambda e: e.memset(small[:, 200:201], 0.0), reads=["dbgf_d"], writes=["dbgf"])
                if stage <= 3:
                    return
                bank_lim[0] = 4
                bank_rr[0] = 0
                QT = arA.alloc("QT", 24 * 1024, [3, S], BF16)
                KT = arA.alloc("KT", 36 * 1024, [3, S], BF16)
                VP = [arA.alloc("VP%d" % i, 48 * 1024 + i * 4096, [NT, 128], BF16) for i in range(2)]
                wB = [arW.alloc("wB%d" % i, 12 * 1024 + i * 18 * 1024, [8, 9 * 128], BF16) for i in range(2)]
                expf = [arT.alloc("expf%d" % i, i * 1536, [384], F32) for i in range(3)]
                pt2 = [arT.alloc("pt2_%d" % i, 4608 + i * 768, [384], BF16) for i in range(3)]
                accb = arT.alloc("accb", 20 * 1024, [S], F32)
                Rr = [arT.alloc("Rr%d" % i, 13312 + i * 2048, [512], F32) for i in range(2)]
                vpc = [0]
                ec = [0]
                for jp in range(2):
                    wb = wB[jp]
                    wk = "wB%d" % jp
                    cols = []
                    for base_c in (768, 1536, 2304):
                        for m in (jp, 2 + jp, 4 + jp):
                            cols.append(base_c + m * 128)
                    p.dma("pool", lambda e, wb=wb, cols=cols: [e.dma_start(out=wb[:, :, i * 128:(i + 1) * 128],
                                                                            in_=w_in[l][:, c0:c0 + 128].rearrange("(k p) n -> p k n", p=128))
                                                               for i, c0 in enumerate(cols)], reads=[("w_in", "f", l)], writes=[wk], n=9)
                    for which, dstT, nm in ((0, QT, "QT"), (1, KT, "KT")):
                        for mi in range(3):
                            for tc in range(4):
                                bk = nb()
                                for k in range(8):
                                    p.op("pe", lambda e, bk=bk, k=k, wb=wb, which=which, mi=mi, tc=tc: e.matmul(
                                        PS(bk), lhsT=wb[:, k, (which * 3 + mi) * 128:(which * 3 + mi + 1) * 128], rhs=hT[:, k, tc * 512:(tc + 1) * 512],
                                        start=(k == 0), stop=(k == 7)), reads=[wk, ("hT", 2 * tc), ("hT", 2 * tc + 1)], writes=[psk(bk)])
                                dst = dstT[:, mi, tc * 512:(tc + 1) * 512]
                                if which == 0:
                                    p.op("act", lambda e, dst=dst, bk=bk: e.copy(out=dst, in_=PS(bk)), reads=[psk(bk)], writes=[(nm, mi, tc)])
                                else:
                                    p.op("dve", lambda e, dst=dst, bk=bk: e.tensor_copy(out=dst, in_=PS(bk)), reads=[psk(bk)], writes=[(nm, mi, tc)])
                    if l == 0 and b == 0 and jp == 0 and "KT" in dbg_t:
                        tmpk = arW.alloc("dbgk", 0, [2, S], F32)
                        g0 = _DBG_GROUPS[0]
                        p.op("dve", lambda e: e.tensor_copy(out=tmpk[:, 0, :], in_=KT[:, g0, :]), reads=[("KT", g0, q_) for q_ in range(4)], writes=["dbgk"])
                        p.op("dve", lambda e: e.tensor_copy(out=tmpk[:, 1, :], in_=QT[:, g0, :]), reads=[("QT", g0, q_) for q_ in range(4)], writes=["dbgk"])
                        finals.append(p.dma("sp", lambda e: [e.dma_start(out=dbg_t["KT"], in_=tmpk)], reads=["dbgk"], writes=["dbgk_d"]))
                        p.op("dve", lambda e: e.memset(small[:, 202:203], 0.0), reads=["dbgk_d"], writes=["dbgk"])
                    for jj in range(2):
                        j = 2 * jp + jj
                        pb0 = 64 * jj
                        zlo = 64 - pb0
                        for gi in _DBG_GROUPS:
                            d = B_GROUPS[gi][1]
                            L_ = S // d
                            nkb = L_ // 128
                            nbq = 512 // d
                            VPb = VP[vpc[0] % 2]
                            vk = "VP%d" % (vpc[0] % 2)
                            vpc[0] += 1
                            p.op("pool", lambda e, VPb=VPb: e.memset(VPb, 1.0), writes=[vk] + [(vk, q_) for q_ in range(4)])
                            for blk4 in range(4):
                                bk = nb()
                                for bi in range(4):
                                    blk = blk4 * 4 + bi
                                    r, kb = divmod(blk, nkb)
                                    st0 = r + d * 128 * kb
                                    for k in range(8):
                                        p.op("pe", lambda e, bk=bk, bi=bi, k=k, st0=st0, d=d, wb=wb, gi=gi, jj=jj: e.matmul(
                                            PS(bk, 64, bi * 64), lhsT=hT[:, k, st0:st0 + 127 * d + 1:d],
                                            rhs=wb[:, k, (6 + gi) * 128 + jj * 64:(6 + gi) * 128 + jj * 64 + 64], start=(k == 0), stop=(k == 7)),
                                             reads=[wk] + hT_all, writes=[psk(bk)])
                                p.op("dve", lambda e, bk=bk, VPb=VPb, blk4=blk4, pb0=pb0: e.tensor_copy(
                                    out=VPb[:, blk4 * 4:(blk4 + 1) * 4, pb0:pb0 + 64], in_=PS(bk, 256).rearrange("p (b d) -> p b d", d=64)),
                                     reads=[psk(bk), vk], writes=[(vk, blk4)])
                            if l == 0 and b == 0 and jp == 0 and jj == 0 and gi == _DBG_GROUPS[0] and "VP" in dbg_t:
                                tmpv = arW.alloc("dbgv", 0, [NT, 128], F32)
                                p.op("dve", lambda e, VPb=VPb: e.tensor_copy(out=tmpv, in_=VPb), reads=[(vk, q_) for q_ in range(4)], writes=["dbgv"])
                                finals.append(p.dma("sp", lambda e: [e.dma_start(out=dbg_t["VP"], in_=tmpv)], reads=["dbgv"], writes=["dbgv_d"]))
                                p.op("dve", lambda e: e.memset(small[:, 201:202], 0.0), reads=["dbgv_d"], writes=["dbgv"])
                            pend = []

                            def do_pv3(r, qb, kbl, bs, VPb=VPb, vk=vk, gi=gi, d=d, nkb=nkb, j=j, jj=jj):
                                i = ec[0] % 3
                                ec[0] += 1
                                w_ = 128 * len(kbl)
                                off0 = 128 + 128 * (qb - kbl[0])
                                p.op("act", lambda e, i=i, bs=bs, w_=w_: e.activation(out=expf[i][:, 0:w_], in_=PS(bs, w_), func=AF.Exp, scale=0.125),
                                     reads=[psk(bs)], writes=["expf%d" % i])
                                p.op("dve", lambda e, i=i, off0=off0, w_=w_: e.tensor_tensor(out=pt2[i][:, 0:w_], in0=expf[i][:, 0:w_], in1=Et[:, 4 * gi + j, off0:off0 + w_], op=ALU.mult),
                                     reads=["expf%d" % i, "Et"], writes=["pt2_%d" % i])
                                if d == 1:
                                    pieces = [(qb // 4, (qb % 4) * 128, (qb % 4) * 128 + 128, 0, 128)]
                                elif d == 4:
                                    pieces = [(qb, r, r + 4 * 127 + 1, 0, 128)]
                                else:
                                    pieces = [(bq_, r, r + 16 * 31 + 1, 32 * bq_, 32 * bq_ + 32) for bq_ in range(4)]
                                for (bq_, c0, c1, a0, a1) in pieces:
                                    for n_, kb in enumerate(kbl):
                                        blk = r * nkb + kb
                                        p.op("pe", lambda e, bq_=bq_, c0=c0, c1=c1, a0=a0, a1=a1, i=i, blk=blk, n_=n_: e.matmul(
                                            PS(4 + bq_)[:, c0:c1:d], lhsT=VPb[:, blk, :], rhs=pt2[i][:, 128 * n_ + a0:128 * n_ + a1],
                                            start=(n_ == 0), stop=(n_ == len(kbl) - 1), skip_group_check=True),
                                             reads=["pt2_%d" % i, (vk, blk // 4)], writes=[psk(4 + bq_)])

                            for r in range(d):
                                for qb in range(nkb):
                                    kbl = [kb for kb in (qb + 1, qb, qb - 1) if 0 <= kb < nkb]
                                    bs = nb()
                                    for n_, kb in enumerate(kbl):
                                        ks0 = r + d * 128 * kb
                                        q0 = r + d * 128 * qb
                                        p.op("pe", lambda e, bs=bs, ks0=ks0, q0=q0, d=d, gi=gi, pb0=pb0, n_=n_: e.matmul(
                                            PS(bs, 128, 128 * n_), lhsT=KT[pb0:pb0 + 64, gi, ks0:ks0 + 127 * d + 1:d], rhs=QT[pb0:pb0 + 64, gi, q0:q0 + 127 * d + 1:d],
                                            start=True, stop=True, skip_group_check=True),
                                             reads=[("KT", gi, tc_) for tc_ in range(4)] + [("QT", gi, tc_) for tc_ in range(4)], writes=[psk(bs)])
                                    pend.append((r, qb, kbl, bs))
                                    if len(pend) > 2:
                                        do_pv3(*pend.pop(0))
                            while pend:
                                do_pv3(*pend.pop(0))
                            for bq_ in range(4):
                                accs = accb[:, bq_ * 512:(bq_ + 1) * 512]
                                if gi == _DBG_GROUPS[0] and gi != _DBG_GROUPS[-1]:
                                    p.op("dve", lambda e, bq_=bq_, accs=accs: e.tensor_copy(out=accs, in_=PS(4 + bq_)), reads=[psk(4 + bq_)], writes=[("accb", bq_)])
                                elif gi != _DBG_GROUPS[-1]:
                                    p.op("dve", lambda e, bq_=bq_, accs=accs: e.tensor_tensor(out=accs, in0=PS(4 + bq_), in1=accs, op=ALU.add),
                                         reads=[psk(4 + bq_), ("accb", bq_)], writes=[("accb", bq_)])
                                elif len(_DBG_GROUPS) > 1:
                                    p.op("dve", lambda e, bq_=bq_, accs=accs: e.tensor_tensor(out=PS(4 + bq_), in0=PS(4 + bq_), in1=accs, op=ALU.add),
                                         reads=[psk(4 + bq_), ("accb", bq_)], writes=[psk(4 + bq_)])
                        for bq_ in range(4):
                            ri = bq_ % 2
                            R = Rr[ri]
                            p.op("dve", lambda e, R=R, bq_=bq_, zlo=zlo: e.reciprocal(out=R[zlo:zlo + 64, :], in_=PS(4 + bq_)[zlo:zlo + 64, :]),
                                 reads=[psk(4 + bq_)], writes=["Rr%d" % ri])
                            p.op("dve", lambda e, R=R, bq_=bq_, zlo=zlo, pb0=pb0, jp=jp: e.tensor_tensor(
                                out=out_bT[pb0:pb0 + 64, jp, bq_ * 512:(bq_ + 1) * 512], in0=PS(4 + bq_)[pb0:pb0 + 64, :], in1=R[zlo:zlo + 64, :], op=ALU.mult),
                                 reads=[psk(4 + bq_), "Rr%d" % ri], writes=[("out_bT", bq_)])
                bank_lim[0] = 8
                if l == 0 and b == 0 and "out_bT" in dbg_t:
                    tmpf = arW.alloc("dbgf", 0, [2, 2048], F32)
                    p.op("dve", lambda e: e.tensor_copy(out=tmpf, in_=out_bT), reads=[("out_bT", q_) for q_ in range(4)], writes=["dbgf"])
                    finals.append(p.dma("sp", lambda e: [e.dma_start(out=dbg_t["out_bT"], in_=tmpf)], reads=["dbgf"], writes=["dbgf_d"]))
                    p.op("dve", lambda e: e.memset(small[:, 200:201], 0.0), reads=["dbgf_d"], writes=["dbgf"])
                if stage <= 4:
                    return

                wG = arW.alloc("wG", 0, [8, 2048], BF16)
                wPA = arW.alloc("wPA", 32 * 1024, [4, D], BF16)
                wPB = arW.alloc("wPB", 40 * 1024, [2, D], BF16)
                wO = arW.alloc("wO", 44 * 1024, [8, D], BF16)
                p.dma("pool", lambda e: [e.dma_start(out=wG, in_=w_in[l][:, 3072:5120].rearrange("(k p) n -> p k n", p=128))], reads=[("w_in", "f", l)], writes=["wG"])
                p.dma("pool", lambda e: [e.dma_start(out=wPA[g_ * 64:(g_ + 1) * 64, c_, :], in_=w_pa[l][(4 * g_ + c_) * 64:(4 * g_ + c_ + 1) * 64, :])
                                         for c_ in range(4) for g_ in range(2)], reads=[("w_po", "f", l)], writes=["wPA"], n=8)
                p.dma("pool", lambda e: [e.dma_start(out=wPB, in_=w_pb[l].rearrange("(j p) n -> p j n", p=128))], reads=[("w_po", "f", l)], writes=["wPB"])
                p.dma("pool", lambda e: [e.dma_start(out=wO, in_=w_o[l].rearrange("(k p) n -> p k n", p=128))], reads=[("w_po", "f", l)], writes=["wO"])
                p.dma("sp", lambda e: [e.dma_start(out=lnp[:], in_=lnp_in[:, l, 0:2, :])], writes=["lnp"])
                make_bc(gbc[:], 16, b)
                xt3 = [arT.alloc("xt%d" % i, i * 4096, [D], F32) for i in range(2)]
                tmp = arT.alloc("tmp", 8192, [D], F32)
                sig = arT.alloc("sig", 12288, [2048], BF16)
                m1 = arT.alloc("m1", 16384, [D], F32)
                mrg = [arT.alloc("mrg%d" % i, 20480 + i * 2048, [D], BF16) for i in range(2)]
                mT = arT.alloc("mT", 24576, [8, 128], BF16)
                h2f = arT.alloc("h2f", 26624, [8, 128], F32)
                mv = small[:, 16:18]
                rstd = small[:, 18:19]

                def stage_a(t):
                    tp = t // 2
                    xb = xt3[t % 2]
                    xk = "xt%d" % (t % 2)
                    p.dma("sp", lambda e: [e.dma_start(out=xb, in_=x_src[b, t * 128:(t + 1) * 128, :])], reads=[xkey(t)], writes=[xk], slot=("xt", t % 2))
                    for n in range(4):
                        bk = nb()
                        for k in range(8):
                            p.op("pe", lambda e, bk=bk, k=k, n=n: e.matmul(PS(bk), lhsT=hT[:, k, t * 128:(t + 1) * 128], rhs=wG[:, k, n * 512:(n + 1) * 512],
                                                                       start=(k == 0), stop=(k == 7)), reads=[("hT", tp), "wG"], writes=[psk(bk)])
                        p.op("act", lambda e, bk=bk, n=n: e.activation(out=sig[:, n * 512:(n + 1) * 512], in_=PS(bk), func=AF.Sigmoid),
                             reads=[psk(bk)], writes=[("sig", n)])
                    for n in range(2):
                        bk = nb()
                        for c in range(4):
                            p.op("pe", lambda e, bk=bk, c=c, n=n: e.matmul(PS(bk), lhsT=out_aT[:, c, t * 128:(t + 1) * 128], rhs=wPA[:, c, n * 512:(n + 1) * 512],
                                                                       start=(c == 0), stop=(c == 3)), reads=[("out_aT", t // 4), "wPA"], writes=[psk(bk)])
                        p.op("dve", lambda e, bk=bk, n=n: e.tensor_tensor(out=m1[:, n * 512:(n + 1) * 512], in0=PS(bk), in1=sig[:, n * 512:(n + 1) * 512], op=ALU.mult),
                             reads=[psk(bk), ("sig", n)], writes=[("m1", n)])
                    for n in range(2):
                        bk = nb()
                        for c in range(2):
                            p.op("pe", lambda e, bk=bk, c=c, n=n: e.matmul(PS(bk), lhsT=out_bT[:, c, t * 128:(t + 1) * 128], rhs=wPB[:, c, n * 512:(n + 1) * 512],
                                                                       start=(c == 0), stop=(c == 1)), reads=[("out_bT", t // 4), "wPB"], writes=[psk(bk)])
                        p.op("dve", lambda e, bk=bk, n=n: e.tensor_tensor(out=tmp[:, n * 512:(n + 1) * 512], in0=PS(bk), in1=sig[:, 1024 + n * 512:1024 + (n + 1) * 512], op=ALU.mult),
                             reads=[psk(bk), ("sig", 2 + n)], writes=[("tmp", n)])
                    p.op("pool", lambda e: e.tensor_tensor(out=mrg[t % 2], in0=m1, in1=tmp, op=ALU.add),
                         reads=[("m1", 0), ("m1", 1), ("tmp", 0), ("tmp", 1)], writes=["mrg%d" % (t % 2)])

                def stage_b(t):
                    tp = t // 2
                    xb = xt3[t % 2]
                    xk = "xt%d" % (t % 2)
                    mk = "mrg%d" % (t % 2)
                    bt = nb()
                    pTm = PSB(bt).rearrange("p (c n) -> p c n", n=128)
                    for c in range(8):
                        p.op("pe", lambda e, c=c: e.transpose(out=pTm[:, c, :], in_=mrg[t % 2][:, c * 128:(c + 1) * 128], identity=identb[:]),
                             reads=[mk, "identb"], writes=[psk(bt)])
                    p.op("act", lambda e: e.copy(out=mT, in_=pTm), reads=[psk(bt)], writes=["mT"])
                    bo = nb(2)
                    for n in range(2):
                        for k in range(8):
                            p.op("pe", lambda e, k=k, n=n: e.matmul(PS(bo + n), lhsT=mT[:, k, :], rhs=wO[:, k, n * 512:(n + 1) * 512], start=(k == 0), stop=(k == 7)),
                                 reads=["mT", "wO"], writes=[psk(bo + n)])
                    p.op("dve", lambda e: e.tensor_tensor(out=tmp, in0=ps_all[:, bo * 512:(bo + 2) * 512], in1=gbc[:], op=ALU.mult),
                         reads=[psk(bo), psk(bo + 1), "gbc"], writes=[("tmp", 0), ("tmp", 1)])
                    p.op("dve", lambda e: e.scalar_tensor_tensor(out=xb, in0=xb, scalar=float(ALPHA), in1=tmp, op0=ALU.mult, op1=ALU.add),
                         reads=[xk, ("tmp", 0), ("tmp", 1)], writes=[xk])
                    ln_stats(xb, mv, rstd, xk, "p3")
                    norm_act(xb, xb, rstd, "p3", [xk], [xk])
                    p.op("pool", lambda e: e.tensor_tensor(out=xb, in0=xb, in1=lnp[:, 0, :], op=ALU.mult), reads=[xk, "lnp"], writes=[xk])
                    p.op("pool", lambda e: e.tensor_tensor(out=xb, in0=xb, in1=lnp[:, 1, :], op=ALU.add), reads=[xk, "lnp"], writes=[xk])
                    p.op("act", lambda e: e.mul(out=m1, in_=xb, mul=float(ALPHA)), reads=[xk], writes=[("m1", 0), ("m1", 1)])
                    p.dma("sp", lambda e: [e.dma_start(out=xs[b, t * 128:(t + 1) * 128, :], in_=m1)], reads=[("m1", 0), ("m1", 1)], writes=[xkey(t)], slot=("xst", 0))
                    if _P3 <= 2:
                        return
                    ln_stats(xb, mv, rstd, xk, "p3b")
                    norm_act(tmp, xb, rstd, "p3b", [xk], [("tmp", 0), ("tmp", 1)])
                    if _P3 <= 3:
                        return
                    bh = nb(2)
                    pTh = ps_all[:, bh * 512:(bh + 2) * 512].rearrange("p (c n) -> p c n", n=128)
                    for c in range(8):
                        p.op("pe", lambda e, c=c: e.transpose(out=pTh[:, c, :], in_=tmp[:, c * 128:(c + 1) * 128], identity=identf[:]),
                             reads=[("tmp", 0), ("tmp", 1), "identf"], writes=[psk(bh + c // 4)])
                    if _SUB <= 1:
                        return
                    for c in range(8):
                        if _SUB == 2 and c % 2 == 1:
                            continue
                        if _SUB == 3 and c % 2 == 0:
                            continue
                        if c < 8:
                            p.op("act", lambda e, c=c: e.activation(out=h2f[:, c, :], in_=pTh[:, c, :], func=AF.Identity,
                                                                    bias=modT[:, 24 + c, b:b + 1], scale=modT[:, 32 + c, b:b + 1]),
                                 reads=[psk(bh + c // 4), "modT"], writes=[("h2f", c)])
                        else:
                            p.op("dve", lambda e, c=c: e.tensor_scalar(out=h2f[:, c, :], in0=pTh[:, c, :], scalar1=modT[:, 32 + c, b:b + 1],
                                                                       scalar2=modT[:, 24 + c, b:b + 1], op0=ALU.mult, op1=ALU.add),
                                 reads=[psk(bh + c // 4), "modT"], writes=[("h2f", c)])
                    if _SUB <= 4:
                        return
                    h2keys = [("h2f", c) for c in range(8)]
                    p.op("pool", lambda e: e.tensor_copy(out=hT[:, :, t * 128:(t + 1) * 128], in_=h2f), reads=h2keys, writes=[("hT", tp)])
                    if _P3 <= 4:
                        return
                    br = nb()
                    for c in range(8):
                        p.op("pe", lambda e, c=c: e.matmul(PS(br, 16), lhsT=h2f[:, c, :], rhs=wR[:, c, :], start=(c == 0), stop=(c == 7)),
                             reads=[("h2f", c), "wR"], writes=[psk(br)])
                    if _P3 <= 5:
                        return
                    sc_ = small[:, 64:80]
                    sel_ = small[:, 80:96]
                    eq_ = small[:, 96:112]
                    s2_ = small[:, 112:128]
                    m1_ = small[:, 128:132]
                    m2_ = small[:, 132:136]
                    gs_ = small[:, 136:140]
                    gm_ = small[:, 140:141]
                    ing_ = small[:, 144:148]
                    t1_ = small[:, 148:149]
                    t2_ = small[:, 149:150]
                    den_ = small[:, 150:151]
                    e2_ = small[:, 160:176]
                    cm_ = small[:, 176:192]
                    v4 = lambda a: a.rearrange("p (g e) -> p g e", e=4)
                    R_ = ["rt"]
                    p.op("act", lambda e: e.activation(out=sc_, in_=PS(br, 16), func=AF.Sigmoid), reads=[psk(br)], writes=R_)
                    p.op("dve", lambda e: e.tensor_tensor(out=sel_, in0=sc_, in1=rb[:], op=ALU.add), reads=R_ + ["rb"], writes=R_)
                    p.op("dve", lambda e: e.tensor_reduce(out=m1_, in_=v4(sel_), axis=AX.X, op=ALU.max), reads=R_, writes=R_)
                    p.op("dve", lambda e: e.tensor_tensor(out=v4(eq_), in0=v4(sel_), in1=m1_.unsqueeze(2).to_broadcast([128, 4, 4]), op=ALU.is_equal), reads=R_, writes=R_)
                    p.op("dve", lambda e: e.scalar_tensor_tensor(out=s2_, in0=eq_, scalar=-1e9, in1=sel_, op0=ALU.mult, op1=ALU.add), reads=R_, writes=R_)
                    p.op("dve", lambda e: e.tensor_reduce(out=m2_, in_=v4(s2_), axis=AX.X, op=ALU.max), reads=R_, writes=R_)
                    p.op("dve", lambda e: e.tensor_tensor(out=gs_, in0=m1_, in1=m2_, op=ALU.add), reads=R_, writes=R_)
                    p.op("dve", lambda e: e.tensor_reduce(out=gm_, in_=gs_, axis=AX.X, op=ALU.max), reads=R_, writes=R_)
                    p.op("dve", lambda e: e.tensor_scalar(out=ing_, in0=gs_, scalar1=gm_, scalar2=None, op0=ALU.is_equal), reads=R_, writes=R_)
                    p.op("dve", lambda e: e.tensor_scalar(out=ing_, in0=ing_, scalar1=1.0, scalar2=1e9, op0=ALU.subtract, op1=ALU.mult), reads=R_, writes=R_)
                    p.op("dve", lambda e: e.tensor_tensor(out=v4(s2_), in0=v4(sel_), in1=ing_.unsqueeze(2).to_broadcast([128, 4, 4]), op=ALU.add), reads=R_, writes=R_)
                    p.op("dve", lambda e: e.tensor_reduce(out=t1_, in_=s2_, axis=AX.X, op=ALU.max), reads=R_, writes=R_)
                    p.op("dve", lambda e: e.tensor_scalar(out=eq_, in0=s2_, scalar1=t1_, scalar2=None, op0=ALU.is_equal), reads=R_, writes=R_)
                    p.op("dve", lambda e: e.scalar_tensor_tensor(out=s2_, in0=eq_, scalar=-1e9, in1=s2_, op0=ALU.mult, op1=ALU.add), reads=R_, writes=R_)
                    p.op("dve", lambda e: e.tensor_reduce(out=t2_, in_=s2_, axis=AX.X, op=ALU.max), reads=R_, writes=R_)
                    p.op("dve", lambda e: e.tensor_scalar(out=e2_, in0=s2_, scalar1=t2_, scalar2=None, op0=ALU.is_equal), reads=R_, writes=R_)
                    p.op("dve", lambda e: e.tensor_tensor(out=eq_, in0=eq_, in1=e2_, op=ALU.add), reads=R_, writes=R_)
                    p.op("dve", lambda e: e.tensor_tensor(out=cm_, in0=eq_, in1=sc_, op=ALU.mult), reads=R_, writes=R_)
                    p.op("dve", lambda e: e.tensor_reduce(out=den_, in_=cm_, axis=AX.X, op=ALU.add), reads=R_, writes=R_)
                    p.op("dve", lambda e: e.reciprocal(out=den_, in_=den_), reads=R_, writes=R_)
                    p.op("dve", lambda e: e.tensor_scalar(out=cm_, in0=cm_, scalar1=den_, scalar2=None, op0=ALU.mult), reads=R_, writes=R_)
                    if _P3 <= 6:
                        return
                    bc_ = nb()
                    p.op("pe", lambda e: e.transpose(out=PS(bc_, 128)[0:16, :], in_=cm_, identity=identf[:]), reads=R_ + ["identf"], writes=[psk(bc_)])
                    p.op("act", lambda e: e.copy(out=combT[0:16, t * 128:(t + 1) * 128], in_=PS(bc_, 128)[0:16, :]), reads=[psk(bc_)], writes=[("combT", t // 2)])

                stage_a(0)
                for t in range(NT if _P3 >= 99 else 1):
                    if t + 1 < NT and _P3 >= 99:
                        stage_a(t + 1)
                    if _P3 >= 2:
                        stage_b(t)
                if stage <= 5:
                    if l == 0 and b == 0 and "combT" in dbg_t:
                        tmpf = arW.alloc("dbgf", 0, [2048], F32)
                        p.op("dve", lambda e: e.tensor_copy(out=tmpf[0:16, :], in_=combT[0:16, :]), reads=[("combT", q_) for q_ in range(8)], writes=["dbgf"])
                        finals.append(p.dma("sp", lambda e: [e.dma_start(out=dbg_t["combT"], in_=tmpf[0:16, :])], reads=["dbgf"], writes=["dbgf_d"]))
                    finals.append(p.dma("sp", lambda e: [e.dma_start(out=y_out[b, 0:128, :], in_=xt3[0])], reads=["xt0"]))
                    return

                bank_lim[0] = 4
                bank_rr[0] = 0
                slots = []
                for si in range(8):
                    ar_ = arW if si < 4 else arA
                    o_ = (si % 4) * 12 * 1024
                    slots.append((ar_.alloc("wEG%d" % si, o_, [8, 256], BF16), ar_.alloc("wEU%d" % si, o_ + 4096, [8, 256], BF16),
                                  ar_.alloc("wED%d" % si, o_ + 8192, [2, D], BF16)))
                p.dma("sp", lambda e: [e.dma_start(out=lnp[:], in_=lnp_in[:, l, 2:4, :])], writes=["lnp"])
                make_bc(gbc[:], 40, b)
                sl = [arT.alloc("sl%d" % i, i * 1024, [256], F32) for i in range(2)]
                s2b = [arT.alloc("s2b%d" % i, 2048 + i * 1024, [256], F32) for i in range(2)]
                actT = [arT.alloc("actT%d" % i, 4096 + i * 512, [256], BF16) for i in range(4)]
                ytmp = [arT.alloc("ytmp%d" % i, 8192 + i * 4096, [D], F32) for i in range(2)]
                xt4 = [arT.alloc("xt%d" % i, 16384 + i * 4096, [D], F32) for i in range(2)]
                cnt = [0]
                yc = [0]

                def load_expert(e_):
                    si = (e_ // 4 % 2) * 4 + e_ % 4
                    g_, u_, d_ = slots[si]
                    p.dma("pool", lambda e: [e.dma_start(out=g_, in_=w_eg[l][e_].rearrange("(k p) f -> p k f", p=128)),
                                             e.dma_start(out=u_, in_=w_eu[l][e_].rearrange("(k p) f -> p k f", p=128)),
                                             e.dma_start(out=d_, in_=w_ed[l][e_].rearrange("(c p) n -> p c n", p=128))],
                          reads=[("w_eg", "f", l), ("w_eu", "f", l), ("w_ed", "f", l)], writes=["wEG%d" % si, "wEU%d" % si, "wED%d" % si], n=3)

                for e_ in range(8):
                    load_expert(e_)
                for eg in range(4):
                    for tc2 in range(8):
                        tok = slice(tc2 * 256, (tc2 + 1) * 256)
                        cbb = {}

                        def front(ei, fc, tok=tok, tc2=tc2, eg=eg):
                            e_ = eg * 4 + ei
                            si = (eg % 2) * 4 + ei
                            g_, u_, d_ = slots[si]
                            if fc == 0:
                                bcb = nb()
                                cbb[ei] = bcb
                                p.op("pe", lambda e: e.matmul(PS(bcb, 256), lhsT=selb[:, e_, :], rhs=combT[:, tok], start=True, stop=True),
                                     reads=["selb", ("combT", tc2)], writes=[psk(bcb)])
                            bcb = cbb[ei]
                            bgu = nb()
                            for k in range(8):
                                p.op("pe", lambda e, k=k: e.matmul(PS(bgu, 256, 0), lhsT=g_[:, k, fc * 128:(fc + 1) * 128], rhs=hT[:, k, tok],
                                                                   start=(k == 0), stop=(k == 7)), reads=["wEG%d" % si, ("hT", tc2)], writes=[psk(bgu)])
                            for k in range(8):
                                p.op("pe", lambda e, k=k: e.matmul(PS(bgu, 256, 256), lhsT=u_[:, k, fc * 128:(fc + 1) * 128], rhs=hT[:, k, tok],
                                                                   start=(k == 0), stop=(k == 7), skip_group_check=True), reads=["wEU%d" % si, ("hT", tc2)], writes=[psk(bgu)])
                            i2 = cnt[0] % 2
                            i4 = cnt[0] % 4
                            cnt[0] += 1
                            p.op("act", lambda e: e.activation(out=sl[i2], in_=PS(bgu, 256, 0), func=AF.Silu), reads=[psk(bgu)], writes=["sl%d" % i2])
                            p.op("dve", lambda e: e.tensor_tensor(out=s2b[i2], in0=PS(bgu, 256, 256), in1=sl[i2], op=ALU.mult),
                                 reads=[psk(bgu), "sl%d" % i2], writes=["s2b%d" % i2])
                            p.op("dve", lambda e: e.tensor_tensor(out=actT[i4], in0=PS(bcb, 256), in1=s2b[i2], op=ALU.mult),
                                 reads=[psk(bcb), "s2b%d" % i2], writes=["actT%d" % i4])
                            return (ei, fc, i4, d_, si)

                        def down(ei, fc, i4, d_, si):
                            for ti in range(2):
                                for n in range(2):
                                    yb = 4 + ti * 2 + n
                                    p.op("pe", lambda e, yb=yb, ti=ti, n=n: e.matmul(
                                        PS(yb), lhsT=actT[i4][:, ti * 128:(ti + 1) * 128], rhs=d_[:, fc, n * 512:(n + 1) * 512],
                                        start=(ei == 0 and fc == 0), stop=(ei == 3 and fc == 1)),
                                         reads=["actT%d" % i4, "wED%d" % si], writes=[psk(yb)])

                        prev = None
                        for ei in range(4):
                            for fc in range(2):
                                cur = front(ei, fc)
                                if prev is not None:
                                    down(*prev)
                                prev = cur
                        down(*prev)
                        for ti in range(2):
                            t = tc2 * 2 + ti
                            yi = yc[0] % 2
                            yc[0] += 1
                            p.op("dve", lambda e, ti=ti, yi=yi: e.tensor_tensor(out=ytmp[yi], in0=ps_all[:, (4 + 2 * ti) * 512:(6 + 2 * ti) * 512], in1=gbc[:], op=ALU.mult),
                                 reads=[psk(4 + 2 * ti), psk(5 + 2 * ti), "gbc"], writes=["ytmp%d" % yi])
                            xb = xt4[yi]
                            xk = "xt%d" % yi
                            p.dma("sp", lambda e, xb=xb, t=t: [e.dma_start(out=xb, in_=xs[b, t * 128:(t + 1) * 128, :])], reads=[xkey(t)], writes=[xk], slot=("xt", yi))
                            p.op("pool", lambda e, xb=xb, yi=yi: e.tensor_tensor(out=xb, in0=xb, in1=ytmp[yi], op=ALU.add), reads=[xk, "ytmp%d" % yi], writes=[xk])
                            p.dma("sp", lambda e, xb=xb, t=t: [e.dma_start(out=xs[b, t * 128:(t + 1) * 128, :], in_=xb)], reads=[xk], writes=[xkey(t)], slot=("xst", 1 + yi))
                    if eg + 2 < 4:
                        for ei in range(4):
                            load_expert((eg + 2) * 4 + ei)
                bank_lim[0] = 8
                for t in range(NT):
                    xb = xt4[t % 2]
                    xk = "xt%d" % (t % 2)
                    p.dma("sp", lambda e, xb=xb, t=t: [e.dma_start(out=xb, in_=xs[b, t * 128:(t + 1) * 128, :])], reads=[xkey(t)], writes=[xk], slot=("xt", t % 2))
                    ln_stats(xb, mv, rstd, xk, "p4")
                    norm_act(xb, xb, rstd, "p4", [xk], [xk])
                    p.op("pool", lambda e, xb=xb: e.tensor_tensor(out=xb, in0=xb, in1=lnp[:, 0, :], op=ALU.mult), reads=[xk, "lnp"], writes=[xk])
                    p.op("pool", lambda e, xb=xb: e.tensor_tensor(out=xb, in0=xb, in1=lnp[:, 1, :], op=ALU.add), reads=[xk, "lnp"], writes=[xk])
                    o_ = p.dma("sp", lambda e, xb=xb, t=t: [e.dma_start(out=x_dst[b, t * 128:(t + 1) * 128, :], in_=xb)], reads=[xk], writes=[xkey(t)], slot=("xout", t % 2))
                    if last:
                        finals.append(o_)

        for l_ in range(n_layers):
            phase0(l_)
            for b_ in range(n_seq):
                block(l_, b_)

        p.emit(final_wait_ops=finals)
    return nc


def prep_shared(inputs):
    f = lambda a: np.ascontiguousarray(np.asarray(a, dtype=np.float32))
    cos, sin, E, sel = _const_tables()
    b_ada = f(inputs["b_ada"])
    sh = {
        "w_ada": f(inputs["w_ada"]),
        "w_po": np.ascontiguousarray(np.concatenate([f(inputs["w_branch_a"]), f(inputs["w_branch_b"]), f(inputs["w_out"])], axis=1)),
        "b_adaT": np.ascontiguousarray(b_ada.reshape(DEPTH, 48, 128).transpose(2, 0, 1)),
        "w_in": f(inputs["w_in"]),
        "w_pa": f(inputs["w_branch_a"]),
        "w_pb": f(inputs["w_branch_b"]),
        "w_o": f(inputs["w_out"]),
        "w_r": f(inputs["w_router"]),
        "w_eg": f(inputs["w_exp_gate"]),
        "w_eu": f(inputs["w_exp_up"]),
        "w_ed": f(inputs["w_exp_down"]),
        "cos_t": cos, "sin_t": sin, "E_t": E, "sel_t": sel,
        "idn": np.eye(128, dtype=np.float32),
    }
    qg = f(inputs["q_norm_g"])
    kg = f(inputs["k_norm_g"])
    qk = np.concatenate([np.tile(qg[:, None, :], (1, 8, 1)), np.tile(kg[:, None, :], (1, 2, 1))], 1).reshape(DEPTH, 640)
    sh["qkg"] = np.ascontiguousarray(np.broadcast_to(qk[None], (128, DEPTH, 640)))
    lnp = np.stack([f(inputs["ln1_g"]), f(inputs["ln1_b"]), f(inputs["ln2_g"]), f(inputs["ln2_b"])], 1)
    sh["lnp"] = np.ascontiguousarray(np.broadcast_to(lnp[None], (128, DEPTH, 4, D)))
    sh["rb"] = np.ascontiguousarray(np.broadcast_to(f(inputs["router_bias"])[None], (128, 16)))
    return sh


_GATHERED = ("w_ada", "w_in", "w_po", "w_eg", "w_eu", "w_ed")


def prep_core(inputs, sh, core, n_cores=8):
    x = np.asarray(inputs["x"], dtype=np.float32)
    c = np.asarray(inputs["c"], dtype=np.float32)
    m = {k: v for k, v in sh.items() if k not in ("w_pa", "w_pb", "w_o")}
    for k in _GATHERED:
        w = m.pop(k)
        w2 = w.reshape(DEPTH, -1, w.shape[-1])
        if n_cores == 1 or not _GATHER:
            m[k] = w2
        else:
            r = w2.shape[1] // n_cores
            m[k + "_sh"] = np.ascontiguousarray(w2[:, core * r:(core + 1) * r, :])
    m["x"] = np.ascontiguousarray(x[2 * core:2 * core + 2])
    cc = c[2 * core:2 * core + 2]
    m["cT"] = np.ascontiguousarray(cc.reshape(2, 8, 128).transpose(2, 1, 0))
    return m


_NC_CACHE = {}


def kernel(**inputs):
    if "nc" not in _NC_CACHE:
        _NC_CACHE["nc"] = build_nc()
    nc = _NC_CACHE["nc"]
    sh = prep_shared(inputs)
    in_maps = [prep_core(inputs, sh, i) for i in range(8)]
    res = run_bass_kernel_spmd(nc, in_maps, core_ids=list(range(8)))
    return np.concatenate([np.asarray(r["y"]) for r in res.results], axis=0).astype(np.float32)
```

```python
import numpy as np
from contextlib import ExitStack
import concourse.bass as bass
import concourse.mybir as mybir
from concourse.bass_utils import run_bass_kernel_spmd

F32 = mybir.dt.float32
BF16 = mybir.dt.bfloat16
ALU = mybir.AluOpType
AF = mybir.ActivationFunctionType
AX = mybir.AxisListType

ENGS = ("pe", "act", "dve", "pool", "sp")

DEPTH = 4
S = 2048
D = 1024
NT = 16
ALPHA = (2.0 * DEPTH) ** 0.25
B_GROUPS = ((128, 1), (512, 4), (2048, 16))
import os as _os
_DBG_GROUPS = [int(c_) for c_ in _os.environ.get('DBG_GROUPS', '012')]
_P3 = int(_os.environ.get('DBG_P3', '99'))
_GATHER = int(_os.environ.get('KGATHER', '0'))
_SUB = int(_os.environ.get('DBG_SUB', '99'))


_op_counter = [0]


class _Op:
    __slots__ = ("eng", "fn", "deps", "needed", "semval", "dma_sem", "dma_val", "ndma", "semidx")

    def __init__(self, eng, fn, deps):
        _op_counter[0] += 1
        self.semidx = _op_counter[0]
        self.eng = eng
        self.fn = fn
        self.deps = deps
        self.needed = False
        self.semval = 0
        self.dma_sem = None
        self.dma_val = 0
        self.ndma = 0


def _base(k):
    return k[0] if isinstance(k, tuple) else k


class Prog:
    def __init__(self, nc):
        self.nc = nc
        self.ops = {e: [] for e in ENGS}
        self.last_w = {}
        self.readers = {}
        self.by_base = {}
        self.base_deps = {}
        self.dma_slots = {}
        self.n_dma_sems = 0

    def _deps(self, reads, writes, eng=None):
        deps = []
        for r in reads:
            w = self.last_w.get(r)
            if w is not None:
                deps.append(w)
            if _base(r) == "ps":
                for o in self.readers.get(r, ()):
                    if o.eng != eng:
                        deps.append(o)
            bd = self.base_deps.get(_base(r))
            if bd:
                deps.extend(bd)
        for w_ in writes:
            w = self.last_w.get(w_)
            if w is not None:
                deps.append(w)
            rs = self.readers.get(w_)
            if rs:
                deps.extend(rs)
            bd = self.base_deps.get(_base(w_))
            if bd:
                deps.extend(bd)
        return deps

    def _track(self, o, reads, writes):
        for r in reads:
            self.readers.setdefault(r, []).append(o)
            self.by_base.setdefault(_base(r), set()).add(r)
        for w in writes:
            self.last_w[w] = o
            self.readers[w] = []
            self.by_base.setdefault(_base(w), set()).add(w)

    @staticmethod
    def _compress(deps):
        best = {}
        for d in deps:
            if d.dma_sem is not None:
                k = ("d", d.dma_sem)
                prev = best.get(k)
                if prev is None or d.dma_val > prev.dma_val:
                    best[k] = d
            else:
                prev = best.get(d.eng)
                if prev is None or d.semidx > prev.semidx:
                    best[d.eng] = d
        return list(best.values())

    def op(self, eng, fn, reads=(), writes=()):
        deps = self._compress(self._deps(reads, writes, eng))
        o = _Op(eng, fn, deps)
        for d in deps:
            d.needed = True
        self.ops[eng].append(o)
        self._track(o, reads, writes)
        return o

    def dma(self, queue, fn, reads=(), writes=(), slot=None, n=1):
        deps = self._compress(self._deps(reads, writes))
        o = _Op(queue, fn, deps)
        for d in deps:
            d.needed = True
        if slot is None:
            if writes:
                slot = ("w", writes[0])
            else:
                self._auto = getattr(self, "_auto", 0) + 1
                slot = ("auto", self._auto % 16)
        st = self.dma_slots.get(slot)
        if st is None:
            st = [self.n_dma_sems, 0]
            self.n_dma_sems += 1
            self.dma_slots[slot] = st
        st[1] += 16 * n
        o.dma_sem = st[0]
        o.dma_val = st[1]
        o.ndma = n
        self.ops[queue].append(o)
        self._track(o, reads, writes)
        return o

    def recycle(self, new_base, old_bases):
        acc = {}
        dmas = []

        def add(o):
            if o.dma_sem is not None:
                dmas.append(o)
            else:
                prev = acc.get(o.eng)
                if prev is None or o.semidx > prev.semidx:
                    acc[o.eng] = o

        for ob in old_bases:
            for o in self.base_deps.get(ob, []):
                add(o)
            for k in self.by_base.get(ob, ()):
                for o in self.readers.get(k, []):
                    add(o)
                w = self.last_w.get(k)
                if w is not None:
                    add(w)
        for k in self.by_base.get(new_base, ()):
            self.last_w.pop(k, None)
            self.readers.pop(k, None)
        self.base_deps[new_base] = self._compress(list(acc.values()) + dmas)
        self.by_base[new_base] = set()

    def emit(self, final_wait_ops=()):
        nc = self.nc
        with ExitStack() as es:
            EPOCH = 12000
            dsem = [es.enter_context(nc.semaphore("d_%d" % i)) for i in range(self.n_dma_sems)]
            esem = {}
            for e in ENGS:
                c = 0
                for o in self.ops[e]:
                    if o.dma_sem is None and o.needed:
                        o.semval = (c // EPOCH, c % EPOCH + 1)
                        c += 1
                esem[e] = [es.enter_context(nc.semaphore("s_%s_%d" % (e, i))) for i in range(c // EPOCH + 1)]
            self.sem_counts = {e: len(v) for e, v in esem.items()}
            block = es.enter_context(nc.Block())

            def run(eng_name):
                def body(eng):
                    waited = {}
                    for o in self.ops[eng_name]:
                        for d in o.deps:
                            if d.dma_sem is not None:
                                key = ("d", d.dma_sem)
                                val = d.dma_val
                                sem = dsem[d.dma_sem]
                            else:
                                if d.eng == "pe" and eng_name == "pe":
                                    continue
                                key = d.eng
                                val = d.semval
                                if waited.get(key, (-1, 0)) >= val:
                                    continue
                                waited[key] = val
                                eng.wait_ge(esem[d.eng][val[0]], val[1])
                                continue
                            if waited.get(key, 0) >= val:
                                continue
                            waited[key] = val
                            eng.wait_ge(sem, val)
                        if o.dma_sem is not None:
                            insts = o.fn(eng)
                            assert len(insts) == o.ndma, (len(insts), o.ndma)
                            for ins in insts:
                                ins.then_inc(dsem[o.dma_sem], 16)
                        else:
                            ins = o.fn(eng)
                            if o.needed:
                                ins.then_inc(esem[eng_name][o.semval[0]], 1)
                    if eng_name == "sp":
                        for d in final_wait_ops:
                            eng.wait_ge(dsem[d.dma_sem], d.dma_val)
                return body

            block.tensor(run("pe"))
            block.scalar(run("act"))
            block.vector(run("dve"))
            block.gpsimd(run("pool"))
            block.sync(run("sp"))


class Arena:
    def __init__(self, p, ap2d, name):
        self.p = p
        self.ap = ap2d
        self.n = ap2d.shape[1]
        self.name = name
        self.regions = []

    def alloc(self, base, lo_bytes, free_shape, dt):
        nel = int(np.prod(free_shape))
        nb = nel * (4 if dt == F32 else 2)
        lo = lo_bytes // 2
        hi = lo + nb // 2
        assert hi <= self.n, (self.name, base, hi, self.n)
        old = []
        keep = []
        for (l, h, b) in self.regions:
            if l < hi and lo < h:
                if b not in old:
                    old.append(b)
                if l < lo:
                    keep.append((l, lo, b))
                if h > hi:
                    keep.append((hi, h, b))
            else:
                keep.append((l, h, b))
        keep.append((lo, hi, base))
        self.regions = keep
        self.p.recycle(base, old)
        v = self.ap[:, lo:hi]
        if dt == F32:
            v = v.bitcast(F32)
        if len(free_shape) == 2:
            v = v.rearrange("p (a b) -> p a b", b=free_shape[1])
        elif len(free_shape) == 3:
            v = v.rearrange("p (a b c) -> p a b c", b=free_shape[1], c=free_shape[2])
        return v


def _const_tables():
    pos = np.arange(S)
    row = (pos // 64).astype(np.float32)
    col = (pos % 64).astype(np.float32)
    inv = (10000.0 ** (-np.arange(0, 32, 2, dtype=np.float32) / 32.0)).astype(np.float32)
    ang = np.concatenate([row[:, None] * inv, col[:, None] * inv], -1).astype(np.float32)
    cos = np.cos(ang).astype(np.float32).reshape(NT, 128, 32).transpose(1, 0, 2)
    sin = np.sin(ang).astype(np.float32).reshape(NT, 128, 32).transpose(1, 0, 2)
    slopes = np.exp2(-8.0 * np.arange(1, 13, dtype=np.float32) / 12.0).astype(np.float32)
    i = np.arange(128)[:, None]
    c = np.arange(384)[None, :]
    delta = np.abs(c - 128 - i).astype(np.float32)
    E = np.zeros((128, 12, 384), np.float32)
    for h in range(12):
        d = B_GROUPS[h // 4][1]
        E[:, h, :] = np.where(delta <= 64, np.exp(-slopes[h] * d * delta), 0.0)
    sel = np.zeros((128, 16, 128), np.float32)
    for e in range(16):
        sel[e, e, :] = 1.0
    return np.ascontiguousarray(cos), np.ascontiguousarray(sin), E, sel


def build_nc(n_layers=DEPTH, n_seq=2, stage=99, dbg=None, n_cores=8):
    nc = bass.Bass("TRN2", target_bir_lowering=False)
    dbg = dbg or {}

    def din(name, shape, dt=F32):
        return nc.dram_tensor(name, list(shape), dt, kind="ExternalInput").ap()

    x_in = din("x", [2, S, D])
    cT_in = din("cT", [128, 8, 2])
    gather_jobs = []

    def gathered(name, rows, cols):
        if n_cores == 1 or not _GATHER:
            full = din(name, [DEPTH, rows, cols])
            return [full[l_] for l_ in range(DEPTH)]
        shd = din(name + "_sh", [DEPTH, rows // n_cores, cols])
        fulls = []
        for l_ in range(DEPTH):
            bnc = nc.dram_tensor("%s_b%d" % (name, l_), [rows // n_cores, cols], F32, kind="Internal").ap()
            ful = nc.dram_tensor("%s_f%d" % (name, l_), [rows, cols], F32, kind="Internal", addr_space="Shared").ap()
            gather_jobs.append((name, l_, shd[l_], bnc, ful))
            fulls.append(ful)
        return fulls

    w_ada = gathered("w_ada", D, 6 * D)
    b_adaT = din("b_adaT", [128, DEPTH, 48])
    w_in = gathered("w_in", D, 5120)
    qkg_in = din("qkg", [128, DEPTH, 640])
    w_po = gathered("w_po", 1792, D)
    w_pa = [w_[0:512, :] for w_ in w_po]
    w_pb = [w_[512:768, :] for w_ in w_po]
    w_o = [w_[768:1792, :] for w_ in w_po]
    lnp_in = din("lnp", [128, DEPTH, 4, D])
    w_r = din("w_r", [D, 16])
    rb_in = din("rb", [128, 16])
    w_eg = [w_.rearrange("(e d) f -> e d f", e=16) for w_ in gathered("w_eg", 16 * D, 256)]
    w_eu = [w_.rearrange("(e d) f -> e d f", e=16) for w_ in gathered("w_eu", 16 * D, 256)]
    w_ed = [w_.rearrange("(e f) d -> e f d", e=16) for w_ in gathered("w_ed", 16 * 256, D)]
    cos_in = din("cos_t", [128, NT, 32])
    sin_in = din("sin_t", [128, NT, 32])
    E_in = din("E_t", [128, 12, 384])
    sel_in = din("sel_t", [128, 16, 128])
    idn_in = din("idn", [128, 128])
    y_out = nc.dram_tensor("y", [2, S, D], F32, kind="ExternalOutput").ap()
    xs = nc.dram_tensor("xs", [2, S, D], F32, kind="Internal").ap()
    dbg_t = {k: nc.dram_tensor("dbg_" + k, list(shp), F32, kind="ExternalOutput").ap() for k, shp in dbg.items()}

    es = ExitStack()
    with es:
        def sb(name, shape, dt):
            return es.enter_context(nc.sbuf_tensor(name, list(shape), dt))

        p = Prog(nc)
        finals = []

        hT = sb("hT", [128, 8, S], BF16)
        ACT_A = sb("arenaA", [128, 28 * 1024], BF16)
        W_A = sb("arenaW", [128, 31 * 1024], BF16)
        T_A = sb("arenaT", [128, 15 * 1024 + 512], BF16)
        identf = sb("identf", [128, 128], F32)
        identb = sb("identb", [128, 128], BF16)
        onesf = sb("onesf", [128, 128], F32)
        selb = sb("selb", [128, 16, 128], BF16)
        combT = sb("combT", [128, S], BF16)
        modT = sb("modT", [128, 48, 2], F32)
        badaT = sb("badaT", [128, DEPTH, 48], F32)
        condT = sb("condT", [128, 8, 2], F32)
        lnp = sb("lnp_sb", [128, 2, D], F32)
        gbc = sb("gbc", [128, D], F32)
        wR = sb("wR", [128, 8, 16], F32)
        rb = sb("rb_sb", [128, 16], F32)
        small = sb("small", [128, 256], F32)
        diag = sb("diag", [128, 2, 128], F32)
        ps_all = es.enter_context(nc.psum_tensor("ps_all", [128, 8 * 512], F32))

        arA = Arena(p, ACT_A[:], "A")
        arW = Arena(p, W_A[:], "W")
        arT = Arena(p, T_A[:], "T")

        def PS(b, n=512, off=0):
            return ps_all[:, b * 512 + off: b * 512 + off + n]

        def PSB(b, nbanks=1):
            return ps_all[:, b * 512:(b + nbanks) * 512].bitcast(BF16)

        psk = lambda b: ("ps", b)
        bank_rr = [0]
        bank_lim = [8]

        def nb(n=1):
            b = bank_rr[0]
            if b % n:
                b += n - (b % n)
            if b + n > bank_lim[0]:
                b = 0
            bank_rr[0] = (b + n) % bank_lim[0]
            return b

        order = {"w_ada": 0, "w_in": 1, "w_po": 2, "w_eg": 3, "w_eu": 4, "w_ed": 5}
        for (name_, l_, shd_, bnc_, ful_) in sorted(gather_jobs, key=lambda j_: (j_[1], order[j_[0]])):
            if l_ >= n_layers:
                continue
            p.dma("sp", lambda e, shd_=shd_, bnc_=bnc_: [e.dma_start(out=bnc_, in_=shd_)], writes=[(name_, "b", l_)], slot=("gb", l_ % 2, name_))
            p.dma("pool", lambda e, bnc_=bnc_, ful_=ful_: [e.collective_compute("AllGather", ALU.bypass, replica_groups=[list(range(n_cores))],
                                                                                 ins=[bnc_], outs=[ful_])],
                  reads=[(name_, "b", l_)], writes=[(name_, "f", l_)], slot=("gf", l_ % 2, name_))

        p.dma("sp", lambda e: [e.dma_start(out=identf[:], in_=idn_in[:])], writes=["identf"])
        p.op("dve", lambda e: e.tensor_copy(out=identb[:], in_=identf[:]), reads=["identf"], writes=["identb"])
        p.op("dve", lambda e: e.memset(onesf[:], 1.0), writes=["onesf"])
        p.op("dve", lambda e: e.memset(combT[:], 0.0), writes=["combT"])
        p.dma("pool", lambda e: [e.dma_start(out=selb[:], in_=sel_in[:])], writes=["selb"])
        p.dma("sp", lambda e: [e.dma_start(out=badaT[:], in_=b_adaT[:])], writes=["badaT"])
        p.dma("sp", lambda e: [e.dma_start(out=condT[:], in_=cT_in[:])], writes=["condT"])
        p.dma("sp", lambda e: [e.dma_start(out=wR[:], in_=w_r.rearrange("(k p) n -> p k n", p=128))], writes=["wR"])
        p.dma("sp", lambda e: [e.dma_start(out=rb[:], in_=rb_in[:])], writes=["rb"])
        p.op("act", lambda e: e.activation(out=condT[:], in_=condT[:], func=AF.Silu), reads=["condT"], writes=["condT"])

        def dbg_out(name, src_ap, reads):
            if name in dbg_t:
                finals.append(p.dma("sp", lambda e: [e.dma_start(out=dbg_t[name], in_=src_ap)], reads=reads))

        def ln_stats(src, mv, rstd, key_src, tag):
            st = small[:, 0:12].rearrange("p (a b) -> p a b", b=6)
            for i in range(2):
                p.op("dve", lambda e, i=i: e.bn_stats(out=st[:, i, :], in_=src[:, i * 512:(i + 1) * 512]),
                     reads=[key_src], writes=[("st", i)])
            p.op("dve", lambda e: e.bn_aggr(out=mv, in_=small[:, 0:12]), reads=[("st", 0), ("st", 1)], writes=[("mv", tag)])
            p.op("act", lambda e: e.activation(out=rstd, in_=mv[:, 1:2], func=AF.Sqrt, bias=1e-5, scale=1.0),
                 reads=[("mv", tag)], writes=[("rstd", tag)])
            p.op("dve", lambda e: e.reciprocal(out=rstd, in_=rstd), reads=[("rstd", tag)], writes=[("rstd", tag)])

        def make_bc(dst, chunk0, b):
            for half in range(2):
                bk = nb()
                for cc in range(4):
                    c = half * 4 + cc
                    dg = diag[:, c % 2, :]
                    p.op("dve", lambda e, dg=dg, c=c: e.tensor_scalar(out=dg, in0=identf[:], scalar1=modT[:, chunk0 + c, b:b + 1],
                                                                      scalar2=None, op0=ALU.mult),
                         reads=["identf", "modT"], writes=[("diag", c % 2)])
                    p.op("pe", lambda e, dg=dg, cc=cc, bk=bk: e.matmul(PS(bk, 128, cc * 128), lhsT=onesf[:], rhs=dg, start=True, stop=True),
                         reads=["onesf", ("diag", c % 2)], writes=[psk(bk)])
                p.op("act", lambda e, half=half, bk=bk: e.copy(out=dst[:, half * 512:(half + 1) * 512], in_=PS(bk)),
                     reads=[psk(bk)], writes=["gbc"])

        def phase0(l):
            wst = [arW.alloc("wada%d" % i, 44 * 1024 + i * 8192, [8, 256], F32) for i in range(2)]
            bkm = nb()
            for s_ in range(24):
                buf = wst[s_ % 2]
                key = ("wada", s_ % 2)
                p.dma("sp", lambda e, buf=buf, s_=s_: [e.dma_start(out=buf, in_=w_ada[l][:, s_ * 256:(s_ + 1) * 256].rearrange("(k p) n -> p k n", p=128))],
                      reads=[("w_ada", "f", l)], writes=["wada%d" % (s_ % 2)], slot=key)
                for jj in range(2):
                    j = 2 * s_ + jj
                    for k in range(8):
                        p.op("pe", lambda e, buf=buf, jj=jj, j=j, k=k: e.matmul(PS(bkm, 2, 2 * j), lhsT=buf[:, k, jj * 128:(jj + 1) * 128],
                                                                                 rhs=condT[:, k, :], start=(k == 0), stop=(k == 7)),
                             reads=["wada%d" % (s_ % 2), "condT"], writes=[psk(bkm)])
            p.op("dve", lambda e: e.tensor_tensor(out=modT[:], in0=PS(bkm, 96).rearrange("p (j b) -> p j b", b=2),
                                                  in1=badaT[:, l, :].unsqueeze(2).to_broadcast([128, 48, 2]), op=ALU.add),
                 reads=[psk(bkm), "badaT"], writes=["modT"])
            for c0 in (8, 32):
                p.op("dve", lambda e, c0=c0: e.tensor_scalar(out=modT[:, c0:c0 + 8, :], in0=modT[:, c0:c0 + 8, :], scalar1=1.0, scalar2=None, op0=ALU.add),
                     reads=["modT"], writes=["modT"])
            if l == 0:
                dbg_out("modT", modT[:], ["modT"])

        def block(l, b):
                x_src = x_in if l == 0 else xs
                last = (l == n_layers - 1)
                x_dst = y_out if last else xs
                xkey = lambda t: ("xs", b, t)

                xt = [arT.alloc("xt%d" % i, i * 4096, [D], F32) for i in range(3)]
                xh = [arT.alloc("xh%d" % i, 12288 + i * 2048, [D], BF16) for i in range(2)]
                mv = small[:, 16:18]
                rstd = small[:, 18:19]
                for tp in range(NT // 2):
                    bk = nb(2)
                    pTv = PSB(bk, 2).rearrange("p (c n) -> p c n", n=256)
                    for ti in range(2):
                        t = tp * 2 + ti
                        xb = xt[t % 3]
                        xk = "xt%d" % (t % 3)
                        p.dma("sp", lambda e, xb=xb, t=t: [e.dma_start(out=xb, in_=x_src[b, t * 128:(t + 1) * 128, :])],
                              reads=[xkey(t)], writes=[xk], slot=("xt", t % 3))
                        ln_stats(xb, mv, rstd, xk, "p1")
                        hb = xh[t % 2]
                        hk = "xh%d" % (t % 2)
                        p.op("dve", lambda e, xb=xb, hb=hb: e.tensor_scalar(out=hb, in0=xb, scalar1=mv[:, 0:1], scalar2=rstd, op0=ALU.subtract, op1=ALU.mult),
                             reads=[xk, ("mv", "p1"), ("rstd", "p1")], writes=[hk])
                        for c in range(8):
                            p.op("pe", lambda e, hb=hb, c=c, ti=ti, pTv=pTv: e.transpose(out=pTv[:, c, ti * 128:(ti + 1) * 128], in_=hb[:, c * 128:(c + 1) * 128], identity=identb[:]),
                                 reads=[hk, "identb"], writes=[psk(bk + c // 4)])
                    for c in range(8):
                        dst = hT[:, c, tp * 256:(tp + 1) * 256]
                        if c < 4:
                            p.op("act", lambda e, c=c, dst=dst, pTv=pTv: e.activation(out=dst, in_=pTv[:, c, :], func=AF.Identity,
                                                                                    bias=modT[:, c, b:b + 1], scale=modT[:, 8 + c, b:b + 1]),
                                 reads=[psk(bk + c // 4), "modT"], writes=[("hT", tp)])
                        else:
                            p.op("dve", lambda e, c=c, dst=dst, pTv=pTv: e.tensor_scalar(out=dst, in0=pTv[:, c, :], scalar1=modT[:, 8 + c, b:b + 1],
                                                                                       scalar2=modT[:, c, b:b + 1], op0=ALU.mult, op1=ALU.add),
                                 reads=[psk(bk + c // 4), "modT"], writes=[("hT", tp)])
                if l == 0 and b == 0:
                    dbg_out("hT", None, None) if False else None
                hT_all = [("hT", tp) for tp in range(8)]
                if stage <= 1:
                    return

                out_aT = arA.alloc("out_aT", 0, [4, S], BF16)
                out_bT = arA.alloc("out_bT", 16384, [2, S], BF16)
                qaT = arA.alloc("qaT", 24576, [4, S], BF16)
                kaT = arA.alloc("kaT", 40960, [2, S], BF16)
                vaP = arA.alloc("vaP", 49152, [NT, 2, 128], BF16)
                wA = arW.alloc("wA", 0, [8, 768], BF16)
                Et = arW.alloc("Et", 48 * 1024, [12, 384], BF16)
                cos_t = arW.alloc("cos", 16 * 1024, [NT, 32], F32)
                sin_t = arW.alloc("sin", 18 * 1024, [NT, 32], F32)
                qkg = arW.alloc("qkg", 12 * 1024, [640], F32)
                p.dma("pool", lambda e: [e.dma_start(out=Et, in_=E_in[:])], writes=["Et"])
                p.dma("sp", lambda e: [e.dma_start(out=cos_t, in_=cos_in[:])], writes=["cos"])
                p.dma("sp", lambda e: [e.dma_start(out=sin_t, in_=sin_in[:])], writes=["sin"])
                p.dma("sp", lambda e: [e.dma_start(out=qkg, in_=qkg_in[:, l, :])], writes=["qkg"])
                p.dma("pool", lambda e: [e.dma_start(out=wA, in_=w_in[l][:, 0:768].rearrange("(k p) n -> p k n", p=128))], reads=[("w_in", "f", l)], writes=["wA"])
                sq = arT.alloc("sq", 0, [640], F32)
                qn = arT.alloc("qn", 2560, [10, 64], F32)
                qr = arT.alloc("qr", 5120, [10, 64], BF16)
                ra = arT.alloc("ra", 6400, [10, 32], F32)
                rb_ = arT.alloc("rb_", 7680, [10, 32], F32)
                pt = [arT.alloc("pt%d" % i, 9216 + i * 1024, [512], BF16) for i in range(4)]
                Rr = [arT.alloc("Rr%d" % i, 13312 + i * 2048, [512], F32) for i in range(2)]
                ss = small[:, 32:42]
                rr = small[:, 48:58]
                p.op("pool", lambda e: e.memset(vaP, 1.0), writes=["vaP"])
                p.op("pool", lambda e: e.memset(kaT, 0.0), writes=["kaT"] + [("kaT", t_) for t_ in range(NT)])
                for t in range(NT):
                    bq = nb()
                    bkv = nb()
                    tp = t // 2
                    for k in range(8):
                        lh = hT[:, k, t * 128:(t + 1) * 128]
                        p.op("pe", lambda e, lh=lh, k=k, bq=bq: e.matmul(PS(bq), lhsT=lh, rhs=wA[:, k, 0:512], start=(k == 0), stop=(k == 7)),
                             reads=[("hT", tp), "wA"], writes=[psk(bq)])
                        p.op("pe", lambda e, lh=lh, k=k, bkv=bkv: e.matmul(PS(bkv, 256), lhsT=lh, rhs=wA[:, k, 512:768], start=(k == 0), stop=(k == 7)),
                             reads=[("hT", tp), "wA"], writes=[psk(bkv)])
                    p.op("act", lambda e, bq=bq: e.activation(out=sq[:, 0:512], in_=PS(bq), func=AF.Square), reads=[psk(bq)], writes=["sq"])
                    p.op("act", lambda e, bkv=bkv: e.activation(out=sq[:, 512:640], in_=PS(bkv, 128), func=AF.Square), reads=[psk(bkv)], writes=["sq"])
                    p.op("dve", lambda e: e.tensor_reduce(out=ss, in_=sq.rearrange("p (h d) -> p h d", d=64), axis=AX.X, op=ALU.add),
                         reads=["sq"], writes=["ss"])
                    p.op("act", lambda e: e.activation(out=rr, in_=ss, func=AF.Sqrt, bias=1e-6, scale=1.0 / 64.0), reads=["ss"], writes=["rr"])
                    p.op("dve", lambda e: e.reciprocal(out=rr, in_=rr), reads=["rr"], writes=["rr"])
                    p.op("dve", lambda e, bq=bq: e.tensor_tensor(out=qn[:, 0:8, :].rearrange("p (c g) d -> p g c d", g=2),
                                                               in0=PS(bq).rearrange("p (g c d) -> p g c d", g=2, c=4),
                                                               in1=rr[:, 0:8].rearrange("p (g c) -> p g c", g=2).unsqueeze(3).to_broadcast([128, 2, 4, 64]), op=ALU.mult),
                         reads=[psk(bq), "rr"], writes=["qn"])
                    p.op("dve", lambda e, bkv=bkv: e.tensor_tensor(out=qn[:, 8:10, :], in0=PS(bkv, 128).rearrange("p (h d) -> p h d", d=64),
                                                                 in1=rr[:, 8:10].unsqueeze(2).to_broadcast([128, 2, 64]), op=ALU.mult),
                         reads=[psk(bkv), "rr"], writes=["qn"])
                    p.op("dve", lambda e: e.tensor_tensor(out=qn, in0=qn, in1=qkg.rearrange("p (h d) -> p h d", d=64), op=ALU.mult),
                         reads=["qn", "qkg"], writes=["qn"])
                    qv = qn.rearrange("p h (d two) -> p h d two", two=2)
                    x0 = qv[:, :, :, 0]
                    x1 = qv[:, :, :, 1]
                    cb_ = cos_t[:, t, :].unsqueeze(1).to_broadcast([128, 10, 32])
                    sb_ = sin_t[:, t, :].unsqueeze(1).to_broadcast([128, 10, 32])
                    p.op("dve", lambda e, x0=x0, cb_=cb_: e.tensor_tensor(out=ra, in0=x0, in1=cb_, op=ALU.mult), reads=["qn", "cos"], writes=["ra"])
                    p.op("dve", lambda e, x1=x1, sb_=sb_: e.tensor_tensor(out=rb_, in0=x1, in1=sb_, op=ALU.mult), reads=["qn", "sin"], writes=["rb_"])
                    p.op("dve", lambda e: e.tensor_tensor(out=qr[:, :, 0:32], in0=ra, in1=rb_, op=ALU.subtract), reads=["ra", "rb_"], writes=["qr"])
                    p.op("dve", lambda e, x0=x0, sb_=sb_: e.tensor_tensor(out=ra, in0=x0, in1=sb_, op=ALU.mult), reads=["qn", "sin", "qr"], writes=["ra"])
                    p.op("dve", lambda e, x1=x1, cb_=cb_: e.tensor_tensor(out=rb_, in0=x1, in1=cb_, op=ALU.mult), reads=["qn", "cos", "qr"], writes=["rb_"])
                    p.op("dve", lambda e: e.tensor_tensor(out=qr[:, :, 32:64], in0=ra, in1=rb_, op=ALU.add), reads=["ra", "rb_"], writes=["qr"])
                    bt = nb()
                    pTq = PSB(bt)[:, 0:640].rearrange("p (c n) -> p c n", n=128)
                    for c in range(5):
                        src = qr.rearrange("p h d -> p (h d)")[:, c * 128:(c + 1) * 128]
                        p.op("pe", lambda e, c=c, src=src, pTq=pTq: e.transpose(out=pTq[:, c, :], in_=src, identity=identb[:]),
                             reads=["qr", "identb"], writes=[psk(bt)])
                    p.op("act", lambda e, t=t, pTq=pTq: e.copy(out=qaT[:, :, t * 128:(t + 1) * 128], in_=pTq[:, 0:4, :]), reads=[psk(bt)], writes=[("qaT", t // 4)])
                    p.op("act", lambda e, t=t, pTq=pTq: e.copy(out=kaT[0:64, 0, t * 128:(t + 1) * 128], in_=pTq[0:64, 4, :]), reads=[psk(bt), "kaT"], writes=[("kaT", t)])
                    p.op("act", lambda e, t=t, pTq=pTq: e.copy(out=kaT[64:128, 1, t * 128:(t + 1) * 128], in_=pTq[64:128, 4, :]), reads=[psk(bt), "kaT"], writes=[("kaT", t)])
                    p.op("dve", lambda e, t=t, bkv=bkv: e.tensor_copy(out=vaP[:, t, 0, 0:64], in_=PS(bkv, 64, 128)), reads=[psk(bkv), "vaP"], writes=[("vaP", t)])
                    p.op("dve", lambda e, t=t, bkv=bkv: e.tensor_copy(out=vaP[:, t, 1, 64:128], in_=PS(bkv, 64, 192)), reads=[psk(bkv), "vaP"], writes=[("vaP", t)])
                if l == 0 and b == 0:
                    dbg_out("qaT", None, None) if False else None

                if stage <= 2:
                    return
                ptc = [0]
                for g in range(2):
                    pb0 = 64 * g
                    for c in range(4):
                        for qc in range(4):
                            bo = nb()
                            pend = []

                            def do_pv(kb, bs, bo=bo, g=g):
                                i = ptc[0] % 4
                                ptc[0] += 1
                                ptb = pt[i]
                                p.op("act", lambda e, bs=bs, ptb=ptb: e.activation(out=ptb, in_=PS(bs), func=AF.Exp, scale=0.125),
                                     reads=[psk(bs)], writes=["pt%d" % i])
                                p.op("pe", lambda e, kb=kb, ptb=ptb: e.matmul(PS(bo), lhsT=vaP[:, kb, g, :], rhs=ptb, start=(kb == 0), stop=(kb == NT - 1)),
                                     reads=["pt%d" % i, ("vaP", kb)], writes=[psk(bo)])

                            for kb in range(NT):
                                bs = nb()
                                while bs == bo:
                                    bs = nb()
                                p.op("pe", lambda e, kb=kb, bs=bs, c=c, qc=qc, g=g: e.matmul(
                                    PS(bs), lhsT=kaT[:, g, kb * 128:(kb + 1) * 128], rhs=qaT[:, c, qc * 512:(qc + 1) * 512], start=True, stop=True),
                                     reads=[("kaT", kb), ("qaT", qc)], writes=[psk(bs)])
                                pend.append((kb, bs))
                                if len(pend) > 2:
                                    do_pv(*pend.pop(0))
                            while pend:
                                do_pv(*pend.pop(0))
                            ri = (g * 16 + c * 4 + qc) % 2
                            R = Rr[ri]
                            zlo = 64 - pb0
                            p.op("dve", lambda e, R=R, bo=bo, zlo=zlo: e.reciprocal(out=R[zlo:zlo + 64, :], in_=PS(bo)[zlo:zlo + 64, :]),
                                 reads=[psk(bo)], writes=["Rr%d" % ri])
                            p.op("dve", lambda e, R=R, bo=bo, zlo=zlo, pb0=pb0, c=c, qc=qc: e.tensor_tensor(
                                out=out_aT[pb0:pb0 + 64, c, qc * 512:(qc + 1) * 512], in0=PS(bo)[pb0:pb0 + 64, :], in1=R[zlo:zlo + 64, :], op=ALU.mult),
                                 reads=[psk(bo), "Rr%d" % ri], writes=[("out_aT", qc)])
                if l == 0 and b == 0 and "out_aT" in dbg_t:
                    tmpf = arW.alloc("dbgf", 12 * 1024, [4, 1024], F32)
                    for hh in range(2):
                        p.op("dve", lambda e, hh=hh: e.tensor_copy(out=tmpf, in_=out_aT[:, :, hh * 1024:(hh + 1) * 1024]), reads=[("out_aT", q_) for q_ in range(4)], writes=["dbgf"])
                        finals.append(p.dma("sp", lambda e, hh=hh: [e.dma_start(out=dbg_t["out_aT"][:, :, hh * 1024:(hh + 1) * 1024], in_=tmpf)], reads=["dbgf"], writes=["dbgf_d"]))
                        p.op("dve", lambda e: e.memset(small[:, 200:201], 0.0), reads=["dbgf_d"], writes=["dbgf"])
                if stage <= 3:
                    return
                bank_lim[0] = 4
                bank_rr[0] = 0
                QT = arA.alloc("QT", 24 * 1024, [3, S], BF16)
                KT = arA.alloc("KT", 36 * 1024, [3, S], BF16)
                VP = [arA.alloc("VP%d" % i, 48 * 1024 + i * 4096, [NT, 128], BF16) for i in range(2)]
                wB = [arW.alloc("wB%d" % i, 12 * 1024 + i * 18 * 1024, [8, 9 * 128], BF16) for i in range(2)]
                expf = [arT.alloc("expf%d" % i, i * 1536, [384], F32) for i in range(3)]
                pt2 = [arT.alloc("pt2_%d" % i, 4608 + i * 768, [384], BF16) for i in range(3)]
                accb = arT.alloc("accb", 20 * 1024, [S], F32)
                Rr = [arT.alloc("Rr%d" % i, 13312 + i * 2048, [512], F32) for i in range(2)]
                vpc = [0]
                ec = [0]
                for jp in range(2):
                    wb = wB[jp]
                    wk = "wB%d" % jp
                    cols = []
                    for base_c in (768, 1536, 2304):
                        for m in (jp, 2 + jp, 4 + jp):
                            cols.append(base_c + m * 128)
                    p.dma("pool", lambda e, wb=wb, cols=cols: [e.dma_start(out=wb[:, :, i * 128:(i + 1) * 128],
                                                                            in_=w_in[l][:, c0:c0 + 128].rearrange("(k p) n -> p k n", p=128))
                                                               for i, c0 in enumerate(cols)], reads=[("w_in", "f", l)], writes=[wk], n=9)
                    for which, dstT, nm in ((0, QT, "QT"), (1, KT, "KT")):
                        for mi in range(3):
                            for tc in range(4):
                                bk = nb()
                                for k in range(8):
                                    p.op("pe", lambda e, bk=bk, k=k, wb=wb, which=which, mi=mi, tc=tc: e.matmul(
                                        PS(bk), lhsT=wb[:, k, (which * 3 + mi) * 128:(which * 3 + mi + 1) * 128], rhs=hT[:, k, tc * 512:(tc + 1) * 512],
                                        start=(k == 0), stop=(k == 7)), reads=[wk, ("hT", 2 * tc), ("hT", 2 * tc + 1)], writes=[psk(bk)])
                                dst = dstT[:, mi, tc * 512:(tc + 1) * 512]
                                if which == 0:
                                    p.op("act", lambda e, dst=dst, bk=bk: e.copy(out=dst, in_=PS(bk)), reads=[psk(bk)], writes=[(nm, mi, tc)])
                                else:
                                    p.op("dve", lambda e, dst=dst, bk=bk: e.tensor_copy(out=dst, in_=PS(bk)), reads=[psk(bk)], writes=[(nm, mi, tc)])
                    if l == 0 and b == 0 and jp == 0 and "KT" in dbg_t:
                        tmpk = arW.alloc("dbgk", 0, [2, S], F32)
                        g0 = _DBG_GROUPS[0]
                        p.op("dve", lambda e: e.tensor_copy(out=tmpk[:, 0, :], in_=KT[:, g0, :]), reads=[("KT", g0, q_) for q_ in range(4)], writes=["dbgk"])
                        p.op("dve", lambda e: e.tensor_copy(out=tmpk[:, 1, :], in_=QT[:, g0, :]), reads=[("QT", g0, q_) for q_ in range(4)], writes=["dbgk"])
                        finals.append(p.dma("sp", lambda e: [e.dma_start(out=dbg_t["KT"], in_=tmpk)], reads=["dbgk"], writes=["dbgk_d"]))
                        p.op("dve", lambda e: e.memset(small[:, 202:203], 0.0), reads=["dbgk_d"], writes=["dbgk"])
                    for jj in range(2):
                        j = 2 * jp + jj
                        pb0 = 64 * jj
                        zlo = 64 - pb0
                        for gi in _DBG_GROUPS:
                            d = B_GROUPS[gi][1]
                            L_ = S // d
                            nkb = L_ // 128
                            nbq = 512 // d
                            VPb = VP[vpc[0] % 2]
                            vk = "VP%d" % (vpc[0] % 2)
                            vpc[0] += 1
                            p.op("pool", lambda e, VPb=VPb: e.memset(VPb, 1.0), writes=[vk] + [(vk, q_) for q_ in range(4)])
                            for blk4 in range(4):
                                bk = nb()
                                for bi in range(4):
                                    blk = blk4 * 4 + bi
                                    r, kb = divmod(blk, nkb)
                                    st0 = r + d * 128 * kb
                                    for k in range(8):
                                        p.op("pe", lambda e, bk=bk, bi=bi, k=k, st0=st0, d=d, wb=wb, gi=gi, jj=jj: e.matmul(
                                            PS(bk, 64, bi * 64), lhsT=hT[:, k, st0:st0 + 127 * d + 1:d],
                                            rhs=wb[:, k, (6 + gi) * 128 + jj * 64:(6 + gi) * 128 + jj * 64 + 64], start=(k == 0), stop=(k == 7)),
                                             reads=[wk] + hT_all, writes=[psk(bk)])
                                p.op("dve", lambda e, bk=bk, VPb=VPb, blk4=blk4, pb0=pb0: e.tensor_copy(
                                    out=VPb[:, blk4 * 4:(blk4 + 1) * 4, pb0:pb0 + 64], in_=PS(bk, 256).rearrange("p (b d) -> p b d", d=64)),
                                     reads=[psk(bk), vk], writes=[(vk, blk4)])
                            if l == 0 and b == 0 and jp == 0 and jj == 0 and gi == _DBG_GROUPS[0] and "VP" in dbg_t:
                                tmpv = arW.alloc("dbgv", 0, [NT, 128], F32)
                                p.op("dve", lambda e, VPb=VPb: e.tensor_copy(out=tmpv, in_=VPb), reads=[(vk, q_) for q_ in range(4)], writes=["dbgv"])
                                finals.append(p.dma("sp", lambda e: [e.dma_start(out=dbg_t["VP"], in_=tmpv)], reads=["dbgv"], writes=["dbgv_d"]))
                                p.op("dve", lambda e: e.memset(small[:, 201:202], 0.0), reads=["dbgv_d"], writes=["dbgv"])
                            pend = []

                            def do_pv3(r, qb, kbl, bs, VPb=VPb, vk=vk, gi=gi, d=d, nkb=nkb, j=j, jj=jj):
                                i = ec[0] % 3
                                ec[0] += 1
                                w_ = 128 * len(kbl)
                                off0 = 128 + 128 * (qb - kbl[0])
                                p.op("act", lambda e, i=i, bs=bs, w_=w_: e.activation(out=expf[i][:, 0:w_], in_=PS(bs, w_), func=AF.Exp, scale=0.125),
                                     reads=[psk(bs)], writes=["expf%d" % i])
                                p.op("dve", lambda e, i=i, off0=off0, w_=w_: e.tensor_tensor(out=pt2[i][:, 0:w_], in0=expf[i][:, 0:w_], in1=Et[:, 4 * gi + j, off0:off0 + w_], op=ALU.mult),
                                     reads=["expf%d" % i, "Et"], writes=["pt2_%d" % i])
                                if d == 1:
                                    pieces = [(qb // 4, (qb % 4) * 128, (qb % 4) * 128 + 128, 0, 128)]
                                elif d == 4:
                                    pieces = [(qb, r, r + 4 * 127 + 1, 0, 128)]
                                else:
                                    pieces = [(bq_, r, r + 16 * 31 + 1, 32 * bq_, 32 * bq_ + 32) for bq_ in range(4)]
                                for (bq_, c0, c1, a0, a1) in pieces:
                                    for n_, kb in enumerate(kbl):
                                        blk = r * nkb + kb
                                        p.op("pe", lambda e, bq_=bq_, c0=c0, c1=c1, a0=a0, a1=a1, i=i, blk=blk, n_=n_: e.matmul(
                                            PS(4 + bq_)[:, c0:c1:d], lhsT=VPb[:, blk, :], rhs=pt2[i][:, 128 * n_ + a0:128 * n_ + a1],
                                            start=(n_ == 0), stop=(n_ == len(kbl) - 1), skip_group_check=True),
                                             reads=["pt2_%d" % i, (vk, blk // 4)], writes=[psk(4 + bq_)])

                            for r in range(d):
                                for qb in range(nkb):
                                    kbl = [kb for kb in (qb + 1, qb, qb - 1) if 0 <= kb < nkb]
                                    bs = nb()
                                    for n_, kb in enumerate(kbl):
                                        ks0 = r + d * 128 * kb
                                        q0 = r + d * 128 * qb
                                        p.op("pe", lambda e, bs=bs, ks0=ks0, q0=q0, d=d, gi=gi, pb0=pb0, n_=n_: e.matmul(
                                            PS(bs, 128, 128 * n_), lhsT=KT[pb0:pb0 + 64, gi, ks0:ks0 + 127 * d + 1:d], rhs=QT[pb0:pb0 + 64, gi, q0:q0 + 127 * d + 1:d],
                                            start=True, stop=True, skip_group_check=True),
                                             reads=[("KT", gi, tc_) for tc_ in range(4)] + [("QT", gi, tc_) for tc_ in range(4)], writes=[psk(bs)])
                                    pend.append((r, qb, kbl, bs))
                                    if len(pend) > 2:
                                        do_pv3(*pend.pop(0))
                            while pend:
                                do_pv3(*pend.pop(0))
                            for bq_ in range(4):
                                accs = accb[:, bq_ * 512:(bq_ + 1) * 512]
                                if gi == _DBG_GROUPS[0] and gi != _DBG_GROUPS[-1]:
                                    p.op("dve", lambda e, bq_=bq_, accs=accs: e.tensor_copy(out=accs, in_=PS(4 + bq_)), reads=[psk(4 + bq_)], writes=[("accb", bq_)])
                                elif gi != _DBG_GROUPS[-1]:
                                    p.op("dve", lambda e, bq_=bq_, accs=accs: e.tensor_tensor(out=accs, in0=PS(4 + bq_), in1=accs, op=ALU.add),
                                         reads=[psk(4 + bq_), ("accb", bq_)], writes=[("accb", bq_)])
                                elif len(_DBG_GROUPS) > 1:
                                    p.op("dve", lambda e, bq_=bq_, accs=accs: e.tensor_tensor(out=PS(4 + bq_), in0=PS(4 + bq_), in1=accs, op=ALU.add),
                                         reads=[psk(4 + bq_), ("accb", bq_)], writes=[psk(4 + bq_)])
                        for bq_ in range(4):
                            ri = bq_ % 2
                            R = Rr[ri]
                            p.op("dve", lambda e, R=R, bq_=bq_, zlo=zlo: e.reciprocal(out=R[zlo:zlo + 64, :], in_=PS(4 + bq_)[zlo:zlo + 64, :]),
                                 reads=[psk(4 + bq_)], writes=["Rr%d" % ri])
                            p.op("dve", lambda e, R=R, bq_=bq_, zlo=zlo, pb0=pb0, jp=jp: e.tensor_tensor(
                                out=out_bT[pb0:pb0 + 64, jp, bq_ * 512:(bq_ + 1) * 512], in0=PS(4 + bq_)[pb0:pb0 + 64, :], in1=R[zlo:zlo + 64, :], op=ALU.mult),
                                 reads=[psk(4 + bq_), "Rr%d" % ri], writes=[("out_bT", bq_)])
                bank_lim[0] = 8
                if l == 0 and b == 0 and "out_bT" in dbg_t:
                    tmpf = arW.alloc("dbgf", 0, [2, 2048], F32)
                    p.op("dve", lambda e: e.tensor_copy(out=tmpf, in_=out_bT), reads=[("out_bT", q_) for q_ in range(4)], writes=["dbgf"])
                    finals.append(p.dma("sp", lambda e: [e.dma_start(out=dbg_t["out_bT"], in_=tmpf)], reads=["dbgf"], writes=["dbgf_d"]))
                    p.op("dve", lambda e: e.memset(small[:, 200:201], 0.0), reads=["dbgf_d"], writes=["dbgf"])
                if stage <= 4:
                    return

                wG = arW.alloc("wG", 0, [8, 2048], BF16)
                wPA = arW.alloc("wPA", 32 * 1024, [4, D], BF16)
                wPB = arW.alloc("wPB", 40 * 1024, [2, D], BF16)
                wO = arW.alloc("wO", 44 * 1024, [8, D], BF16)
                p.dma("pool", lambda e: [e.dma_start(out=wG, in_=w_in[l][:, 3072:5120].rearrange("(k p) n -> p k n", p=128))], reads=[("w_in", "f", l)], writes=["wG"])
                p.dma("pool", lambda e: [e.dma_start(out=wPA[g_ * 64:(g_ + 1) * 64, c_, :], in_=w_pa[l][(4 * g_ + c_) * 64:(4 * g_ + c_ + 1) * 64, :])
                                         for c_ in range(4) for g_ in range(2)], reads=[("w_po", "f", l)], writes=["wPA"], n=8)
                p.dma("pool", lambda e: [e.dma_start(out=wPB, in_=w_pb[l].rearrange("(j p) n -> p j n", p=128))], reads=[("w_po", "f", l)], writes=["wPB"])
                p.dma("pool", lambda e: [e.dma_start(out=wO, in_=w_o[l].rearrange("(k p) n -> p k n", p=128))], reads=[("w_po", "f", l)], writes=["wO"])
                p.dma("sp", lambda e: [e.dma_start(out=lnp[:], in_=lnp_in[:, l, 0:2, :])], writes=["lnp"])
                make_bc(gbc[:], 16, b)
                xt3 = [arT.alloc("xt%d" % i, i * 4096, [D], F32) for i in range(2)]
                tmp = arT.alloc("tmp", 8192, [D], F32)
                sig = arT.alloc("sig", 12288, [2048], BF16)
                m1 = arT.alloc("m1", 16384, [D], F32)
                mrg = [arT.alloc("mrg%d" % i, 20480 + i * 2048, [D], BF16) for i in range(2)]
                mT = arT.alloc("mT", 24576, [8, 128], BF16)
                h2f = arT.alloc("h2f", 26624, [8, 128], F32)
                mv = small[:, 16:18]
                rstd = small[:, 18:19]

                def stage_a(t):
                    tp = t // 2
                    xb = xt3[t % 2]
                    xk = "xt%d" % (t % 2)
                    p.dma("sp", lambda e: [e.dma_start(out=xb, in_=x_src[b, t * 128:(t + 1) * 128, :])], reads=[xkey(t)], writes=[xk], slot=("xt", t % 2))
                    for n in range(4):
                        bk = nb()
                        for k in range(8):
                            p.op("pe", lambda e, bk=bk, k=k, n=n: e.matmul(PS(bk), lhsT=hT[:, k, t * 128:(t + 1) * 128], rhs=wG[:, k, n * 512:(n + 1) * 512],
                                                                       start=(k == 0), stop=(k == 7)), reads=[("hT", tp), "wG"], writes=[psk(bk)])
                        p.op("act", lambda e, bk=bk, n=n: e.activation(out=sig[:, n * 512:(n + 1) * 512], in_=PS(bk), func=AF.Sigmoid),
                             reads=[psk(bk)], writes=[("sig", n)])
                    for n in range(2):
                        bk = nb()
                        for c in range(4):
                            p.op("pe", lambda e, bk=bk, c=c, n=n: e.matmul(PS(bk), lhsT=out_aT[:, c, t * 128:(t + 1) * 128], rhs=wPA[:, c, n * 512:(n + 1) * 512],
                                                                       start=(c == 0), stop=(c == 3)), reads=[("out_aT", t // 4), "wPA"], writes=[psk(bk)])
                        p.op("dve", lambda e, bk=bk, n=n: e.tensor_tensor(out=m1[:, n * 512:(n + 1) * 512], in0=PS(bk), in1=sig[:, n * 512:(n + 1) * 512], op=ALU.mult),
                             reads=[psk(bk), ("sig", n)], writes=[("m1", n)])
                    for n in range(2):
                        bk = nb()
                        for c in range(2):
                            p.op("pe", lambda e, bk=bk, c=c, n=n: e.matmul(PS(bk), lhsT=out_bT[:, c, t * 128:(t + 1) * 128], rhs=wPB[:, c, n * 512:(n + 1) * 512],
                                                                       start=(c == 0), stop=(c == 1)), reads=[("out_bT", t // 4), "wPB"], writes=[psk(bk)])
                        p.op("dve", lambda e, bk=bk, n=n: e.tensor_tensor(out=tmp[:, n * 512:(n + 1) * 512], in0=PS(bk), in1=sig[:, 1024 + n * 512:1024 + (n + 1) * 512], op=ALU.mult),
                             reads=[psk(bk), ("sig", 2 + n)], writes=[("tmp", n)])
                    p.op("pool", lambda e: e.tensor_tensor(out=mrg[t % 2], in0=m1, in1=tmp, op=ALU.add),
                         reads=[("m1", 0), ("m1", 1), ("tmp", 0), ("tmp", 1)], writes=["mrg%d" % (t % 2)])

                def stage_b(t):
                    tp = t // 2
                    xb = xt3[t % 2]
                    xk = "xt%d" % (t % 2)
                    mk = "mrg%d" % (t % 2)
                    bt = nb()
                    pTm = PSB(bt).rearrange("p (c n) -> p c n", n=128)
                    for c in range(8):
                        p.op("pe", lambda e, c=c: e.transpose(out=pTm[:, c, :], in_=mrg[t % 2][:, c * 128:(c + 1) * 128], identity=identb[:]),
                             reads=[mk, "identb"], writes=[psk(bt)])
                    p.op("act", lambda e: e.copy(out=mT, in_=pTm), reads=[psk(bt)], writes=["mT"])
                    bo = nb(2)
                    for n in range(2):
                        for k in range(8):
                            p.op("pe", lambda e, k=k, n=n: e.matmul(PS(bo + n), lhsT=mT[:, k, :], rhs=wO[:, k, n * 512:(n + 1) * 512], start=(k == 0), stop=(k == 7)),
                                 reads=["mT", "wO"], writes=[psk(bo + n)])
                    p.op("dve", lambda e: e.tensor_tensor(out=tmp, in0=ps_all[:, bo * 512:(bo + 2) * 512], in1=gbc[:], op=ALU.mult),
                         reads=[psk(bo), psk(bo + 1), "gbc"], writes=[("tmp", 0), ("tmp", 1)])
                    p.op("dve", lambda e: e.scalar_tensor_tensor(out=xb, in0=xb, scalar=float(ALPHA), in1=tmp, op0=ALU.mult, op1=ALU.add),
                         reads=[xk, ("tmp", 0), ("tmp", 1)], writes=[xk])
                    ln_stats(xb, mv, rstd, xk, "p3")
                    p.op("dve", lambda e: e.tensor_scalar(out=xb, in0=xb, scalar1=mv[:, 0:1], scalar2=rstd, op0=ALU.subtract, op1=ALU.mult),
                         reads=[xk, ("mv", "p3"), ("rstd", "p3")], writes=[xk])
                    p.op("pool", lambda e: e.tensor_tensor(out=xb, in0=xb, in1=lnp[:, 0, :], op=ALU.mult), reads=[xk, "lnp"], writes=[xk])
                    p.op("pool", lambda e: e.tensor_tensor(out=xb, in0=xb, in1=lnp[:, 1, :], op=ALU.add), reads=[xk, "lnp"], writes=[xk])
                    p.op("act", lambda e: e.mul(out=m1, in_=xb, mul=float(ALPHA)), reads=[xk], writes=[("m1", 0), ("m1", 1)])
                    p.dma("sp", lambda e: [e.dma_start(out=xs[b, t * 128:(t + 1) * 128, :], in_=m1)], reads=[("m1", 0), ("m1", 1)], writes=[xkey(t)], slot=("xst", 0))
                    if _P3 <= 2:
                        return
                    ln_stats(xb, mv, rstd, xk, "p3b")
                    p.op("dve", lambda e: e.tensor_scalar(out=tmp, in0=xb, scalar1=mv[:, 0:1], scalar2=rstd, op0=ALU.subtract, op1=ALU.mult),
                         reads=[xk, ("mv", "p3b"), ("rstd", "p3b")], writes=[("tmp", 0), ("tmp", 1)])
                    if _P3 <= 3:
                        return
                    bh = nb(2)
                    pTh = ps_all[:, bh * 512:(bh + 2) * 512].rearrange("p (c n) -> p c n", n=128)
                    for c in range(8):
                        p.op("pe", lambda e, c=c: e.transpose(out=pTh[:, c, :], in_=tmp[:, c * 128:(c + 1) * 128], identity=identf[:]),
                             reads=[("tmp", 0), ("tmp", 1), "identf"], writes=[psk(bh + c // 4)])
                    if _SUB <= 1:
                        return
                    for c in range(8):
                        if _SUB == 2 and c % 2 == 1:
                            continue
                        if _SUB == 3 and c % 2 == 0:
                            continue
                        if c < 4:
                            p.op("act", lambda e, c=c: e.activation(out=h2f[:, c, :], in_=pTh[:, c, :], func=AF.Identity,
                                                                    bias=modT[:, 24 + c, b:b + 1], scale=modT[:, 32 + c, b:b + 1]),
                                 reads=[psk(bh + c // 4), "modT"], writes=[("h2f", c)])
                        else:
                            p.op("dve", lambda e, c=c: e.tensor_scalar(out=h2f[:, c, :], in0=pTh[:, c, :], scalar1=modT[:, 32 + c, b:b + 1],
                                                                       scalar2=modT[:, 24 + c, b:b + 1], op0=ALU.mult, op1=ALU.add),
                                 reads=[psk(bh + c // 4), "modT"], writes=[("h2f", c)])
                    if _SUB <= 4:
                        return
                    h2keys = [("h2f", c) for c in range(8)]
                    p.op("pool", lambda e: e.tensor_copy(out=hT[:, :, t * 128:(t + 1) * 128], in_=h2f), reads=h2keys, writes=[("hT", tp)])
                    if _P3 <= 4:
                        return
                    br = nb()
                    for c in range(8):
                        p.op("pe", lambda e, c=c: e.matmul(PS(br, 16), lhsT=h2f[:, c, :], rhs=wR[:, c, :], start=(c == 0), stop=(c == 7)),
                             reads=[("h2f", c), "wR"], writes=[psk(br)])
                    if _P3 <= 5:
                        return
                    sc_ = small[:, 64:80]
                    sel_ = small[:, 80:96]
                    eq_ = small[:, 96:112]
                    s2_ = small[:, 112:128]
                    m1_ = small[:, 128:132]
                    m2_ = small[:, 132:136]
                    gs_ = small[:, 136:140]
                    gm_ = small[:, 140:141]
                    ing_ = small[:, 144:148]
                    t1_ = small[:, 148:149]
                    t2_ = small[:, 149:150]
                    den_ = small[:, 150:151]
                    e2_ = small[:, 160:176]
                    cm_ = small[:, 176:192]
                    v4 = lambda a: a.rearrange("p (g e) -> p g e", e=4)
                    R_ = ["rt"]
                    p.op("act", lambda e: e.activation(out=sc_, in_=PS(br, 16), func=AF.Sigmoid), reads=[psk(br)], writes=R_)
                    p.op("dve", lambda e: e.tensor_tensor(out=sel_, in0=sc_, in1=rb[:], op=ALU.add), reads=R_ + ["rb"], writes=R_)
                    p.op("dve", lambda e: e.tensor_reduce(out=m1_, in_=v4(sel_), axis=AX.X, op=ALU.max), reads=R_, writes=R_)
                    p.op("dve", lambda e: e.tensor_tensor(out=v4(eq_), in0=v4(sel_), in1=m1_.unsqueeze(2).to_broadcast([128, 4, 4]), op=ALU.is_equal), reads=R_, writes=R_)
                    p.op("dve", lambda e: e.scalar_tensor_tensor(out=s2_, in0=eq_, scalar=-1e9, in1=sel_, op0=ALU.mult, op1=ALU.add), reads=R_, writes=R_)
                    p.op("dve", lambda e: e.tensor_reduce(out=m2_, in_=v4(s2_), axis=AX.X, op=ALU.max), reads=R_, writes=R_)
                    p.op("dve", lambda e: e.tensor_tensor(out=gs_, in0=m1_, in1=m2_, op=ALU.add), reads=R_, writes=R_)
                    p.op("dve", lambda e: e.tensor_reduce(out=gm_, in_=gs_, axis=AX.X, op=ALU.max), reads=R_, writes=R_)
                    p.op("dve", lambda e: e.tensor_scalar(out=ing_, in0=gs_, scalar1=gm_, scalar2=None, op0=ALU.is_equal), reads=R_, writes=R_)
                    p.op("dve", lambda e: e.tensor_scalar(out=ing_, in0=ing_, scalar1=1.0, scalar2=1e9, op0=ALU.subtract, op1=ALU.mult), reads=R_, writes=R_)
                    p.op("dve", lambda e: e.tensor_tensor(out=v4(s2_), in0=v4(sel_), in1=ing_.unsqueeze(2).to_broadcast([128, 4, 4]), op=ALU.add), reads=R_, writes=R_)
                    p.op("dve", lambda e: e.tensor_reduce(out=t1_, in_=s2_, axis=AX.X, op=ALU.max), reads=R_, writes=R_)
                    p.op("dve", lambda e: e.tensor_scalar(out=eq_, in0=s2_, scalar1=t1_, scalar2=None, op0=ALU.is_equal), reads=R_, writes=R_)
                    p.op("dve", lambda e: e.scalar_tensor_tensor(out=s2_, in0=eq_, scalar=-1e9, in1=s2_, op0=ALU.mult, op1=ALU.add), reads=R_, writes=R_)
                    p.op("dve", lambda e: e.tensor_reduce(out=t2_, in_=s2_, axis=AX.X, op=ALU.max), reads=R_, writes=R_)
                    p.op("dve", lambda e: e.tensor_scalar(out=e2_, in0=s2_, scalar1=t2_, scalar2=None, op0=ALU.is_equal), reads=R_, writes=R_)
                    p.op("dve", lambda e: e.tensor_tensor(out=eq_, in0=eq_, in1=e2_, op=ALU.add), reads=R_, writes=R_)
                    p.op("dve", lambda e: e.tensor_tensor(out=cm_, in0=eq_, in1=sc_, op=ALU.mult), reads=R_, writes=R_)
                    p.op("dve", lambda e: e.tensor_reduce(out=den_, in_=cm_, axis=AX.X, op=ALU.add), reads=R_, writes=R_)
                    p.op("dve", lambda e: e.reciprocal(out=den_, in_=den_), reads=R_, writes=R_)
                    p.op("dve", lambda e: e.tensor_scalar(out=cm_, in0=cm_, scalar1=den_, scalar2=None, op0=ALU.mult), reads=R_, writes=R_)
                    if _P3 <= 6:
                        return
                    bc_ = nb()
                    p.op("pe", lambda e: e.transpose(out=PS(bc_, 128)[0:16, :], in_=cm_, identity=identf[:]), reads=R_ + ["identf"], writes=[psk(bc_)])
                    p.op("act", lambda e: e.copy(out=combT[0:16, t * 128:(t + 1) * 128], in_=PS(bc_, 128)[0:16, :]), reads=[psk(bc_)], writes=[("combT", t // 2)])

                stage_a(0)
                for t in range(NT if _P3 >= 99 else 1):
                    if t + 1 < NT and _P3 >= 99:
                        stage_a(t + 1)
                    if _P3 >= 2:
                        stage_b(t)
                if stage <= 5:
                    if l == 0 and b == 0 and "combT" in dbg_t:
                        tmpf = arW.alloc("dbgf", 0, [2048], F32)
                        p.op("dve", lambda e: e.tensor_copy(out=tmpf[0:16, :], in_=combT[0:16, :]), reads=[("combT", q_) for q_ in range(8)], writes=["dbgf"])
                        finals.append(p.dma("sp", lambda e: [e.dma_start(out=dbg_t["combT"], in_=tmpf[0:16, :])], reads=["dbgf"], writes=["dbgf_d"]))
                    finals.append(p.dma("sp", lambda e: [e.dma_start(out=y_out[b, 0:128, :], in_=xt3[0])], reads=["xt0"]))
                    return

                bank_lim[0] = 4
                bank_rr[0] = 0
                slots = []
                for si in range(8):
                    ar_ = arW if si < 4 else arA
                    o_ = (si % 4) * 12 * 1024
                    slots.append((ar_.alloc("wEG%d" % si, o_, [8, 256], BF16), ar_.alloc("wEU%d" % si, o_ + 4096, [8, 256], BF16),
                                  ar_.alloc("wED%d" % si, o_ + 8192, [2, D], BF16)))
                p.dma("sp", lambda e: [e.dma_start(out=lnp[:], in_=lnp_in[:, l, 2:4, :])], writes=["lnp"])
                make_bc(gbc[:], 40, b)
                sl = [arT.alloc("sl%d" % i, i * 1024, [256], F32) for i in range(2)]
                s2b = [arT.alloc("s2b%d" % i, 2048 + i * 1024, [256], F32) for i in range(2)]
                actT = [arT.alloc("actT%d" % i, 4096 + i * 512, [256], BF16) for i in range(4)]
                ytmp = [arT.alloc("ytmp%d" % i, 8192 + i * 4096, [D], F32) for i in range(2)]
                xt4 = [arT.alloc("xt%d" % i, 16384 + i * 4096, [D], F32) for i in range(2)]
                cnt = [0]
                yc = [0]

                def load_expert(e_):
                    si = (e_ // 4 % 2) * 4 + e_ % 4
                    g_, u_, d_ = slots[si]
                    p.dma("pool", lambda e: [e.dma_start(out=g_, in_=w_eg[l][e_].rearrange("(k p) f -> p k f", p=128)),
                                             e.dma_start(out=u_, in_=w_eu[l][e_].rearrange("(k p) f -> p k f", p=128)),
                                             e.dma_start(out=d_, in_=w_ed[l][e_].rearrange("(c p) n -> p c n", p=128))],
                          reads=[("w_eg", "f", l), ("w_eu", "f", l), ("w_ed", "f", l)], writes=["wEG%d" % si, "wEU%d" % si, "wED%d" % si], n=3)

                for e_ in range(8):
                    load_expert(e_)
                for eg in range(4):
                    for tc2 in range(8):
                        tok = slice(tc2 * 256, (tc2 + 1) * 256)
                        cbb = {}

                        def front(ei, fc, tok=tok, tc2=tc2, eg=eg):
                            e_ = eg * 4 + ei
                            si = (eg % 2) * 4 + ei
                            g_, u_, d_ = slots[si]
                            if fc == 0:
                                bcb = nb()
                                cbb[ei] = bcb
                                p.op("pe", lambda e: e.matmul(PS(bcb, 256), lhsT=selb[:, e_, :], rhs=combT[:, tok], start=True, stop=True),
                                     reads=["selb", ("combT", tc2)], writes=[psk(bcb)])
                            bcb = cbb[ei]
                            bgu = nb()
                            for k in range(8):
                                p.op("pe", lambda e, k=k: e.matmul(PS(bgu, 256, 0), lhsT=g_[:, k, fc * 128:(fc + 1) * 128], rhs=hT[:, k, tok],
                                                                   start=(k == 0), stop=(k == 7)), reads=["wEG%d" % si, ("hT", tc2)], writes=[psk(bgu)])
                            for k in range(8):
                                p.op("pe", lambda e, k=k: e.matmul(PS(bgu, 256, 256), lhsT=u_[:, k, fc * 128:(fc + 1) * 128], rhs=hT[:, k, tok],
                                                                   start=(k == 0), stop=(k == 7), skip_group_check=True), reads=["wEU%d" % si, ("hT", tc2)], writes=[psk(bgu)])
                            i2 = cnt[0] % 2
                            i4 = cnt[0] % 4
                            cnt[0] += 1
                            p.op("act", lambda e: e.activation(out=sl[i2], in_=PS(bgu, 256, 0), func=AF.Silu), reads=[psk(bgu)], writes=["sl%d" % i2])
                            p.op("dve", lambda e: e.tensor_tensor(out=s2b[i2], in0=PS(bgu, 256, 256), in1=sl[i2], op=ALU.mult),
                                 reads=[psk(bgu), "sl%d" % i2], writes=["s2b%d" % i2])
                            p.op("dve", lambda e: e.tensor_tensor(out=actT[i4], in0=PS(bcb, 256), in1=s2b[i2], op=ALU.mult),
                                 reads=[psk(bcb), "s2b%d" % i2], writes=["actT%d" % i4])
                            return (ei, fc, i4, d_, si)

                        def down(ei, fc, i4, d_, si):
                            for ti in range(2):
                                for n in range(2):
                                    yb = 4 + ti * 2 + n
                                    p.op("pe", lambda e, yb=yb, ti=ti, n=n: e.matmul(
                                        PS(yb), lhsT=actT[i4][:, ti * 128:(ti + 1) * 128], rhs=d_[:, fc, n * 512:(n + 1) * 512],
                                        start=(ei == 0 and fc == 0), stop=(ei == 3 and fc == 1)),
                                         reads=["actT%d" % i4, "wED%d" % si], writes=[psk(yb)])

                        prev = None
                        for ei in range(4):
                            for fc in range(2):
                                cur = front(ei, fc)
                                if prev is not None:
                                    down(*prev)
                                prev = cur
                        down(*prev)
                        for ti in range(2):
                            t = tc2 * 2 + ti
                            yi = yc[0] % 2
                            yc[0] += 1
                            p.op("dve", lambda e, ti=ti, yi=yi: e.tensor_tensor(out=ytmp[yi], in0=ps_all[:, (4 + 2 * ti) * 512:(6 + 2 * ti) * 512], in1=gbc[:], op=ALU.mult),
                                 reads=[psk(4 + 2 * ti), psk(5 + 2 * ti), "gbc"], writes=["ytmp%d" % yi])
                            xb = xt4[yi]
                            xk = "xt%d" % yi
                            p.dma("sp", lambda e, xb=xb, t=t: [e.dma_start(out=xb, in_=xs[b, t * 128:(t + 1) * 128, :])], reads=[xkey(t)], writes=[xk], slot=("xt", yi))
                            p.op("pool", lambda e, xb=xb, yi=yi: e.tensor_tensor(out=xb, in0=xb, in1=ytmp[yi], op=ALU.add), reads=[xk, "ytmp%d" % yi], writes=[xk])
                            p.dma("sp", lambda e, xb=xb, t=t: [e.dma_start(out=xs[b, t * 128:(t + 1) * 128, :], in_=xb)], reads=[xk], writes=[xkey(t)], slot=("xst", 1 + yi))
                    if eg + 2 < 4:
                        for ei in range(4):
                            load_expert((eg + 2) * 4 + ei)
                bank_lim[0] = 8
                for t in range(NT):
                    xb = xt4[t % 2]
                    xk = "xt%d" % (t % 2)
                    p.dma("sp", lambda e, xb=xb, t=t: [e.dma_start(out=xb, in_=xs[b, t * 128:(t + 1) * 128, :])], reads=[xkey(t)], writes=[xk], slot=("xt", t % 2))
                    ln_stats(xb, mv, rstd, xk, "p4")
                    p.op("dve", lambda e, xb=xb: e.tensor_scalar(out=xb, in0=xb, scalar1=mv[:, 0:1], scalar2=rstd, op0=ALU.subtract, op1=ALU.mult),
                         reads=[xk, ("mv", "p4"), ("rstd", "p4")], writes=[xk])
                    p.op("pool", lambda e, xb=xb: e.tensor_tensor(out=xb, in0=xb, in1=lnp[:, 0, :], op=ALU.mult), reads=[xk, "lnp"], writes=[xk])
                    p.op("pool", lambda e, xb=xb: e.tensor_tensor(out=xb, in0=xb, in1=lnp[:, 1, :], op=ALU.add), reads=[xk, "lnp"], writes=[xk])
                    o_ = p.dma("sp", lambda e, xb=xb, t=t: [e.dma_start(out=x_dst[b, t * 128:(t + 1) * 128, :], in_=xb)], reads=[xk], writes=[xkey(t)], slot=("xout", t % 2))
                    if last:
                        finals.append(o_)

        for l_ in range(n_layers):
            phase0(l_)
            for b_ in range(n_seq):
                block(l_, b_)

        p.emit(final_wait_ops=finals)
    return nc


def prep_shared(inputs):
    f = lambda a: np.ascontiguousarray(np.asarray(a, dtype=np.float32))
    cos, sin, E, sel = _const_tables()
    b_ada = f(inputs["b_ada"])
    sh = {
        "w_ada": f(inputs["w_ada"]),
        "w_po": np.ascontiguousarray(np.concatenate([f(inputs["w_branch_a"]), f(inputs["w_branch_b"]), f(inputs["w_out"])], axis=1)),
        "b_adaT": np.ascontiguousarray(b_ada.reshape(DEPTH, 48, 128).transpose(2, 0, 1)),
        "w_in": f(inputs["w_in"]),
        "w_pa": f(inputs["w_branch_a"]),
        "w_pb": f(inputs["w_branch_b"]),
        "w_o": f(inputs["w_out"]),
        "w_r": f(inputs["w_router"]),
        "w_eg": f(inputs["w_exp_gate"]),
        "w_eu": f(inputs["w_exp_up"]),
        "w_ed": f(inputs["w_exp_down"]),
        "cos_t": cos, "sin_t": sin, "E_t": E, "sel_t": sel,
        "idn": np.eye(128, dtype=np.float32),
    }
    qg = f(inputs["q_norm_g"])
    kg = f(inputs["k_norm_g"])
    qk = np.concatenate([np.tile(qg[:, None, :], (1, 8, 1)), np.tile(kg[:, None, :], (1, 2, 1))], 1).reshape(DEPTH, 640)
    sh["qkg"] = np.ascontiguousarray(np.broadcast_to(qk[None], (128, DEPTH, 640)))
    lnp = np.stack([f(inputs["ln1_g"]), f(inputs["ln1_b"]), f(inputs["ln2_g"]), f(inputs["ln2_b"])], 1)
    sh["lnp"] = np.ascontiguousarray(np.broadcast_to(lnp[None], (128, DEPTH, 4, D)))
    sh["rb"] = np.ascontiguousarray(np.broadcast_to(f(inputs["router_bias"])[None], (128, 16)))
    return sh


_GATHERED = ("w_ada", "w_in", "w_po", "w_eg", "w_eu", "w_ed")


def prep_core(inputs, sh, core, n_cores=8):
    x = np.asarray(inputs["x"], dtype=np.float32)
    c = np.asarray(inputs["c"], dtype=np.float32)
    m = {k: v for k, v in sh.items() if k not in ("w_pa", "w_pb", "w_o")}
    for k in _GATHERED:
        w = m.pop(k)
        w2 = w.reshape(DEPTH, -1, w.shape[-1])
        if n_cores == 1 or not _GATHER:
            m[k] = w2
        else:
            r = w2.shape[1] // n_cores
            m[k + "_sh"] = np.ascontiguousarray(w2[:, core * r:(core + 1) * r, :])
    m["x"] = np.ascontiguousarray(x[2 * core:2 * core + 2])
    cc = c[2 * core:2 * core + 2]
    m["cT"] = np.ascontiguousarray(cc.reshape(2, 8, 128).transpose(2, 1, 0))
    return m


_NC_CACHE = {}


def kernel(**inputs):
    if "nc" not in _NC_CACHE:
        _NC_CACHE["nc"] = build_nc()
    nc = _NC_CACHE["nc"]
    sh = prep_shared(inputs)
    in_maps = [prep_core(inputs, sh, i) for i in range(8)]
    res = run_bass_kernel_spmd(nc, in_maps, core_ids=list(range(8)))
    return np.concatenate([np.asarray(r["y"]) for r in res.results], axis=0).astype(np.float32)
```

```python
import numpy as np
from contextlib import ExitStack
import concourse.bass as bass
import concourse.mybir as mybir
from concourse.bass_utils import run_bass_kernel_spmd

F32 = mybir.dt.float32
BF16 = mybir.dt.bfloat16
ALU = mybir.AluOpType
AF = mybir.ActivationFunctionType
AX = mybir.AxisListType

ENGS = ("pe", "act", "dve", "pool", "sp")

DEPTH = 4
S = 2048
D = 1024
NT = 16
ALPHA = (2.0 * DEPTH) ** 0.25
B_GROUPS = ((128, 1), (512, 4), (2048, 16))
import os as _os
_DBG_GROUPS = [int(c_) for c_ in _os.environ.get('DBG_GROUPS', '012')]
_P3 = int(_os.environ.get('DBG_P3', '99'))
_GATHER = int(_os.environ.get('KGATHER', '0'))
_SUB = int(_os.environ.get('DBG_SUB', '99'))


_op_counter = [0]


class _Op:
    __slots__ = ("eng", "fn", "deps", "needed", "semval", "dma_sem", "dma_val", "ndma", "semidx")

    def __init__(self, eng, fn, deps):
        _op_counter[0] += 1
        self.semidx = _op_counter[0]
        self.eng = eng
        self.fn = fn
        self.deps = deps
        self.needed = False
        self.semval = 0
        self.dma_sem = None
        self.dma_val = 0
        self.ndma = 0


def _base(k):
    return k[0] if isinstance(k, tuple) else k


class Prog:
    def __init__(self, nc):
        self.nc = nc
        self.ops = {e: [] for e in ENGS}
        self.last_w = {}
        self.readers = {}
        self.by_base = {}
        self.base_deps = {}
        self.dma_slots = {}
        self.n_dma_sems = 0

    def _deps(self, reads, writes, eng=None):
        deps = []
        for r in reads:
            w = self.last_w.get(r)
            if w is not None:
                deps.append(w)
            if _base(r) == "ps":
                for o in self.readers.get(r, ()):
                    if o.eng != eng:
                        deps.append(o)
            bd = self.base_deps.get(_base(r))
            if bd:
                deps.extend(bd)
        for w_ in writes:
            w = self.last_w.get(w_)
            if w is not None:
                deps.append(w)
            rs = self.readers.get(w_)
            if rs:
                deps.extend(rs)
            bd = self.base_deps.get(_base(w_))
            if bd:
                deps.extend(bd)
        return deps

    def _track(self, o, reads, writes):
        for r in reads:
            self.readers.setdefault(r, []).append(o)
            self.by_base.setdefault(_base(r), set()).add(r)
        for w in writes:
            self.last_w[w] = o
            self.readers[w] = []
            self.by_base.setdefault(_base(w), set()).add(w)

    @staticmethod
    def _compress(deps):
        best = {}
        for d in deps:
            if d.dma_sem is not None:
                k = ("d", d.dma_sem)
                prev = best.get(k)
                if prev is None or d.dma_val > prev.dma_val:
                    best[k] = d
            else:
                prev = best.get(d.eng)
                if prev is None or d.semidx > prev.semidx:
                    best[d.eng] = d
        return list(best.values())

    def op(self, eng, fn, reads=(), writes=()):
        deps = self._compress(self._deps(reads, writes, eng))
        o = _Op(eng, fn, deps)
        for d in deps:
            d.needed = True
        self.ops[eng].append(o)
        self._track(o, reads, writes)
        return o

    def dma(self, queue, fn, reads=(), writes=(), slot=None, n=1):
        deps = self._compress(self._deps(reads, writes))
        o = _Op(queue, fn, deps)
        for d in deps:
            d.needed = True
        if slot is None:
            if writes:
                slot = ("w", writes[0])
            else:
                self._auto = getattr(self, "_auto", 0) + 1
                slot = ("auto", self._auto % 16)
        st = self.dma_slots.get(slot)
        if st is None:
            st = [self.n_dma_sems, 0]
            self.n_dma_sems += 1
            self.dma_slots[slot] = st
        st[1] += 16 * n
        o.dma_sem = st[0]
        o.dma_val = st[1]
        o.ndma = n
        self.ops[queue].append(o)
        self._track(o, reads, writes)
        return o

    def recycle(self, new_base, old_bases):
        acc = {}
        dmas = []

        def add(o):
            if o.dma_sem is not None:
                dmas.append(o)
            else:
                prev = acc.get(o.eng)
                if prev is None or o.semidx > prev.semidx:
                    acc[o.eng] = o

        for ob in old_bases:
            for o in self.base_deps.get(ob, []):
                add(o)
            for k in self.by_base.get(ob, ()):
                for o in self.readers.get(k, []):
                    add(o)
                w = self.last_w.get(k)
                if w is not None:
                    add(w)
        for k in self.by_base.get(new_base, ()):
            self.last_w.pop(k, None)
            self.readers.pop(k, None)
        self.base_deps[new_base] = self._compress(list(acc.values()) + dmas)
        self.by_base[new_base] = set()

    def emit(self, final_wait_ops=()):
        nc = self.nc
        with ExitStack() as es:
            EPOCH = 12000
            dsem = [es.enter_context(nc.semaphore("d_%d" % i)) for i in range(self.n_dma_sems)]
            esem = {}
            for e in ENGS:
                c = 0
                for o in self.ops[e]:
                    if o.dma_sem is None and o.needed:
                        o.semval = (c // EPOCH, c % EPOCH + 1)
                        c += 1
                esem[e] = [es.enter_context(nc.semaphore("s_%s_%d" % (e, i))) for i in range(c // EPOCH + 1)]
            self.sem_counts = {e: len(v) for e, v in esem.items()}
            block = es.enter_context(nc.Block())

            def run(eng_name):
                def body(eng):
                    waited = {}
                    for o in self.ops[eng_name]:
                        for d in o.deps:
                            if d.dma_sem is not None:
                                key = ("d", d.dma_sem)
                                val = d.dma_val
                                sem = dsem[d.dma_sem]
                            else:
                                if d.eng == "pe" and eng_name == "pe":
                                    continue
                                key = d.eng
                                val = d.semval
                                if waited.get(key, (-1, 0)) >= val:
                                    continue
                                waited[key] = val
                                eng.wait_ge(esem[d.eng][val[0]], val[1])
                                continue
                            if waited.get(key, 0) >= val:
                                continue
                            waited[key] = val
                            eng.wait_ge(sem, val)
                        if o.dma_sem is not None:
                            insts = o.fn(eng)
                            assert len(insts) == o.ndma, (len(insts), o.ndma)
                            for ins in insts:
                                ins.then_inc(dsem[o.dma_sem], 16)
                        else:
                            ins = o.fn(eng)
                            if o.needed:
                                ins.then_inc(esem[eng_name][o.semval[0]], 1)
                    if eng_name == "sp":
                        for d in final_wait_ops:
                            eng.wait_ge(dsem[d.dma_sem], d.dma_val)
                return body

            block.tensor(run("pe"))
            block.scalar(run("act"))
            block.vector(run("dve"))
            block.gpsimd(run("pool"))
            block.sync(run("sp"))


class Arena:
    def __init__(self, p, ap2d, name):
        self.p = p
        self.ap = ap2d
        self.n = ap2d.shape[1]
        self.name = name
        self.regions = []

    def alloc(self, base, lo_bytes, free_shape, dt):
        nel = int(np.prod(free_shape))
        nb = nel * (4 if dt == F32 else 2)
        lo = lo_bytes // 2
        hi = lo + nb // 2
        assert hi <= self.n, (self.name, base, hi, self.n)
        old = []
        keep = []
        for (l, h, b) in self.regions:
            if l < hi and lo < h:
                if b not in old:
                    old.append(b)
                if l < lo:
                    keep.append((l, lo, b))
                if h > hi:
                    keep.append((hi, h, b))
            else:
                keep.append((l, h, b))
        keep.append((lo, hi, base))
        self.regions = keep
        self.p.recycle(base, old)
        v = self.ap[:, lo:hi]
        if dt == F32:
            v = v.bitcast(F32)
        if len(free_shape) == 2:
            v = v.rearrange("p (a b) -> p a b", b=free_shape[1])
        elif len(free_shape) == 3:
            v = v.rearrange("p (a b c) -> p a b c", b=free_shape[1], c=free_shape[2])
        return v


def _const_tables():
    pos = np.arange(S)
    row = (pos // 64).astype(np.float32)
    col = (pos % 64).astype(np.float32)
    inv = (10000.0 ** (-np.arange(0, 32, 2, dtype=np.float32) / 32.0)).astype(np.float32)
    ang = np.concatenate([row[:, None] * inv, col[:, None] * inv], -1).astype(np.float32)
    cos = np.cos(ang).astype(np.float32).reshape(NT, 128, 32).transpose(1, 0, 2)
    sin = np.sin(ang).astype(np.float32).reshape(NT, 128, 32).transpose(1, 0, 2)
    slopes = np.exp2(-8.0 * np.arange(1, 13, dtype=np.float32) / 12.0).astype(np.float32)
    i = np.arange(128)[:, None]
    c = np.arange(384)[None, :]
    delta = np.abs(c - 128 - i).astype(np.float32)
    E = np.zeros((128, 12, 384), np.float32)
    for h in range(12):
        d = B_GROUPS[h // 4][1]
        E[:, h, :] = np.where(delta <= 64, np.exp(-slopes[h] * d * delta), 0.0)
    sel = np.zeros((128, 16, 128), np.float32)
    for e in range(16):
        sel[e, e, :] = 1.0
    return np.ascontiguousarray(cos), np.ascontiguousarray(sin), E, sel


def build_nc(n_layers=DEPTH, n_seq=2, stage=99, dbg=None, n_cores=8):
    nc = bass.Bass("TRN2", target_bir_lowering=False)
    dbg = dbg or {}

    def din(name, shape, dt=F32):
        return nc.dram_tensor(name, list(shape), dt, kind="ExternalInput").ap()

    x_in = din("x", [2, S, D])
    cT_in = din("cT", [128, 8, 2])
    gather_jobs = []

    def gathered(name, rows, cols):
        if n_cores == 1 or not _GATHER:
            full = din(name, [DEPTH, rows, cols])
            return [full[l_] for l_ in range(DEPTH)]
        shd = din(name + "_sh", [DEPTH, rows // n_cores, cols])
        fulls = []
        for l_ in range(DEPTH):
            bnc = nc.dram_tensor("%s_b%d" % (name, l_), [rows // n_cores, cols], F32, kind="Internal").ap()
            ful = nc.dram_tensor("%s_f%d" % (name, l_), [rows, cols], F32, kind="Internal", addr_space="Shared").ap()
            gather_jobs.append((name, l_, shd[l_], bnc, ful))
            fulls.append(ful)
        return fulls

    w_ada = gathered("w_ada", D, 6 * D)
    b_adaT = din("b_adaT", [128, DEPTH, 48])
    w_in = gathered("w_in", D, 5120)
    qkg_in = din("qkg", [128, DEPTH, 640])
    w_po = gathered("w_po", 1792, D)
    w_pa = [w_[0:512, :] for w_ in w_po]
    w_pb = [w_[512:768, :] for w_ in w_po]
    w_o = [w_[768:1792, :] for w_ in w_po]
    lnp_in = din("lnp", [128, DEPTH, 4, D])
    w_r = din("w_r", [D, 16])
    rb_in = din("rb", [128, 16])
    w_eg = [w_.rearrange("(e d) f -> e d f", e=16) for w_ in gathered("w_eg", 16 * D, 256)]
    w_eu = [w_.rearrange("(e d) f -> e d f", e=16) for w_ in gathered("w_eu", 16 * D, 256)]
    w_ed = [w_.rearrange("(e f) d -> e f d", e=16) for w_ in gathered("w_ed", 16 * 256, D)]
    cos_in = din("cos_t", [128, NT, 32])
    sin_in = din("sin_t", [128, NT, 32])
    E_in = din("E_t", [128, 12, 384])
    sel_in = din("sel_t", [128, 16, 128])
    idn_in = din("idn", [128, 128])
    y_out = nc.dram_tensor("y", [2, S, D], F32, kind="ExternalOutput").ap()
    xs = nc.dram_tensor("xs", [2, S, D], F32, kind="Internal").ap()
    dbg_t = {k: nc.dram_tensor("dbg_" + k, list(shp), F32, kind="ExternalOutput").ap() for k, shp in dbg.items()}

    es = ExitStack()
    with es:
        def sb(name, shape, dt):
            return es.enter_context(nc.sbuf_tensor(name, list(shape), dt))

        p = Prog(nc)
        finals = []

        hT = sb("hT", [128, 8, S], BF16)
        ACT_A = sb("arenaA", [128, 28 * 1024], BF16)
        W_A = sb("arenaW", [128, 31 * 1024], BF16)
        T_A = sb("arenaT", [128, 15 * 1024 + 512], BF16)
        identf = sb("identf", [128, 128], F32)
        identb = sb("identb", [128, 128], BF16)
        onesf = sb("onesf", [128, 128], F32)
        selb = sb("selb", [128, 16, 128], BF16)
        combT = sb("combT", [128, S], BF16)
        modT = sb("modT", [128, 48, 2], F32)
        badaT = sb("badaT", [128, DEPTH, 48], F32)
        condT = sb("condT", [128, 8, 2], F32)
        lnp = sb("lnp_sb", [128, 2, D], F32)
        gbc = sb("gbc", [128, D], F32)
        wR = sb("wR", [128, 8, 16], F32)
        rb = sb("rb_sb", [128, 16], F32)
        small = sb("small", [128, 256], F32)
        diag = sb("diag", [128, 2, 128], F32)
        ps_all = es.enter_context(nc.psum_tensor("ps_all", [128, 8 * 512], F32))

        arA = Arena(p, ACT_A[:], "A")
        arW = Arena(p, W_A[:], "W")
        arT = Arena(p, T_A[:], "T")

        def PS(b, n=512, off=0):
            return ps_all[:, b * 512 + off: b * 512 + off + n]

        def PSB(b, nbanks=1):
            return ps_all[:, b * 512:(b + nbanks) * 512].bitcast(BF16)

        psk = lambda b: ("ps", b)
        bank_rr = [0]
        bank_lim = [8]

        def nb(n=1):
            b = bank_rr[0]
            if b % n:
                b += n - (b % n)
            if b + n > bank_lim[0]:
                b = 0
            bank_rr[0] = (b + n) % bank_lim[0]
            return b

        order = {"w_ada": 0, "w_in": 1, "w_po": 2, "w_eg": 3, "w_eu": 4, "w_ed": 5}
        for (name_, l_, shd_, bnc_, ful_) in sorted(gather_jobs, key=lambda j_: (j_[1], order[j_[0]])):
            if l_ >= n_layers:
                continue
            p.dma("sp", lambda e, shd_=shd_, bnc_=bnc_: [e.dma_start(out=bnc_, in_=shd_)], writes=[(name_, "b", l_)], slot=("gb", l_ % 2, name_))
            p.dma("pool", lambda e, bnc_=bnc_, ful_=ful_: [e.collective_compute("AllGather", ALU.bypass, replica_groups=[list(range(n_cores))],
                                                                                 ins=[bnc_], outs=[ful_])],
                  reads=[(name_, "b", l_)], writes=[(name_, "f", l_)], slot=("gf", l_ % 2, name_))

        p.dma("sp", lambda e: [e.dma_start(out=identf[:], in_=idn_in[:])], writes=["identf"])
        p.op("dve", lambda e: e.tensor_copy(out=identb[:], in_=identf[:]), reads=["identf"], writes=["identb"])
        p.op("dve", lambda e: e.memset(onesf[:], 1.0), writes=["onesf"])
        p.op("dve", lambda e: e.memset(combT[:], 0.0), writes=["combT"])
        p.dma("pool", lambda e: [e.dma_start(out=selb[:], in_=sel_in[:])], writes=["selb"])
        p.dma("sp", lambda e: [e.dma_start(out=badaT[:], in_=b_adaT[:])], writes=["badaT"])
        p.dma("sp", lambda e: [e.dma_start(out=condT[:], in_=cT_in[:])], writes=["condT"])
        p.dma("sp", lambda e: [e.dma_start(out=wR[:], in_=w_r.rearrange("(k p) n -> p k n", p=128))], writes=["wR"])
        p.dma("sp", lambda e: [e.dma_start(out=rb[:], in_=rb_in[:])], writes=["rb"])
        p.op("act", lambda e: e.activation(out=condT[:], in_=condT[:], func=AF.Silu), reads=["condT"], writes=["condT"])

        def dbg_out(name, src_ap, reads):
            if name in dbg_t:
                finals.append(p.dma("sp", lambda e: [e.dma_start(out=dbg_t[name], in_=src_ap)], reads=reads))

        def ln_stats(src, mv, rstd, key_src, tag):
            st = small[:, 0:12].rearrange("p (a b) -> p a b", b=6)
            for i in range(2):
                p.op("dve", lambda e, i=i: e.bn_stats(out=st[:, i, :], in_=src[:, i * 512:(i + 1) * 512]),
                     reads=[key_src], writes=[("st", i)])
            p.op("dve", lambda e: e.bn_aggr(out=mv, in_=small[:, 0:12]), reads=[("st", 0), ("st", 1)], writes=[("mv", tag)])
            p.op("act", lambda e: e.activation(out=rstd, in_=mv[:, 1:2], func=AF.Sqrt, bias=1e-5, scale=1.0),
                 reads=[("mv", tag)], writes=[("rstd", tag)])
            p.op("dve", lambda e: e.reciprocal(out=rstd, in_=rstd), reads=[("rstd", tag)], writes=[("rstd", tag)])

        def make_bc(dst, chunk0, b):
            for half in range(2):
                bk = nb()
                for cc in range(4):
                    c = half * 4 + cc
                    dg = diag[:, c % 2, :]
                    p.op("dve", lambda e, dg=dg, c=c: e.tensor_scalar(out=dg, in0=identf[:], scalar1=modT[:, chunk0 + c, b:b + 1],
                                                                      scalar2=None, op0=ALU.mult),
                         reads=["identf", "modT"], writes=[("diag", c % 2)])
                    p.op("pe", lambda e, dg=dg, cc=cc, bk=bk: e.matmul(PS(bk, 128, cc * 128), lhsT=onesf[:], rhs=dg, start=True, stop=True),
                         reads=["onesf", ("diag", c % 2)], writes=[psk(bk)])
                p.op("act", lambda e, half=half, bk=bk: e.copy(out=dst[:, half * 512:(half + 1) * 512], in_=PS(bk)),
                     reads=[psk(bk)], writes=["gbc"])

        def phase0(l):
            wst = [arW.alloc("wada%d" % i, 44 * 1024 + i * 8192, [8, 256], F32) for i in range(2)]
            bkm = nb()
            for s_ in range(24):
                buf = wst[s_ % 2]
                key = ("wada", s_ % 2)
                p.dma("sp", lambda e, buf=buf, s_=s_: [e.dma_start(out=buf, in_=w_ada[l][:, s_ * 256:(s_ + 1) * 256].rearrange("(k p) n -> p k n", p=128))],
                      reads=[("w_ada", "f", l)], writes=["wada%d" % (s_ % 2)], slot=key)
                for jj in range(2):
                    j = 2 * s_ + jj
                    for k in range(8):
                        p.op("pe", lambda e, buf=buf, jj=jj, j=j, k=k: e.matmul(PS(bkm, 2, 2 * j), lhsT=buf[:, k, jj * 128:(jj + 1) * 128],
                                                                                 rhs=condT[:, k, :], start=(k == 0), stop=(k == 7)),
                             reads=["wada%d" % (s_ % 2), "condT"], writes=[psk(bkm)])
            p.op("dve", lambda e: e.tensor_tensor(out=modT[:], in0=PS(bkm, 96).rearrange("p (j b) -> p j b", b=2),
                                                  in1=badaT[:, l, :].unsqueeze(2).to_broadcast([128, 48, 2]), op=ALU.add),
                 reads=[psk(bkm), "badaT"], writes=["modT"])
            for c0 in (8, 32):
                p.op("dve", lambda e, c0=c0: e.tensor_scalar(out=modT[:, c0:c0 + 8, :], in0=modT[:, c0:c0 + 8, :], scalar1=1.0, scalar2=None, op0=ALU.add),
                     reads=["modT"], writes=["modT"])
            if l == 0:
                dbg_out("modT", modT[:], ["modT"])

        def block(l, b):
                x_src = x_in if l == 0 else xs
                last = (l == n_layers - 1)
                x_dst = y_out if last else xs
                xkey = lambda t: ("xs", b, t)

                xt = [arT.alloc("xt%d" % i, i * 4096, [D], F32) for i in range(3)]
                xh = [arT.alloc("xh%d" % i, 12288 + i * 2048, [D], BF16) for i in range(2)]
                mv = small[:, 16:18]
                rstd = small[:, 18:19]
                for tp in range(NT // 2):
                    bk = nb(2)
                    pTv = PSB(bk, 2).rearrange("p (c n) -> p c n", n=256)
                    for ti in range(2):
                        t = tp * 2 + ti
                        xb = xt[t % 3]
                        xk = "xt%d" % (t % 3)
                        p.dma("sp", lambda e, xb=xb, t=t: [e.dma_start(out=xb, in_=x_src[b, t * 128:(t + 1) * 128, :])],
                              reads=[xkey(t)], writes=[xk], slot=("xt", t % 3))
                        ln_stats(xb, mv, rstd, xk, "p1")
                        hb = xh[t % 2]
                        hk = "xh%d" % (t % 2)
                        p.op("dve", lambda e, xb=xb, hb=hb: e.tensor_scalar(out=hb, in0=xb, scalar1=mv[:, 0:1], scalar2=rstd, op0=ALU.subtract, op1=ALU.mult),
                             reads=[xk, ("mv", "p1"), ("rstd", "p1")], writes=[hk])
                        for c in range(8):
                            p.op("pe", lambda e, hb=hb, c=c, ti=ti, pTv=pTv: e.transpose(out=pTv[:, c, ti * 128:(ti + 1) * 128], in_=hb[:, c * 128:(c + 1) * 128], identity=identb[:]),
                                 reads=[hk, "identb"], writes=[psk(bk + c // 4)])
                    for c in range(8):
                        dst = hT[:, c, tp * 256:(tp + 1) * 256]
                        if c < 4:
                            p.op("act", lambda e, c=c, dst=dst, pTv=pTv: e.activation(out=dst, in_=pTv[:, c, :], func=AF.Identity,
                                                                                    bias=modT[:, c, b:b + 1], scale=modT[:, 8 + c, b:b + 1]),
                                 reads=[psk(bk + c // 4), "modT"], writes=[("hT", tp)])
                        else:
                            p.op("dve", lambda e, c=c, dst=dst, pTv=pTv: e.tensor_scalar(out=dst, in0=pTv[:, c, :], scalar1=modT[:, 8 + c, b:b + 1],
                                                                                       scalar2=modT[:, c, b:b + 1], op0=ALU.mult, op1=ALU.add),
                                 reads=[psk(bk + c // 4), "modT"], writes=[("hT", tp)])
                if l == 0 and b == 0:
                    dbg_out("hT", None, None) if False else None
                hT_all = [("hT", tp) for tp in range(8)]
                if stage <= 1:
                    return

                out_aT = arA.alloc("out_aT", 0, [4, S], BF16)
                out_bT = arA.alloc("out_bT", 16384, [2, S], BF16)
                qaT = arA.alloc("qaT", 24576, [4, S], BF16)
                kaT = arA.alloc("kaT", 40960, [2, S], BF16)
                vaP = arA.alloc("vaP", 49152, [NT, 2, 128], BF16)
                wA = arW.alloc("wA", 0, [8, 768], BF16)
                Et = arW.alloc("Et", 48 * 1024, [12, 384], BF16)
                cos_t = arW.alloc("cos", 16 * 1024, [NT, 32], F32)
                sin_t = arW.alloc("sin", 18 * 1024, [NT, 32], F32)
                qkg = arW.alloc("qkg", 12 * 1024, [640], F32)
                p.dma("pool", lambda e: [e.dma_start(out=Et, in_=E_in[:])], writes=["Et"])
                p.dma("sp", lambda e: [e.dma_start(out=cos_t, in_=cos_in[:])], writes=["cos"])
                p.dma("sp", lambda e: [e.dma_start(out=sin_t, in_=sin_in[:])], writes=["sin"])
                p.dma("sp", lambda e: [e.dma_start(out=qkg, in_=qkg_in[:, l, :])], writes=["qkg"])
                p.dma("pool", lambda e: [e.dma_start(out=wA, in_=w_in[l][:, 0:768].rearrange("(k p) n -> p k n", p=128))], reads=[("w_in", "f", l)], writes=["wA"])
                sq = arT.alloc("sq", 0, [640], F32)
                qn = arT.alloc("qn", 2560, [10, 64], F32)
                qr = arT.alloc("qr", 5120, [10, 64], BF16)
                ra = arT.alloc("ra", 6400, [10, 32], F32)
                rb_ = arT.alloc("rb_", 7680, [10, 32], F32)
                pt = [arT.alloc("pt%d" % i, 9216 + i * 1024, [512], BF16) for i in range(4)]
                Rr = [arT.alloc("Rr%d" % i, 13312 + i * 2048, [512], F32) for i in range(2)]
                ss = small[:, 32:42]
                rr = small[:, 48:58]
                p.op("pool", lambda e: e.memset(vaP, 1.0), writes=["vaP"])
                p.op("pool", lambda e: e.memset(kaT, 0.0), writes=["kaT"] + [("kaT", t_) for t_ in range(NT)])
                for t in range(NT):
                    bq = nb()
                    bkv = nb()
                    tp = t // 2
                    for k in range(8):
                        lh = hT[:, k, t * 128:(t + 1) * 128]
                        p.op("pe", lambda e, lh=lh, k=k, bq=bq: e.matmul(PS(bq), lhsT=lh, rhs=wA[:, k, 0:512], start=(k == 0), stop=(k == 7)),
                             reads=[("hT", tp), "wA"], writes=[psk(bq)])
                        p.op("pe", lambda e, lh=lh, k=k, bkv=bkv: e.matmul(PS(bkv, 256), lhsT=lh, rhs=wA[:, k, 512:768], start=(k == 0), stop=(k == 7)),
                             reads=[("hT", tp), "wA"], writes=[psk(bkv)])
                    p.op("act", lambda e, bq=bq: e.activation(out=sq[:, 0:512], in_=PS(bq), func=AF.Square), reads=[psk(bq)], writes=["sq"])
                    p.op("act", lambda e, bkv=bkv: e.activation(out=sq[:, 512:640], in_=PS(bkv, 128), func=AF.Square), reads=[psk(bkv)], writes=["sq"])
                    p.op("dve", lambda e: e.tensor_reduce(out=ss, in_=sq.rearrange("p (h d) -> p h d", d=64), axis=AX.X, op=ALU.add),
                         reads=["sq"], writes=["ss"])
                    p.op("act", lambda e: e.activation(out=rr, in_=ss, func=AF.Sqrt, bias=1e-6, scale=1.0 / 64.0), reads=["ss"], writes=["rr"])
                    p.op("dve", lambda e: e.reciprocal(out=rr, in_=rr), reads=["rr"], writes=["rr"])
                    p.op("dve", lambda e, bq=bq: e.tensor_tensor(out=qn[:, 0:8, :].rearrange("p (c g) d -> p g c d", g=2),
                                                               in0=PS(bq).rearrange("p (g c d) -> p g c d", g=2, c=4),
                                                               in1=rr[:, 0:8].rearrange("p (g c) -> p g c", g=2).unsqueeze(3).to_broadcast([128, 2, 4, 64]), op=ALU.mult),
                         reads=[psk(bq), "rr"], writes=["qn"])
                    p.op("dve", lambda e, bkv=bkv: e.tensor_tensor(out=qn[:, 8:10, :], in0=PS(bkv, 128).rearrange("p (h d) -> p h d", d=64),
                                                                 in1=rr[:, 8:10].unsqueeze(2).to_broadcast([128, 2, 64]), op=ALU.mult),
                         reads=[psk(bkv), "rr"], writes=["qn"])
                    p.op("dve", lambda e: e.tensor_tensor(out=qn, in0=qn, in1=qkg.rearrange("p (h d) -> p h d", d=64), op=ALU.mult),
                         reads=["qn", "qkg"], writes=["qn"])
                    qv = qn.rearrange("p h (d two) -> p h d two", two=2)
                    x0 = qv[:, :, :, 0]
                    x1 = qv[:, :, :, 1]
                    cb_ = cos_t[:, t, :].unsqueeze(1).to_broadcast([128, 10, 32])
                    sb_ = sin_t[:, t, :].unsqueeze(1).to_broadcast([128, 10, 32])
                    p.op("dve", lambda e, x0=x0, cb_=cb_: e.tensor_tensor(out=ra, in0=x0, in1=cb_, op=ALU.mult), reads=["qn", "cos"], writes=["ra"])
                    p.op("dve", lambda e, x1=x1, sb_=sb_: e.tensor_tensor(out=rb_, in0=x1, in1=sb_, op=ALU.mult), reads=["qn", "sin"], writes=["rb_"])
                    p.op("dve", lambda e: e.tensor_tensor(out=qr[:, :, 0:32], in0=ra, in1=rb_, op=ALU.subtract), reads=["ra", "rb_"], writes=["qr"])
                    p.op("dve", lambda e, x0=x0, sb_=sb_: e.tensor_tensor(out=ra, in0=x0, in1=sb_, op=ALU.mult), reads=["qn", "sin", "qr"], writes=["ra"])
                    p.op("dve", lambda e, x1=x1, cb_=cb_: e.tensor_tensor(out=rb_, in0=x1, in1=cb_, op=ALU.mult), reads=["qn", "cos", "qr"], writes=["rb_"])
                    p.op("dve", lambda e: e.tensor_tensor(out=qr[:, :, 32:64], in0=ra, in1=rb_, op=ALU.add), reads=["ra", "rb_"], writes=["qr"])
                    bt = nb()
                    pTq = PSB(bt)[:, 0:640].rearrange("p (c n) -> p c n", n=128)
                    for c in range(5):
                        src = qr.rearrange("p h d -> p (h d)")[:, c * 128:(c + 1) * 128]
                        p.op("pe", lambda e, c=c, src=src, pTq=pTq: e.transpose(out=pTq[:, c, :], in_=src, identity=identb[:]),
                             reads=["qr", "identb"], writes=[psk(bt)])
                    p.op("act", lambda e, t=t, pTq=pTq: e.copy(out=qaT[:, :, t * 128:(t + 1) * 128], in_=pTq[:, 0:4, :]), reads=[psk(bt)], writes=[("qaT", t // 4)])
                    p.op("act", lambda e, t=t, pTq=pTq: e.copy(out=kaT[0:64, 0, t * 128:(t + 1) * 128], in_=pTq[0:64, 4, :]), reads=[psk(bt), "kaT"], writes=[("kaT", t)])
                    p.op("act", lambda e, t=t, pTq=pTq: e.copy(out=kaT[64:128, 1, t * 128:(t + 1) * 128], in_=pTq[64:128, 4, :]), reads=[psk(bt), "kaT"], writes=[("kaT", t)])
                    p.op("dve", lambda e, t=t, bkv=bkv: e.tensor_copy(out=vaP[:, t, 0, 0:64], in_=PS(bkv, 64, 128)), reads=[psk(bkv), "vaP"], writes=[("vaP", t)])
                    p.op("dve", lambda e, t=t, bkv=bkv: e.tensor_copy(out=vaP[:, t, 1, 64:128], in_=PS(bkv, 64, 192)), reads=[psk(bkv), "vaP"], writes=[("vaP", t)])
                if l == 0 and b == 0:
                    dbg_out("qaT", None, None) if False else None

                if stage <= 2:
                    return
                ptc = [0]
                for g in range(2):
                    pb0 = 64 * g
                    for c in range(4):
                        for qc in range(4):
                            bo = nb()
                            pend = []

                            def do_pv(kb, bs, bo=bo, g=g):
                                i = ptc[0] % 4
                                ptc[0] += 1
                                ptb = pt[i]
                                p.op("act", lambda e, bs=bs, ptb=ptb: e.activation(out=ptb, in_=PS(bs), func=AF.Exp, scale=0.125),
                                     reads=[psk(bs)], writes=["pt%d" % i])
                                p.op("pe", lambda e, kb=kb, ptb=ptb: e.matmul(PS(bo), lhsT=vaP[:, kb, g, :], rhs=ptb, start=(kb == 0), stop=(kb == NT - 1)),
                                     reads=["pt%d" % i, ("vaP", kb)], writes=[psk(bo)])

                            for kb in range(NT):
                                bs = nb()
                                while bs == bo:
                                    bs = nb()
                                p.op("pe", lambda e, kb=kb, bs=bs, c=c, qc=qc, g=g: e.matmul(
                                    PS(bs), lhsT=kaT[:, g, kb * 128:(kb + 1) * 128], rhs=qaT[:, c, qc * 512:(qc + 1) * 512], start=True, stop=True),
                                     reads=[("kaT", kb), ("qaT", qc)], writes=[psk(bs)])
                                pend.append((kb, bs))
                                if len(pend) > 2:
                                    do_pv(*pend.pop(0))
                            while pend:
                                do_pv(*pend.pop(0))
                            ri = (g * 16 + c * 4 + qc) % 2
                            R = Rr[ri]
                            zlo = 64 - pb0
                            p.op("dve", lambda e, R=R, bo=bo, zlo=zlo: e.reciprocal(out=R[zlo:zlo + 64, :], in_=PS(bo)[zlo:zlo + 64, :]),
                                 reads=[psk(bo)], writes=["Rr%d" % ri])
                            p.op("dve", lambda e, R=R, bo=bo, zlo=zlo, pb0=pb0, c=c, qc=qc: e.tensor_tensor(
                                out=out_aT[pb0:pb0 + 64, c, qc * 512:(qc + 1) * 512], in0=PS(bo)[pb0:pb0 + 64, :], in1=R[zlo:zlo + 64, :], op=ALU.mult),
                                 reads=[psk(bo), "Rr%d" % ri], writes=[("out_aT", qc)])
                if l == 0 and b == 0 and "out_aT" in dbg_t:
                    tmpf = arW.alloc("dbgf", 12 * 1024, [4, 1024], F32)
                    for hh in range(2):
                        p.op("dve", lambda e, hh=hh: e.tensor_copy(out=tmpf, in_=out_aT[:, :, hh * 1024:(hh + 1) * 1024]), reads=[("out_aT", q_) for q_ in range(4)], writes=["dbgf"])
                        finals.append(p.dma("sp", lambda e, hh=hh: [e.dma_start(out=dbg_t["out_aT"][:, :, hh * 1024:(hh + 1) * 1024], in_=tmpf)], reads=["dbgf"], writes=["dbgf_d"]))
                        p.op("dve", lambda e: e.memset(small[:, 200:201], 0.0), reads=["dbgf_d"], writes=["dbgf"])
                if stage <= 3:
                    return
                bank_lim[0] = 4
                bank_rr[0] = 0
                QT = arA.alloc("QT", 24 * 1024, [3, S], BF16)
                KT = arA.alloc("KT", 36 * 1024, [3, S], BF16)
                VP = [arA.alloc("VP%d" % i, 48 * 1024 + i * 4096, [NT, 128], BF16) for i in range(2)]
                wB = [arW.alloc("wB%d" % i, 12 * 1024 + i * 18 * 1024, [8, 9 * 128], BF16) for i in range(2)]
                expf = [arT.alloc("expf%d" % i, i * 1536, [384], F32) for i in range(3)]
                pt2 = [arT.alloc("pt2_%d" % i, 4608 + i * 768, [384], BF16) for i in range(3)]
                accb = arT.alloc("accb", 20 * 1024, [S], F32)
                QTp = arW.alloc("QTp", 0, [3, S], BF16)
                Rr = [arT.alloc("Rr%d" % i, 13312 + i * 2048, [512], F32) for i in range(2)]
                vpc = [0]
                ec = [0]
                for jp in range(2):
                    wb = wB[jp]
                    wk = "wB%d" % jp
                    cols = []
                    for base_c in (768, 1536, 2304):
                        for m in (jp, 2 + jp, 4 + jp):
                            cols.append(base_c + m * 128)
                    p.dma("pool", lambda e, wb=wb, cols=cols: [e.dma_start(out=wb[:, :, i * 128:(i + 1) * 128],
                                                                            in_=w_in[l][:, c0:c0 + 128].rearrange("(k p) n -> p k n", p=128))
                                                               for i, c0 in enumerate(cols)], reads=[("w_in", "f", l)], writes=[wk], n=9)
                    for which, dstT, nm in ((0, QT, "QT"), (1, KT, "KT")):
                        for mi in range(3):
                            for tc in range(4):
                                bk = nb()
                                for k in range(8):
                                    p.op("pe", lambda e, bk=bk, k=k, wb=wb, which=which, mi=mi, tc=tc: e.matmul(
                                        PS(bk), lhsT=wb[:, k, (which * 3 + mi) * 128:(which * 3 + mi + 1) * 128], rhs=hT[:, k, tc * 512:(tc + 1) * 512],
                                        start=(k == 0), stop=(k == 7)), reads=[wk, ("hT", 2 * tc), ("hT", 2 * tc + 1)], writes=[psk(bk)])
                                dst = dstT[:, mi, tc * 512:(tc + 1) * 512]
                                if which == 0:
                                    p.op("act", lambda e, dst=dst, bk=bk: e.copy(out=dst, in_=PS(bk)), reads=[psk(bk)], writes=[(nm, mi, tc)])
                                else:
                                    p.op("dve", lambda e, dst=dst, bk=bk: e.tensor_copy(out=dst, in_=PS(bk)), reads=[psk(bk)], writes=[(nm, mi, tc)])
                    if l == 0 and b == 0 and jp == 0 and "KT" in dbg_t:
                        tmpk = arW.alloc("dbgk", 0, [2, S], F32)
                        g0 = _DBG_GROUPS[0]
                        p.op("dve", lambda e: e.tensor_copy(out=tmpk[:, 0, :], in_=KT[:, g0, :]), reads=[("KT", g0, q_) for q_ in range(4)], writes=["dbgk"])
                        p.op("dve", lambda e: e.tensor_copy(out=tmpk[:, 1, :], in_=QT[:, g0, :]), reads=[("QT", g0, q_) for q_ in range(4)], writes=["dbgk"])
                        finals.append(p.dma("sp", lambda e: [e.dma_start(out=dbg_t["KT"], in_=tmpk)], reads=["dbgk"], writes=["dbgk_d"]))
                        p.op("dve", lambda e: e.memset(small[:, 202:203], 0.0), reads=["dbgk_d"], writes=["dbgk"])
                    for jj in range(2):
                        j = 2 * jp + jj
                        pb0 = 64 * jj
                        zlo = 64 - pb0
                        p.op("pool", lambda e, zlo=zlo: e.memset(QTp[zlo:zlo + 64, :, :], 0.0), writes=["QTp"])
                        p.op("act", lambda e, pb0=pb0: e.copy(out=QTp[pb0:pb0 + 64, :, :], in_=QT[pb0:pb0 + 64, :, :]),
                             reads=[("QT", mi_, tc_) for mi_ in range(3) for tc_ in range(4)] + ["QTp"], writes=["QTp"])
                        for gi in _DBG_GROUPS:
                            d = B_GROUPS[gi][1]
                            L_ = S // d
                            nkb = L_ // 128
                            nbq = 512 // d
                            VPb = VP[vpc[0] % 2]
                            vk = "VP%d" % (vpc[0] % 2)
                            vpc[0] += 1
                            p.op("pool", lambda e, VPb=VPb: e.memset(VPb, 1.0), writes=[vk] + [(vk, q_) for q_ in range(4)])
                            for blk4 in range(4):
                                bk = nb()
                                for bi in range(4):
                                    blk = blk4 * 4 + bi
                                    r, kb = divmod(blk, nkb)
                                    st0 = r + d * 128 * kb
                                    for k in range(8):
                                        p.op("pe", lambda e, bk=bk, bi=bi, k=k, st0=st0, d=d, wb=wb, gi=gi, jj=jj: e.matmul(
                                            PS(bk, 64, bi * 64), lhsT=hT[:, k, st0:st0 + 127 * d + 1:d],
                                            rhs=wb[:, k, (6 + gi) * 128 + jj * 64:(6 + gi) * 128 + jj * 64 + 64], start=(k == 0), stop=(k == 7)),
                                             reads=[wk] + hT_all, writes=[psk(bk)])
                                p.op("dve", lambda e, bk=bk, VPb=VPb, blk4=blk4, pb0=pb0: e.tensor_copy(
                                    out=VPb[:, blk4 * 4:(blk4 + 1) * 4, pb0:pb0 + 64], in_=PS(bk, 256).rearrange("p (b d) -> p b d", d=64)),
                                     reads=[psk(bk), vk], writes=[(vk, blk4)])
                            if l == 0 and b == 0 and jp == 0 and jj == 0 and gi == _DBG_GROUPS[0] and "VP" in dbg_t:
                                tmpv = arW.alloc("dbgv", 0, [NT, 128], F32)
                                p.op("dve", lambda e, VPb=VPb: e.tensor_copy(out=tmpv, in_=VPb), reads=[(vk, q_) for q_ in range(4)], writes=["dbgv"])
                                finals.append(p.dma("sp", lambda e: [e.dma_start(out=dbg_t["VP"], in_=tmpv)], reads=["dbgv"], writes=["dbgv_d"]))
                                p.op("dve", lambda e: e.memset(small[:, 201:202], 0.0), reads=["dbgv_d"], writes=["dbgv"])
                            pend = []

                            def do_pv3(r, qb, kbl, bs, VPb=VPb, vk=vk, gi=gi, d=d, nkb=nkb, j=j, jj=jj):
                                i = ec[0] % 3
                                ec[0] += 1
                                w_ = 128 * len(kbl)
                                off0 = 128 + 128 * (qb - kbl[0])
                                p.op("act", lambda e, i=i, bs=bs, w_=w_: e.activation(out=expf[i][:, 0:w_], in_=PS(bs, w_), func=AF.Exp, scale=0.125),
                                     reads=[psk(bs)], writes=["expf%d" % i])
                                p.op("dve", lambda e, i=i, off0=off0, w_=w_: e.tensor_tensor(out=pt2[i][:, 0:w_], in0=expf[i][:, 0:w_], in1=Et[:, 4 * gi + j, off0:off0 + w_], op=ALU.mult),
                                     reads=["expf%d" % i, "Et"], writes=["pt2_%d" % i])
                                if d == 1:
                                    pieces = [(qb // 4, (qb % 4) * 128, (qb % 4) * 128 + 128, 0, 128)]
                                elif d == 4:
                                    pieces = [(qb, r, r + 4 * 127 + 1, 0, 128)]
                                else:
                                    pieces = [(bq_, r, r + 16 * 31 + 1, 32 * bq_, 32 * bq_ + 32) for bq_ in range(4)]
                                for (bq_, c0, c1, a0, a1) in pieces:
                                    for n_, kb in enumerate(kbl):
                                        blk = r * nkb + kb
                                        p.op("pe", lambda e, bq_=bq_, c0=c0, c1=c1, a0=a0, a1=a1, i=i, blk=blk, n_=n_: e.matmul(
                                            PS(4 + bq_)[:, c0:c1:d], lhsT=VPb[:, blk, :], rhs=pt2[i][:, 128 * n_ + a0:128 * n_ + a1],
                                            start=(n_ == 0), stop=(n_ == len(kbl) - 1), skip_group_check=True),
                                             reads=["pt2_%d" % i, (vk, blk // 4)], writes=[psk(4 + bq_)])

                            for r in range(d):
                                for qb in range(nkb):
                                    kbl = [kb for kb in (qb + 1, qb, qb - 1) if 0 <= kb < nkb]
                                    bs = nb()
                                    for n_, kb in enumerate(kbl):
                                        ks0 = r + d * 128 * kb
                                        q0 = r + d * 128 * qb
                                        p.op("pe", lambda e, bs=bs, ks0=ks0, q0=q0, d=d, gi=gi, pb0=pb0, n_=n_: e.matmul(
                                            PS(bs, 128, 128 * n_), lhsT=KT[:, gi, ks0:ks0 + 127 * d + 1:d], rhs=QTp[:, gi, q0:q0 + 127 * d + 1:d],
                                            start=True, stop=True, skip_group_check=True),
                                             reads=[("KT", gi, tc_) for tc_ in range(4)] + ["QTp"], writes=[psk(bs)])
                                    pend.append((r, qb, kbl, bs))
                                    if len(pend) > 2:
                                        do_pv3(*pend.pop(0))
                            while pend:
                                do_pv3(*pend.pop(0))
                            for bq_ in range(4):
                                accs = accb[:, bq_ * 512:(bq_ + 1) * 512]
                                if gi == _DBG_GROUPS[0] and gi != _DBG_GROUPS[-1]:
                                    p.op("dve", lambda e, bq_=bq_, accs=accs: e.tensor_copy(out=accs, in_=PS(4 + bq_)), reads=[psk(4 + bq_)], writes=[("accb", bq_)])
                                elif gi != _DBG_GROUPS[-1]:
                                    p.op("dve", lambda e, bq_=bq_, accs=accs: e.tensor_tensor(out=accs, in0=PS(4 + bq_), in1=accs, op=ALU.add),
                                         reads=[psk(4 + bq_), ("accb", bq_)], writes=[("accb", bq_)])
                                elif len(_DBG_GROUPS) > 1:
                                    p.op("dve", lambda e, bq_=bq_, accs=accs: e.tensor_tensor(out=PS(4 + bq_), in0=PS(4 + bq_), in1=accs, op=ALU.add),
                                         reads=[psk(4 + bq_), ("accb", bq_)], writes=[psk(4 + bq_)])
                        for bq_ in range(4):
                            ri = bq_ % 2
                            R = Rr[ri]
                            p.op("dve", lambda e, R=R, bq_=bq_, zlo=zlo: e.reciprocal(out=R[zlo:zlo + 64, :], in_=PS(4 + bq_)[zlo:zlo + 64, :]),
                                 reads=[psk(4 + bq_)], writes=["Rr%d" % ri])
                            p.op("dve", lambda e, R=R, bq_=bq_, zlo=zlo, pb0=pb0, jp=jp: e.tensor_tensor(
                                out=out_bT[pb0:pb0 + 64, jp, bq_ * 512:(bq_ + 1) * 512], in0=PS(4 + bq_)[pb0:pb0 + 64, :], in1=R[zlo:zlo + 64, :], op=ALU.mult),
                                 reads=[psk(4 + bq_), "Rr%d" % ri], writes=[("out_bT", bq_)])
                bank_lim[0] = 8
                if l == 0 and b == 0 and "out_bT" in dbg_t:
                    tmpf = arW.alloc("dbgf", 0, [2, 2048], F32)
                    p.op("dve", lambda e: e.tensor_copy(out=tmpf, in_=out_bT), reads=[("out_bT", q_) for q_ in range(4)], writes=["dbgf"])
                    finals.append(p.dma("sp", lambda e: [e.dma_start(out=dbg_t["out_bT"], in_=tmpf)], reads=["dbgf"], writes=["dbgf_d"]))
                    p.op("dve", lambda e: e.memset(small[:, 200:201], 0.0), reads=["dbgf_d"], writes=["dbgf"])
                if stage <= 4:
                    return

                wG = arW.alloc("wG", 0, [8, 2048], BF16)
                wPA = arW.alloc("wPA", 32 * 1024, [4, D], BF16)
                wPB = arW.alloc("wPB", 40 * 1024, [2, D], BF16)
                wO = arW.alloc("wO", 44 * 1024, [8, D], BF16)
                p.dma("pool", lambda e: [e.dma_start(out=wG, in_=w_in[l][:, 3072:5120].rearrange("(k p) n -> p k n", p=128))], reads=[("w_in", "f", l)], writes=["wG"])
                p.dma("pool", lambda e: [e.dma_start(out=wPA[g_ * 64:(g_ + 1) * 64, c_, :], in_=w_pa[l][(4 * g_ + c_) * 64:(4 * g_ + c_ + 1) * 64, :])
                                         for c_ in range(4) for g_ in range(2)], reads=[("w_po", "f", l)], writes=["wPA"], n=8)
                p.dma("pool", lambda e: [e.dma_start(out=wPB, in_=w_pb[l].rearrange("(j p) n -> p j n", p=128))], reads=[("w_po", "f", l)], writes=["wPB"])
                p.dma("pool", lambda e: [e.dma_start(out=wO, in_=w_o[l].rearrange("(k p) n -> p k n", p=128))], reads=[("w_po", "f", l)], writes=["wO"])
                p.dma("sp", lambda e: [e.dma_start(out=lnp[:], in_=lnp_in[:, l, 0:2, :])], writes=["lnp"])
                make_bc(gbc[:], 16, b)
                xt3 = [arT.alloc("xt%d" % i, i * 4096, [D], F32) for i in range(2)]
                tmp = arT.alloc("tmp", 8192, [D], F32)
                sig = arT.alloc("sig", 12288, [2048], BF16)
                m1 = arT.alloc("m1", 16384, [D], F32)
                mrg = [arT.alloc("mrg%d" % i, 20480 + i * 2048, [D], BF16) for i in range(2)]
                mT = arT.alloc("mT", 24576, [8, 128], BF16)
                h2f = arT.alloc("h2f", 26624, [8, 128], F32)
                mv = small[:, 16:18]
                rstd = small[:, 18:19]

                def stage_a(t):
                    tp = t // 2
                    xb = xt3[t % 2]
                    xk = "xt%d" % (t % 2)
                    p.dma("sp", lambda e: [e.dma_start(out=xb, in_=x_src[b, t * 128:(t + 1) * 128, :])], reads=[xkey(t)], writes=[xk], slot=("xt", t % 2))
                    for n in range(4):
                        bk = nb()
                        for k in range(8):
                            p.op("pe", lambda e, bk=bk, k=k, n=n: e.matmul(PS(bk), lhsT=hT[:, k, t * 128:(t + 1) * 128], rhs=wG[:, k, n * 512:(n + 1) * 512],
                                                                       start=(k == 0), stop=(k == 7)), reads=[("hT", tp), "wG"], writes=[psk(bk)])
                        p.op("act", lambda e, bk=bk, n=n: e.activation(out=sig[:, n * 512:(n + 1) * 512], in_=PS(bk), func=AF.Sigmoid),
                             reads=[psk(bk)], writes=[("sig", n)])
                    for n in range(2):
                        bk = nb()
                        for c in range(4):
                            p.op("pe", lambda e, bk=bk, c=c, n=n: e.matmul(PS(bk), lhsT=out_aT[:, c, t * 128:(t + 1) * 128], rhs=wPA[:, c, n * 512:(n + 1) * 512],
                                                                       start=(c == 0), stop=(c == 3)), reads=[("out_aT", t // 4), "wPA"], writes=[psk(bk)])
                        p.op("dve", lambda e, bk=bk, n=n: e.tensor_tensor(out=m1[:, n * 512:(n + 1) * 512], in0=PS(bk), in1=sig[:, n * 512:(n + 1) * 512], op=ALU.mult),
                             reads=[psk(bk), ("sig", n)], writes=[("m1", n)])
                    for n in range(2):
                        bk = nb()
                        for c in range(2):
                            p.op("pe", lambda e, bk=bk, c=c, n=n: e.matmul(PS(bk), lhsT=out_bT[:, c, t * 128:(t + 1) * 128], rhs=wPB[:, c, n * 512:(n + 1) * 512],
                                                                       start=(c == 0), stop=(c == 1)), reads=[("out_bT", t // 4), "wPB"], writes=[psk(bk)])
                        p.op("dve", lambda e, bk=bk, n=n: e.tensor_tensor(out=tmp[:, n * 512:(n + 1) * 512], in0=PS(bk), in1=sig[:, 1024 + n * 512:1024 + (n + 1) * 512], op=ALU.mult),
                             reads=[psk(bk), ("sig", 2 + n)], writes=[("tmp", n)])
                    p.op("pool", lambda e: e.tensor_tensor(out=mrg[t % 2], in0=m1, in1=tmp, op=ALU.add),
                         reads=[("m1", 0), ("m1", 1), ("tmp", 0), ("tmp", 1)], writes=["mrg%d" % (t % 2)])

                def stage_b(t):
                    tp = t // 2
                    xb = xt3[t % 2]
                    xk = "xt%d" % (t % 2)
                    mk = "mrg%d" % (t % 2)
                    bt = nb()
                    pTm = PSB(bt).rearrange("p (c n) -> p c n", n=128)
                    for c in range(8):
                        p.op("pe", lambda e, c=c: e.transpose(out=pTm[:, c, :], in_=mrg[t % 2][:, c * 128:(c + 1) * 128], identity=identb[:]),
                             reads=[mk, "identb"], writes=[psk(bt)])
                    p.op("act", lambda e: e.copy(out=mT, in_=pTm), reads=[psk(bt)], writes=["mT"])
                    bo = nb(2)
                    for n in range(2):
                        for k in range(8):
                            p.op("pe", lambda e, k=k, n=n: e.matmul(PS(bo + n), lhsT=mT[:, k, :], rhs=wO[:, k, n * 512:(n + 1) * 512], start=(k == 0), stop=(k == 7)),
                                 reads=["mT", "wO"], writes=[psk(bo + n)])
                    p.op("dve", lambda e: e.tensor_tensor(out=tmp, in0=ps_all[:, bo * 512:(bo + 2) * 512], in1=gbc[:], op=ALU.mult),
                         reads=[psk(bo), psk(bo + 1), "gbc"], writes=[("tmp", 0), ("tmp", 1)])
                    p.op("dve", lambda e: e.scalar_tensor_tensor(out=xb, in0=xb, scalar=float(ALPHA), in1=tmp, op0=ALU.mult, op1=ALU.add),
                         reads=[xk, ("tmp", 0), ("tmp", 1)], writes=[xk])
                    ln_stats(xb, mv, rstd, xk, "p3")
                    p.op("dve", lambda e: e.tensor_scalar(out=xb, in0=xb, scalar1=mv[:, 0:1], scalar2=rstd, op0=ALU.subtract, op1=ALU.mult),
                         reads=[xk, ("mv", "p3"), ("rstd", "p3")], writes=[xk])
                    p.op("pool", lambda e: e.tensor_tensor(out=xb, in0=xb, in1=lnp[:, 0, :], op=ALU.mult), reads=[xk, "lnp"], writes=[xk])
                    p.op("pool", lambda e: e.tensor_tensor(out=xb, in0=xb, in1=lnp[:, 1, :], op=ALU.add), reads=[xk, "lnp"], writes=[xk])
                    p.op("act", lambda e: e.mul(out=m1, in_=xb, mul=float(ALPHA)), reads=[xk], writes=[("m1", 0), ("m1", 1)])
                    p.dma("sp", lambda e: [e.dma_start(out=xs[b, t * 128:(t + 1) * 128, :], in_=m1)], reads=[("m1", 0), ("m1", 1)], writes=[xkey(t)], slot=("xst", 0))
                    if _P3 <= 2:
                        return
                    ln_stats(xb, mv, rstd, xk, "p3b")
                    p.op("dve", lambda e: e.tensor_scalar(out=tmp, in0=xb, scalar1=mv[:, 0:1], scalar2=rstd, op0=ALU.subtract, op1=ALU.mult),
                         reads=[xk, ("mv", "p3b"), ("rstd", "p3b")], writes=[("tmp", 0), ("tmp", 1)])
                    if _P3 <= 3:
                        return
                    bh = nb(2)
                    pTh = ps_all[:, bh * 512:(bh + 2) * 512].rearrange("p (c n) -> p c n", n=128)
                    for c in range(8):
                        p.op("pe", lambda e, c=c: e.transpose(out=pTh[:, c, :], in_=tmp[:, c * 128:(c + 1) * 128], identity=identf[:]),
                             reads=[("tmp", 0), ("tmp", 1), "identf"], writes=[psk(bh + c // 4)])
                    if _SUB <= 1:
                        return
                    for c in range(8):
                        if _SUB == 2 and c % 2 == 1:
                            continue
                        if _SUB == 3 and c % 2 == 0:
                            continue
                        if c < 4:
                            p.op("act", lambda e, c=c: e.activation(out=h2f[:, c, :], in_=pTh[:, c, :], func=AF.Identity,
                                                                    bias=modT[:, 24 + c, b:b + 1], scale=modT[:, 32 + c, b:b + 1]),
                                 reads=[psk(bh + c // 4), "modT"], writes=[("h2f", c)])
                        else:
                            p.op("dve", lambda e, c=c: e.tensor_scalar(out=h2f[:, c, :], in0=pTh[:, c, :], scalar1=modT[:, 32 + c, b:b + 1],
                                                                       scalar2=modT[:, 24 + c, b:b + 1], op0=ALU.mult, op1=ALU.add),
                                 reads=[psk(bh + c // 4), "modT"], writes=[("h2f", c)])
                    if _SUB <= 4:
                        return
                    h2keys = [("h2f", c) for c in range(8)]
                    p.op("pool", lambda e: e.tensor_copy(out=hT[:, :, t * 128:(t + 1) * 128], in_=h2f), reads=h2keys, writes=[("hT", tp)])
                    if _P3 <= 4:
                        return
                    br = nb()
                    for c in range(8):
                        p.op("pe", lambda e, c=c: e.matmul(PS(br, 16), lhsT=h2f[:, c, :], rhs=wR[:, c, :], start=(c == 0), stop=(c == 7)),
                             reads=[("h2f", c), "wR"], writes=[psk(br)])
                    if _P3 <= 5:
                        return
                    sc_ = small[:, 64:80]
                    sel_ = small[:, 80:96]
                    eq_ = small[:, 96:112]
                    s2_ = small[:, 112:128]
                    m1_ = small[:, 128:132]
                    m2_ = small[:, 132:136]
                    gs_ = small[:, 136:140]
                    gm_ = small[:, 140:141]
                    ing_ = small[:, 144:148]
                    t1_ = small[:, 148:149]
                    t2_ = small[:, 149:150]
                    den_ = small[:, 150:151]
                    e2_ = small[:, 160:176]
                    cm_ = small[:, 176:192]
                    v4 = lambda a: a.rearrange("p (g e) -> p g e", e=4)
                    R_ = ["rt"]
                    p.op("act", lambda e: e.activation(out=sc_, in_=PS(br, 16), func=AF.Sigmoid), reads=[psk(br)], writes=R_)
                    p.op("dve", lambda e: e.tensor_tensor(out=sel_, in0=sc_, in1=rb[:], op=ALU.add), reads=R_ + ["rb"], writes=R_)
                    p.op("dve", lambda e: e.tensor_reduce(out=m1_, in_=v4(sel_), axis=AX.X, op=ALU.max), reads=R_, writes=R_)
                    p.op("dve", lambda e: e.tensor_tensor(out=v4(eq_), in0=v4(sel_), in1=m1_.unsqueeze(2).to_broadcast([128, 4, 4]), op=ALU.is_equal), reads=R_, writes=R_)
                    p.op("dve", lambda e: e.scalar_tensor_tensor(out=s2_, in0=eq_, scalar=-1e9, in1=sel_, op0=ALU.mult, op1=ALU.add), reads=R_, writes=R_)
                    p.op("dve", lambda e: e.tensor_reduce(out=m2_, in_=v4(s2_), axis=AX.X, op=ALU.max), reads=R_, writes=R_)
                    p.op("dve", lambda e: e.tensor_tensor(out=gs_, in0=m1_, in1=m2_, op=ALU.add), reads=R_, writes=R_)
                    p.op("dve", lambda e: e.tensor_reduce(out=gm_, in_=gs_, axis=AX.X, op=ALU.max), reads=R_, writes=R_)
                    p.op("dve", lambda e: e.tensor_scalar(out=ing_, in0=gs_, scalar1=gm_, scalar2=None, op0=ALU.is_equal), reads=R_, writes=R_)
                    p.op("dve", lambda e: e.tensor_scalar(out=ing_, in0=ing_, scalar1=1.0, scalar2=1e9, op0=ALU.subtract, op1=ALU.mult), reads=R_, writes=R_)
                    p.op("dve", lambda e: e.tensor_tensor(out=v4(s2_), in0=v4(sel_), in1=ing_.unsqueeze(2).to_broadcast([128, 4, 4]), op=ALU.add), reads=R_, writes=R_)
                    p.op("dve", lambda e: e.tensor_reduce(out=t1_, in_=s2_, axis=AX.X, op=ALU.max), reads=R_, writes=R_)
                    p.op("dve", lambda e: e.tensor_scalar(out=eq_, in0=s2_, scalar1=t1_, scalar2=None, op0=ALU.is_equal), reads=R_, writes=R_)
                    p.op("dve", lambda e: e.scalar_tensor_tensor(out=s2_, in0=eq_, scalar=-1e9, in1=s2_, op0=ALU.mult, op1=ALU.add), reads=R_, writes=R_)
                    p.op("dve", lambda e: e.tensor_reduce(out=t2_, in_=s2_, axis=AX.X, op=ALU.max), reads=R_, writes=R_)
                    p.op("dve", lambda e: e.tensor_scalar(out=e2_, in0=s2_, scalar1=t2_, scalar2=None, op0=ALU.is_equal), reads=R_, writes=R_)
                    p.op("dve", lambda e: e.tensor_tensor(out=eq_, in0=eq_, in1=e2_, op=ALU.add), reads=R_, writes=R_)
                    p.op("dve", lambda e: e.tensor_tensor(out=cm_, in0=eq_, in1=sc_, op=ALU.mult), reads=R_, writes=R_)
                    p.op("dve", lambda e: e.tensor_reduce(out=den_, in_=cm_, axis=AX.X, op=ALU.add), reads=R_, writes=R_)
                    p.op("dve", lambda e: e.reciprocal(out=den_, in_=den_), reads=R_, writes=R_)
                    p.op("dve", lambda e: e.tensor_scalar(out=cm_, in0=cm_, scalar1=den_, scalar2=None, op0=ALU.mult), reads=R_, writes=R_)
                    if _P3 <= 6:
                        return
                    bc_ = nb()
                    p.op("pe", lambda e: e.transpose(out=PS(bc_, 128)[0:16, :], in_=cm_, identity=identf[:]), reads=R_ + ["identf"], writes=[psk(bc_)])
                    p.op("act", lambda e: e.copy(out=combT[0:16, t * 128:(t + 1) * 128], in_=PS(bc_, 128)[0:16, :]), reads=[psk(bc_)], writes=[("combT", t // 2)])

                stage_a(0)
                for t in range(NT if _P3 >= 99 else 1):
                    if t + 1 < NT and _P3 >= 99:
                        stage_a(t + 1)
                    if _P3 >= 2:
                        stage_b(t)
                if stage <= 5:
                    if l == 0 and b == 0 and "combT" in dbg_t:
                        tmpf = arW.alloc("dbgf", 0, [2048], F32)
                        p.op("dve", lambda e: e.tensor_copy(out=tmpf[0:16, :], in_=combT[0:16, :]), reads=[("combT", q_) for q_ in range(8)], writes=["dbgf"])
                        finals.append(p.dma("sp", lambda e: [e.dma_start(out=dbg_t["combT"], in_=tmpf[0:16, :])], reads=["dbgf"], writes=["dbgf_d"]))
                    finals.append(p.dma("sp", lambda e: [e.dma_start(out=y_out[b, 0:128, :], in_=xt3[0])], reads=["xt0"]))
                    return

                bank_lim[0] = 4
                bank_rr[0] = 0
                slots = []
                for si in range(8):
                    ar_ = arW if si < 4 else arA
                    o_ = (si % 4) * 12 * 1024
                    slots.append((ar_.alloc("wEG%d" % si, o_, [8, 256], BF16), ar_.alloc("wEU%d" % si, o_ + 4096, [8, 256], BF16),
                                  ar_.alloc("wED%d" % si, o_ + 8192, [2, D], BF16)))
                p.dma("sp", lambda e: [e.dma_start(out=lnp[:], in_=lnp_in[:, l, 2:4, :])], writes=["lnp"])
                make_bc(gbc[:], 40, b)
                sl = [arT.alloc("sl%d" % i, i * 1024, [256], F32) for i in range(2)]
                s2b = [arT.alloc("s2b%d" % i, 2048 + i * 1024, [256], F32) for i in range(2)]
                actT = [arT.alloc("actT%d" % i, 4096 + i * 512, [256], BF16) for i in range(4)]
                ytmp = [arT.alloc("ytmp%d" % i, 8192 + i * 4096, [D], F32) for i in range(2)]
                xt4 = [arT.alloc("xt%d" % i, 16384 + i * 4096, [D], F32) for i in range(2)]
                cnt = [0]
                yc = [0]

                def load_expert(e_):
                    si = (e_ // 4 % 2) * 4 + e_ % 4
                    g_, u_, d_ = slots[si]
                    p.dma("pool", lambda e: [e.dma_start(out=g_, in_=w_eg[l][e_].rearrange("(k p) f -> p k f", p=128)),
                                             e.dma_start(out=u_, in_=w_eu[l][e_].rearrange("(k p) f -> p k f", p=128)),
                                             e.dma_start(out=d_, in_=w_ed[l][e_].rearrange("(c p) n -> p c n", p=128))],
                          reads=[("w_eg", "f", l), ("w_eu", "f", l), ("w_ed", "f", l)], writes=["wEG%d" % si, "wEU%d" % si, "wED%d" % si], n=3)

                for e_ in range(8):
                    load_expert(e_)
                for eg in range(4):
                    for tc2 in range(8):
                        tok = slice(tc2 * 256, (tc2 + 1) * 256)
                        cbb = {}

                        def front(ei, fc, tok=tok, tc2=tc2, eg=eg):
                            e_ = eg * 4 + ei
                            si = (eg % 2) * 4 + ei
                            g_, u_, d_ = slots[si]
                            if fc == 0:
                                bcb = nb()
                                cbb[ei] = bcb
                                p.op("pe", lambda e: e.matmul(PS(bcb, 256), lhsT=selb[:, e_, :], rhs=combT[:, tok], start=True, stop=True),
                                     reads=["selb", ("combT", tc2)], writes=[psk(bcb)])
                            bcb = cbb[ei]
                            bgu = nb()
                            for k in range(8):
                                p.op("pe", lambda e, k=k: e.matmul(PS(bgu, 256, 0), lhsT=g_[:, k, fc * 128:(fc + 1) * 128], rhs=hT[:, k, tok],
                                                                   start=(k == 0), stop=(k == 7)), reads=["wEG%d" % si, ("hT", tc2)], writes=[psk(bgu)])
                            for k in range(8):
                                p.op("pe", lambda e, k=k: e.matmul(PS(bgu, 256, 256), lhsT=u_[:, k, fc * 128:(fc + 1) * 128], rhs=hT[:, k, tok],
                                                                   start=(k == 0), stop=(k == 7), skip_group_check=True), reads=["wEU%d" % si, ("hT", tc2)], writes=[psk(bgu)])
                            i2 = cnt[0] % 2
                            i4 = cnt[0] % 4
                            cnt[0] += 1
                            p.op("act", lambda e: e.activation(out=sl[i2], in_=PS(bgu, 256, 0), func=AF.Silu), reads=[psk(bgu)], writes=["sl%d" % i2])
                            p.op("dve", lambda e: e.tensor_tensor(out=s2b[i2], in0=PS(bgu, 256, 256), in1=sl[i2], op=ALU.mult),
                                 reads=[psk(bgu), "sl%d" % i2], writes=["s2b%d" % i2])
                            p.op("dve", lambda e: e.tensor_tensor(out=actT[i4], in0=PS(bcb, 256), in1=s2b[i2], op=ALU.mult),
                                 reads=[psk(bcb), "s2b%d" % i2], writes=["actT%d" % i4])
                            return (ei, fc, i4, d_, si)

                        def down(ei, fc, i4, d_, si):
                            for ti in range(2):
                                for n in range(2):
                                    yb = 4 + ti * 2 + n
                                    p.op("pe", lambda e, yb=yb, ti=ti, n=n: e.matmul(
                                        PS(yb), lhsT=actT[i4][:, ti * 128:(ti + 1) * 128], rhs=d_[:, fc, n * 512:(n + 1) * 512],
                                        start=(ei == 0 and fc == 0), stop=(ei == 3 and fc == 1)),
                                         reads=["actT%d" % i4, "wED%d" % si], writes=[psk(yb)])

                        prev = None
                        for ei in range(4):
                            for fc in range(2):
                                cur = front(ei, fc)
                                if prev is not None:
                                    down(*prev)
                                prev = cur
                        down(*prev)
                        for ti in range(2):
                            t = tc2 * 2 + ti
                            yi = yc[0] % 2
                            yc[0] += 1
                            p.op("dve", lambda e, ti=ti, yi=yi: e.tensor_tensor(out=ytmp[yi], in0=ps_all[:, (4 + 2 * ti) * 512:(6 + 2 * ti) * 512], in1=gbc[:], op=ALU.mult),
                                 reads=[psk(4 + 2 * ti), psk(5 + 2 * ti), "gbc"], writes=["ytmp%d" % yi])
                            xb = xt4[yi]
                            xk = "xt%d" % yi
                            p.dma("sp", lambda e, xb=xb, t=t: [e.dma_start(out=xb, in_=xs[b, t * 128:(t + 1) * 128, :])], reads=[xkey(t)], writes=[xk], slot=("xt", yi))
                            p.op("pool", lambda e, xb=xb, yi=yi: e.tensor_tensor(out=xb, in0=xb, in1=ytmp[yi], op=ALU.add), reads=[xk, "ytmp%d" % yi], writes=[xk])
                            p.dma("sp", lambda e, xb=xb, t=t: [e.dma_start(out=xs[b, t * 128:(t + 1) * 128, :], in_=xb)], reads=[xk], writes=[xkey(t)], slot=("xst", 1 + yi))
                    if eg + 2 < 4:
                        for ei in range(4):
                            load_expert((eg + 2) * 4 + ei)
                bank_lim[0] = 8
                for t in range(NT):
                    xb = xt4[t % 2]
                    xk = "xt%d" % (t % 2)
                    p.dma("sp", lambda e, xb=xb, t=t: [e.dma_start(out=xb, in_=xs[b, t * 128:(t + 1) * 128, :])], reads=[xkey(t)], writes=[xk], slot=("xt", t % 2))
                    ln_stats(xb, mv, rstd, xk, "p4")
                    p.op("dve", lambda e, xb=xb: e.tensor_scalar(out=xb, in0=xb, scalar1=mv[:, 0:1], scalar2=rstd, op0=ALU.subtract, op1=ALU.mult),
                         reads=[xk, ("mv", "p4"), ("rstd", "p4")], writes=[xk])
                    p.op("pool", lambda e, xb=xb: e.tensor_tensor(out=xb, in0=xb, in1=lnp[:, 0, :], op=ALU.mult), reads=[xk, "lnp"], writes=[xk])
                    p.op("pool", lambda e, xb=xb: e.tensor_tensor(out=xb, in0=xb, in1=lnp[:, 1, :], op=ALU.add), reads=[xk, "lnp"], writes=[xk])
                    o_ = p.dma("sp", lambda e, xb=xb, t=t: [e.dma_start(out=x_dst[b, t * 128:(t + 1) * 128, :], in_=xb)], reads=[xk], writes=[xkey(t)], slot=("xout", t % 2))
                    if last:
                        finals.append(o_)

        for l_ in range(n_layers):
            phase0(l_)
            for b_ in range(n_seq):
                block(l_, b_)

        p.emit(final_wait_ops=finals)
    return nc


def prep_shared(inputs):
    f = lambda a: np.ascontiguousarray(np.asarray(a, dtype=np.float32))
    cos, sin, E, sel = _const_tables()
    b_ada = f(inputs["b_ada"])
    sh = {
        "w_ada": f(inputs["w_ada"]),
        "w_po": np.ascontiguousarray(np.concatenate([f(inputs["w_branch_a"]), f(inputs["w_branch_b"]), f(inputs["w_out"])], axis=1)),
        "b_adaT": np.ascontiguousarray(b_ada.reshape(DEPTH, 48, 128).transpose(2, 0, 1)),
        "w_in": f(inputs["w_in"]),
        "w_pa": f(inputs["w_branch_a"]),
        "w_pb": f(inputs["w_branch_b"]),
        "w_o": f(inputs["w_out"]),
        "w_r": f(inputs["w_router"]),
        "w_eg": f(inputs["w_exp_gate"]),
        "w_eu": f(inputs["w_exp_up"]),
        "w_ed": f(inputs["w_exp_down"]),
        "cos_t": cos, "sin_t": sin, "E_t": E, "sel_t": sel,
        "idn": np.eye(128, dtype=np.float32),
    }
    qg = f(inputs["q_norm_g"])
    kg = f(inputs["k_norm_g"])
    qk = np.concatenate([np.tile(qg[:, None, :], (1, 8, 1)), np.tile(kg[:, None, :], (1, 2, 1))], 1).reshape(DEPTH, 640)
    sh["qkg"] = np.ascontiguousarray(np.broadcast_to(qk[None], (128, DEPTH, 640)))
    lnp = np.stack([f(inputs["ln1_g"]), f(inputs["ln1_b"]), f(inputs["ln2_g"]), f(inputs["ln2_b"])], 1)
    sh["lnp"] = np.ascontiguousarray(np.broadcast_to(lnp[None], (128, DEPTH, 4, D)))
    sh["rb"] = np.ascontiguousarray(np.broadcast_to(f(inputs["router_bias"])[None], (128, 16)))
    return sh


_GATHERED = ("w_ada", "w_in", "w_po", "w_eg", "w_eu", "w_ed")


def prep_core(inputs, sh, core, n_cores=8):
    x = np.asarray(inputs["x"], dtype=np.float32)
    c = np.asarray(inputs["c"], dtype=np.float32)
    m = {k: v for k, v in sh.items() if k not in ("w_pa", "w_pb", "w_o")}
    for k in _GATHERED:
        w = m.pop(k)
        w2 = w.reshape(DEPTH, -1, w.shape[-1])
        if n_cores == 1 or not _GATHER:
            m[k] = w2
        else:
            r = w2.shape[1] // n_cores
            m[k + "_sh"] = np.ascontiguousarray(w2[:, core * r:(core + 1) * r, :])
    m["x"] = np.ascontiguousarray(x[2 * core:2 * core + 2])
    cc = c[2 * core:2 * core + 2]
    m["cT"] = np.ascontiguousarray(cc.reshape(2, 8, 128).transpose(2, 1, 0))
    return m


_NC_CACHE = {}


def kernel(**inputs):
    if "nc" not in _NC_CACHE:
        _NC_CACHE["nc"] = build_nc()
    nc = _NC_CACHE["nc"]
    sh = prep_shared(inputs)
    in_maps = [prep_core(inputs, sh, i) for i in range(8)]
    res = run_bass_kernel_spmd(nc, in_maps, core_ids=list(range(8)))
    return np.concatenate([np.asarray(r["y"]) for r in res.results], axis=0).astype(np.float32)
```
